# Optimizing a Trainium2 kernel written in Bass

```python
import math
import jax, jax.numpy as jnp
from jax import lax
import numpy as np

D_MODEL = 1024
BATCH = 8
SEQ = 2048
DEPTH = 4

POOL_WIDTH = 512
POOL_WINDOWS = (2, 4, 8, 16)
POOL_GROUPS = len(POOL_WINDOWS)
POOL_GROUP_DIM = POOL_WIDTH // POOL_GROUPS
GLA_HEADS = 4
GLA_DK = 64
GLA_DV = 128
GLA_KEY_WIDTH = GLA_HEADS * GLA_DK
GLA_VAL_WIDTH = GLA_HEADS * GLA_DV
GLA_GATE_RANK = 16
GLA_GATE_TAU = 16.0
GLA_CHUNK = 64
MLA_HEADS = 8
MLA_Q_RANK = 384
MLA_KV_RANK = 256
MLA_NOPE = 64
MLA_ROPE = 32
MLA_V = 64
MLA_QK = MLA_NOPE + MLA_ROPE
MLA_VAL_WIDTH = MLA_HEADS * MLA_V
ROPE_BASE = 10000.0
Q_BLOCK = 128
N_BRANCH = 3
D_FF = -(-8 * D_MODEL // (3 * 256)) * 256
EPS = 1e-6

IN_SIZES = (POOL_WIDTH, GLA_KEY_WIDTH, GLA_KEY_WIDTH, GLA_VAL_WIDTH, GLA_VAL_WIDTH, GLA_GATE_RANK,
            MLA_Q_RANK, MLA_KV_RANK, MLA_ROPE, N_BRANCH * D_MODEL)
IN_WIDTH = sum(IN_SIZES)
IN_OFFSETS = tuple(int(v) for v in np.cumsum(IN_SIZES)[:-1])

kernel_name = "hybrid_pool_gla_mla_sandwich"


def rms_norm(x, g):
    xf = x.astype(jnp.float32)
    y = xf * lax.rsqrt(jnp.mean(xf * xf, axis=-1, keepdims=True) + EPS)
    return (y * g.astype(jnp.float32)).astype(x.dtype)


def rope_tables(positions):
    inv_freq = ROPE_BASE ** (-jnp.arange(0, MLA_ROPE, 2, dtype=jnp.float32) / MLA_ROPE)
    ang = positions.astype(jnp.float32)[..., None] * inv_freq
    return jnp.cos(ang), jnp.sin(ang)


def apply_rope(x, cos, sin):
    half = x.shape[-1] // 2
    x1, x2 = x[..., :half], x[..., half:]
    return jnp.concatenate([x1 * cos - x2 * sin, x2 * cos + x1 * sin], axis=-1).astype(x.dtype)


def pool_mixer(u, w_pool, pool_scale):
    b, s, _ = u.shape
    uf = u.astype(jnp.float32).reshape(b, s, POOL_GROUPS, POOL_GROUP_DIM)
    cs = jnp.cumsum(uf, axis=1)
    t = jnp.arange(s)
    means = []
    for g, w in enumerate(POOL_WINDOWS):
        c = cs[:, :, g]
        prev = jnp.pad(c, ((0, 0), (w, 0), (0, 0)))[:, :s]
        cnt = jnp.minimum(t + 1, w).astype(jnp.float32)[None, :, None]
        means.append((c - prev) / cnt)
    pooled = jnp.stack(means, axis=2)
    diff = (pooled - uf).astype(u.dtype)
    y = jnp.einsum('bsgc,gcd->bsgd', diff, w_pool).reshape(b, s, POOL_WIDTH)
    return y * pool_scale


def gla_mixer(q, k, v, r, a1, w_a2, b_a, g_norm):
    b, s, _ = q.shape
    n = s // GLA_CHUNK
    f32 = jnp.float32
    log_a = jax.nn.log_sigmoid((a1 @ w_a2 + b_a).astype(f32)) / GLA_GATE_TAU

    def heads(t, d):
        return t.astype(f32).reshape(b, n, GLA_CHUNK, GLA_HEADS, d).transpose(0, 3, 1, 2, 4)

    qh = heads(q, GLA_DK) * (GLA_DK ** -0.5)
    kh = heads(k, GLA_DK)
    vh = heads(v, GLA_DV)
    cum = jnp.cumsum(heads(log_a, GLA_DK), axis=3)
    cum_last = cum[:, :, :, -1:, :]
    q_dec = qh * jnp.exp(cum)
    k_inv = kh * jnp.exp(-cum)
    k_end = kh * jnp.exp(cum_last - cum)
    mask = jnp.tril(jnp.ones((GLA_CHUNK, GLA_CHUNK), dtype=bool))
    att = jnp.where(mask, jnp.einsum('bhntd,bhnsd->bhnts', q_dec, k_inv), 0.0)
    o_intra = jnp.einsum('bhnts,bhnse->bhnte', att, vh)
    kv = jnp.einsum('bhnsd,bhnse->bhnde', k_end, vh)
    decay = jnp.exp(cum_last[:, :, :, 0, :])

    def step(state, inp):
        dec, kv_n = inp
        return dec[..., None] * state + kv_n, state

    s0 = jnp.zeros((b, GLA_HEADS, GLA_DK, GLA_DV), f32)
    _, states = lax.scan(step, s0, (jnp.moveaxis(decay, 2, 0), jnp.moveaxis(kv, 2, 0)))
    states = jnp.moveaxis(states, 0, 2)
    o_inter = jnp.einsum('bhntd,bhnde->bhnte', q_dec, states)
    o = (o_intra + o_inter).transpose(0, 2, 3, 1, 4).reshape(b, s, GLA_HEADS, GLA_DV)
    o = o * lax.rsqrt(jnp.mean(o * o, axis=-1, keepdims=True) + EPS)
    o = o * g_norm.astype(f32).reshape(GLA_HEADS, GLA_DV)
    o = o.reshape(b, s, GLA_VAL_WIDTH) * jax.nn.silu(r.astype(f32))
    return o.astype(q.dtype)


def mla_mixer(cq, ckv, kr, cos, sin, q_norm, w_uq, kv_norm, w_ukv):
    b, s, _ = cq.shape
    qf = (rms_norm(cq, q_norm) @ w_uq).reshape(b, s, MLA_HEADS, MLA_QK)
    kvf = (rms_norm(ckv, kv_norm) @ w_ukv).reshape(b, s, MLA_HEADS, MLA_NOPE + MLA_V)
    q_nope, q_rope = qf[..., :MLA_NOPE], qf[..., MLA_NOPE:]
    k_nope, v = kvf[..., :MLA_NOPE], kvf[..., MLA_NOPE:]
    q_rope = apply_rope(q_rope, cos[:, :, None], sin[:, :, None])
    k_rope = apply_rope(kr, cos, sin)
    q = jnp.concatenate([q_nope, q_rope], axis=-1)
    k = jnp.concatenate([k_nope, jnp.broadcast_to(k_rope[:, :, None], (b, s, MLA_HEADS, MLA_ROPE))], axis=-1)
    nb = s // Q_BLOCK
    qb = q.reshape(b, nb, Q_BLOCK, MLA_HEADS, MLA_QK).transpose(1, 0, 2, 3, 4)
    starts = jnp.arange(nb) * Q_BLOCK
    kpos = jnp.arange(s)
    scale = MLA_QK ** -0.5

    def attend(args):
        q_blk, start = args
        sc = jnp.einsum('bqhd,bkhd->bhqk', q_blk, k).astype(jnp.float32) * scale
        qpos = start + jnp.arange(Q_BLOCK)
        sc = jnp.where(qpos[:, None] >= kpos[None, :], sc, jnp.finfo(jnp.float32).min)
        p = jax.nn.softmax(sc, axis=-1)
        return jnp.einsum('bhqk,bkhe->bqhe', p.astype(v.dtype), v)

    o = lax.map(attend, (qb, starts))
    return o.transpose(1, 0, 2, 3, 4).reshape(b, s, MLA_VAL_WIDTH)


def hybrid_layer(x, cos, sin, n_pre_mix, n_post_mix, n_pre_ffn, n_post_ffn, w_in, w_pool, pool_scale, w_a,
                 w_gla_a2, b_gla_a, gla_norm, w_b, mla_q_norm, w_mla_uq, mla_kv_norm, w_mla_ukv, w_c, w_o,
                 w_ffn_gu, w_ffn_down):
    b, s, d = x.shape
    h = rms_norm(x, n_pre_mix)
    proj = h @ w_in
    (u_pool, g_q, g_k, g_v, g_r, g_a1, m_cq, m_ckv, m_kr, gate_logits) = jnp.split(proj, IN_OFFSETS, axis=-1)
    y_a = pool_mixer(u_pool, w_pool, pool_scale) @ w_a
    y_b = gla_mixer(g_q, g_k, g_v, g_r, g_a1, w_gla_a2, b_gla_a, gla_norm) @ w_b
    y_c = mla_mixer(m_cq, m_ckv, m_kr, cos, sin, mla_q_norm, w_mla_uq, mla_kv_norm, w_mla_ukv) @ w_c
    gates = jax.nn.sigmoid(gate_logits.astype(jnp.float32)).astype(x.dtype).reshape(b, s, N_BRANCH, d)
    merged = gates[:, :, 0] * y_a + gates[:, :, 1] * y_b + gates[:, :, 2] * y_c
    x = x + rms_norm(merged @ w_o, n_post_mix)
    h = rms_norm(x, n_pre_ffn)
    gu = h @ w_ffn_gu
    ffn = (jax.nn.silu(gu[..., :D_FF]) * gu[..., D_FF:]) @ w_ffn_down
    return x + rms_norm(ffn, n_post_ffn)


def setup_inputs(seed: int = 0) -> dict:
    key = jax.random.key(seed)
    ks = jax.random.split(key, 24)
    f32 = jnp.float32

    def dense(k, shape, fan_in):
        return jax.random.normal(k, shape, f32) * (fan_in ** -0.5)

    def gain(k, shape):
        return 1.0 + 0.02 * jax.random.normal(k, shape, f32)

    x = jax.random.normal(ks[0], (BATCH, SEQ, D_MODEL), f32)
    offsets = jax.random.randint(ks[1], (BATCH, 1), 0, 4096, dtype=jnp.int32)
    positions = offsets + jnp.arange(SEQ, dtype=jnp.int32)[None, :]
    return {
        "x": x,
        "positions": positions,
        "norm_pre_mix": gain(ks[2], (DEPTH, D_MODEL)),
        "norm_post_mix": gain(ks[3], (DEPTH, D_MODEL)),
        "norm_pre_ffn": gain(ks[4], (DEPTH, D_MODEL)),
        "norm_post_ffn": gain(ks[5], (DEPTH, D_MODEL)),
        "w_in": dense(ks[6], (DEPTH, D_MODEL, IN_WIDTH), D_MODEL),
        "w_pool": dense(ks[7], (DEPTH, POOL_GROUPS, POOL_GROUP_DIM, POOL_GROUP_DIM), POOL_GROUP_DIM),
        "pool_scale": gain(ks[8], (DEPTH, POOL_WIDTH)),
        "w_a": dense(ks[9], (DEPTH, POOL_WIDTH, D_MODEL), POOL_WIDTH),
        "w_gla_a2": dense(ks[10], (DEPTH, GLA_GATE_RANK, GLA_KEY_WIDTH), GLA_GATE_RANK),
        "b_gla_a": 0.1 * jax.random.normal(ks[11], (DEPTH, GLA_KEY_WIDTH), f32),
        "gla_norm": gain(ks[12], (DEPTH, GLA_VAL_WIDTH)),
        "w_b": dense(ks[13], (DEPTH, GLA_VAL_WIDTH, D_MODEL), GLA_VAL_WIDTH),
        "mla_q_norm": gain(ks[14], (DEPTH, MLA_Q_RANK)),
        "w_mla_uq": dense(ks[15], (DEPTH, MLA_Q_RANK, MLA_HEADS * MLA_QK), MLA_Q_RANK),
        "mla_kv_norm": gain(ks[16], (DEPTH, MLA_KV_RANK)),
        "w_mla_ukv": dense(ks[17], (DEPTH, MLA_KV_RANK, MLA_HEADS * (MLA_NOPE + MLA_V)), MLA_KV_RANK),
        "w_c": dense(ks[18], (DEPTH, MLA_VAL_WIDTH, D_MODEL), MLA_VAL_WIDTH),
        "w_o": dense(ks[19], (DEPTH, D_MODEL, D_MODEL), D_MODEL),
        "w_ffn_gu": dense(ks[20], (DEPTH, D_MODEL, 2 * D_FF), D_MODEL),
        "w_ffn_down": dense(ks[21], (DEPTH, D_FF, D_MODEL), D_FF),
    }


def reference(x, positions, norm_pre_mix, norm_post_mix, norm_pre_ffn, norm_post_ffn, w_in, w_pool,
              pool_scale, w_a, w_gla_a2, b_gla_a, gla_norm, w_b, mla_q_norm, w_mla_uq, mla_kv_norm,
              w_mla_ukv, w_c, w_o, w_ffn_gu, w_ffn_down):
    cos, sin = rope_tables(positions)
    for l in range(DEPTH):
        x = hybrid_layer(x, cos, sin, norm_pre_mix[l], norm_post_mix[l], norm_pre_ffn[l], norm_post_ffn[l],
                         w_in[l], w_pool[l], pool_scale[l], w_a[l], w_gla_a2[l], b_gla_a[l], gla_norm[l],
                         w_b[l], mla_q_norm[l], w_mla_uq[l], mla_kv_norm[l], w_mla_ukv[l], w_c[l], w_o[l],
                         w_ffn_gu[l], w_ffn_down[l])
    return x
```

```python
import numpy as np
from contextlib import ExitStack
import concourse.bass as bass
import concourse.mybir as mybir

F32 = mybir.dt.float32
BF16 = mybir.dt.bfloat16
I32 = mybir.dt.int32
AF = mybir.ActivationFunctionType
ALU = mybir.AluOpType
AX = mybir.AxisListType
ESZ = {F32: 4, BF16: 2, I32: 4}

SAME_ENGINE_SYNC = True
SEM_EPOCH = 30000
N_DMA_SEMS = 8


class View:
    def __init__(self, ap, space, p0, p1, b0, b1):
        self.ap, self.space, self.p0, self.p1, self.b0, self.b1 = ap, space, p0, p1, b0, b1

    def r(self, pattern, **kw):
        return View(self.ap.rearrange(pattern, **kw), self.space, self.p0, self.p1, self.b0, self.b1)

    def bc(self, shape):
        return View(self.ap.to_broadcast(shape), self.space, self.p0, self.p1, self.b0, self.b1)

    def bitcast(self, dt):
        return View(self.ap.bitcast(dt), self.space, self.p0, self.p1, self.b0, self.b1)

    def sub(self, fn):
        return View(fn(self.ap), self.space, self.p0, self.p1, self.b0, self.b1)


class Buf:
    def __init__(self, handle, space, P, F, dtype, off):
        self.t, self.space, self.P, self.F, self.dtype, self.off = handle, space, P, F, dtype, off
        self.esz = ESZ[dtype]

    def __getitem__(self, key):
        ps, cs = key
        p0, p1, _ = ps.indices(self.P)
        c0, c1, _ = cs.indices(self.F)
        return View(self.t[p0:p1, c0:c1], self.space, p0, p1,
                    self.off + c0 * self.esz, self.off + c1 * self.esz)

    def all(self):
        return self[:, :]


class DView(View):
    pass


def dram_view(ap, name, lo, hi):
    return View(ap, "d:" + name, 0, 1, lo, hi)


class Ins:
    __slots__ = ("eng", "fn", "kw", "waits", "tok", "inc", "is_dma")


class EngProxy:
    def __init__(self, K, eng):
        self.K, self.eng = K, eng

    def __getattr__(self, fn):
        def call(**kw):
            return self.K._record(self.eng, fn, kw)
        return call


OUT_KEYS = ("out", "accum_out", "ap")


class Kern:
    def __init__(self, nc):
        self.nc = nc
        self.es = ExitStack()
        self.sb_off = 229376 - nc.sbuf_bytes_remaining
        self.sb_end = 229376
        self.streams = {e: [] for e in ("tensor", "vector", "scalar", "gpsimd", "sync")}
        self.sems = {}
        self.cnt = {e: 0 for e in self.streams}
        self.dma_sems = {}
        self.dma_rr = {e: 0 for e in self.streams}
        self.seen = {e: {} for e in self.streams}
        self.recs = {}
        self.T = EngProxy(self, "tensor")
        self.V = EngProxy(self, "vector")
        self.A = EngProxy(self, "scalar")
        self.G = EngProxy(self, "gpsimd")
        self.S = EngProxy(self, "sync")
        self.nsem = 0
        self.nbuf = 0

    def sem(self, name):
        self.nsem += 1
        return self.es.enter_context(self.nc.semaphore(name))

    def sbuf(self, P, F, dtype, name=None, off=None):
        esz = ESZ[dtype]
        nbytes = F * esz
        if off is None:
            off = (self.sb_off + 63) // 64 * 64
            self.sb_off = off + nbytes
            assert self.sb_off <= self.sb_end, (name, self.sb_off)
        self.nbuf += 1
        name = f"{name or 'sb'}_{self.nbuf}"
        h = self.nc.alloc_sbuf_tensor_at(name, [P, F], dtype, offset=off)
        return Buf(h, "sb", P, F, dtype, off)

    def psum_banks(self, n=8):
        banks = []
        for i in range(n):
            h = self.es.enter_context(self.nc.psum_tensor(f"psb{i}", [128, 512], F32))
            banks.append(Buf(h, "ps", 128, 512, F32, i * 2048))
        return banks

    def _tok_compute(self, eng):
        self.cnt[eng] += 1
        c = self.cnt[eng]
        ep = (c - 1) // SEM_EPOCH
        lst = self.sems.setdefault(eng, [])
        while len(lst) <= ep:
            lst.append(self.sem(f"s_{eng}_{len(lst)}"))
        return (lst[ep], c - ep * SEM_EPOCH)

    def _record(self, eng, fn, kw):
        ins = Ins()
        ins.eng, ins.fn, ins.kw, ins.is_dma = eng, fn, kw, fn.startswith("dma_start")
        reads, writes = [], []
        for k, v in list(kw.items()):
            if isinstance(v, View):
                if v.space == "ps":
                    bb = (v.b0 // 2048) * 2048
                    v = View(v.ap, "ps", (v.p0 // 32) * 32, ((v.p1 + 31) // 32) * 32, bb, bb + 2048)
                    kw[k] = v
                (writes if k in OUT_KEYS else reads).append(v)
        deps = {}

        def add_dep(tok):
            s, val = tok
            if deps.get(id(s), (None, 0))[1] < val:
                deps[id(s)] = (s, val)

        for v, is_w in [(x, False) for x in reads] + [(x, True) for x in writes]:
            lst = self.recs.setdefault(v.space, [])
            for r in lst:
                if r[0] < v.p1 and v.p0 < r[1] and r[2] < v.b1 and v.b0 < r[3] and (is_w or r[4] or (v.space == "ps" and r[5] != eng)):
                    if r[5] == eng and not r[6] and not ins.is_dma:
                        if eng == "tensor" or not SAME_ENGINE_SYNC:
                            continue
                    add_dep(r[7])
        if ins.is_dma:
            pool = self.dma_sems.setdefault(eng, [])
            if len(pool) < N_DMA_SEMS:
                pool.append([self.sem(f"d_{eng}_{len(pool)}"), 0])
            i = self.dma_rr[eng] % len(pool) if len(pool) == N_DMA_SEMS else len(pool) - 1
            self.dma_rr[eng] += 1
            ent = pool[i]
            if ent[1] > 0:
                add_dep((ent[0], ent[1]))
            ent[1] += 16
            ins.tok, ins.inc = (ent[0], ent[1]), 16
        else:
            ins.tok, ins.inc = self._tok_compute(eng), 1
        seen = self.seen[eng]
        ins.waits = []
        for s, val in deps.values():
            if seen.get(id(s), 0) < val:
                seen[id(s)] = val
                ins.waits.append((s, val))
        for v, is_w in [(x, False) for x in reads] + [(x, True) for x in writes]:
            lst = self.recs[v.space]
            new = []
            for r in lst:
                covered = v.p0 <= r[0] and r[1] <= v.p1 and v.b0 <= r[2] and r[3] <= v.b1
                if covered and (is_w or (not r[4] and ((r[5] == eng and not r[6] and not ins.is_dma) or v.space == "ps"))):
                    continue
                new.append(r)
            new.append([v.p0, v.p1, v.b0, v.b1, is_w, eng, ins.is_dma, ins.tok])
            self.recs[v.space] = new
        self.streams[eng].append(ins)
        return ins

    def wait_all(self, eng, toks):
        ins = Ins()
        ins.eng, ins.fn, ins.kw, ins.is_dma = eng, None, {}, False
        ins.waits = list(toks)
        ins.tok, ins.inc = None, 0
        self.streams[eng].append(ins)

    def emit(self):
        nc = self.nc
        block = self.es.enter_context(nc.Block())

        def run(engname):
            def body(e):
                for ins in self.streams[engname]:
                    for s, val in ins.waits:
                        e.wait_ge(s, val)
                    if ins.fn is None:
                        continue
                    kw = {k: (v.ap if isinstance(v, View) else v) for k, v in ins.kw.items()}
                    r = getattr(e, ins.fn)(**kw)
                    r.then_inc(ins.tok[0], ins.inc)
            return body

        block.tensor(run("tensor"))
        block.vector(run("vector"))
        block.scalar(run("scalar"))
        block.gpsimd(run("gpsimd"))
        block.sync(run("sync"))

    def close(self):
        self.es.close()


import math
from concourse.bass_utils import run_bass_kernel_spmd

D = 1024
S = 2048
L_FULL = 4
DFF = 2816
NV = 45
NCST = 850
EPS = 1e-6
TT = 512
NTT = S // TT
W1C = 5840
POOL0, QK0, R0, V0, CQ0, CKV0, SM0, G0 = 0, 512, 1024, 1536, 2048, 2432, 2688, 2768
WSLOT = 4096
NSLOT = 3
C1_2PI = 6.28125
C2_2PI = 2.0 * math.pi - 6.28125
import os
NOALIAS0 = bool(int(os.environ.get('NOALIAS0', '0')))


class _Stop(Exception):
    pass


def build_program(depth=L_FULL, debug=(), stop=None):
    nc = bass.Bass("TRN2", target_bir_lowering=False)
    K = Kern(nc)

    def din(name, shape, dt=F32):
        return nc.dram_tensor(name, shape, dt, kind="ExternalInput").ap()

    x_in = din("x", [S, D])
    pos_in = din("pos", [128, S], I32)
    cst_in = din("cst", [128, NCST])
    vecs_in = din("vecs", [128, L_FULL * NV])
    W1 = din("w1", [L_FULL * D, W1C])
    WP = din("wp", [L_FULL * 128, 512])
    WABC = din("wabc", [L_FULL * 1536, D])
    WA2 = din("wa2", [L_FULL * 16, 256])
    BA = din("ba", [L_FULL, 256])
    WUQ = din("wuq", [L_FULL * 384, 768])
    WUQS = din("wuqs", [L_FULL * 384, 768])
    WUKV = din("wukv", [L_FULL * 256, 1024])
    WO = din("wo", [L_FULL * D, D])
    WGU = din("wgu", [L_FULL * D, 2 * DFF])
    WD = din("wd", [L_FULL * DFF, D])
    out_d = nc.dram_tensor("out", [S, D], F32, kind="ExternalOutput").ap()
    xs_d = nc.dram_tensor("xs", [128, 8 * S], F32, kind="Internal").ap()
    dbg_out = {}

    banks = []
    for i in range(7):
        h = K.es.enter_context(nc.psum_tensor(f"psb{i}", [128, 512], F32))
        banks.append(Buf(h, "ps", 128, 512, F32, i * 2048))
    hb = K.es.enter_context(nc.psum_tensor("psb7", [128, 1024], BF16))
    bankT = Buf(hb, "ps", 128, 1024, BF16, 7 * 2048)
    rr_state = {"list": list(range(7)), "i": 0}

    def set_rr(lst):
        rr_state["list"], rr_state["i"] = list(lst), 0

    def rr():
        b = banks[rr_state["list"][rr_state["i"] % len(rr_state["list"])]]
        rr_state["i"] += 1
        return b

    cst = K.sbuf(128, NCST, F32, "cst")
    vecs = K.sbuf(128, L_FULL * NV, F32, "vecs")
    ident_b = K.sbuf(128, 128, BF16, "identb")
    ones_b = K.sbuf(128, 128, BF16, "onesb")
    ones_f = K.sbuf(128, 128, F32, "onesf")
    maskneg_b = K.sbuf(128, 512, BF16, "maskneg")
    epsc = K.sbuf(128, 1, F32, "eps")
    ropeC = K.sbuf(128, S, F32, "ropeC")
    ropeS = K.sbuf(128, S, F32, "ropeS")
    hT = K.sbuf(128, 8 * S, BF16, "hT")
    wslots = [K.sbuf(128, WSLOT, BF16, f"wslot{i}") for i in range(NSLOT)]
    rs_sb = K.sbuf(128, 512, F32, "rs")
    ident_fb = K.sbuf(128, 128, F32, "identf")
    U_fb = K.sbuf(128, 128, F32, "Uf")
    ident_f = ident_fb.all()
    U_f = U_fb.all()
    arena0 = K.sb_off
    ARENA_END = K.sb_end
    gmask = cst[:, 256:320]
    invcnt = cst[:, 320:336]
    invf = cst[:, 336:337]
    sgn = cst[:, 337:338]

    class Arena:
        def __init__(self):
            self.off = arena0

        def alloc(self, P, F, dt, name):
            b = K.sbuf(P, F, dt, name, off=(self.off + 63) // 64 * 64)
            self.off = b.off + F * ESZ[dt]
            assert self.off <= ARENA_END, (name, self.off, ARENA_END)
            return b

    ws_i = [0]

    def wload(W2d, row0, nk, c0, ncols):
        assert nk * ncols <= WSLOT
        slot = wslots[ws_i[0] % NSLOT]
        ws_i[0] += 1
        dst = slot[:, 0:nk * ncols]
        src = W2d[row0:row0 + nk * 128, c0:c0 + ncols].rearrange("(k p) n -> p k n", p=128)
        K.G.dma_start(out=dst.r("p (k n) -> p k n", k=nk), in_=src)

        def w(k, a, b):
            return slot[:, k * ncols + a:k * ncols + b]
        return w

    evac_i = [0]

    def evac_copy(out, in_):
        evac_i[0] += 1
        if evac_i[0] % 2:
            K.A.copy(out=out, in_=in_)
        else:
            K.V.tensor_copy(out=out, in_=in_)

    def hcol(c, t0, t1):
        return hT[:, c * S + t0:c * S + t1]

    def vcol(l, j):
        return vecs[:, l * NV + j:l * NV + j + 1]

    def dbg(name, view, P, F, dt=F32):
        if name not in debug:
            return
        d = nc.dram_tensor("dbg_" + name, [P, F], dt, kind="ExternalOutput").ap()
        i = K.S.dma_start(out=dram_view(d, "dbg_" + name, 0, 1), in_=view)
        dbg_out[name] = i.tok

    def rstd_from(bank_view, n, P=128):
        K.A.activation(out=rs_sb[0:P, :], in_=bank_view, func=AF.Sqrt, bias=epsc[0:P, :], scale=1.0 / n)
        K.V.reciprocal(out=rs_sb[0:P, :], in_=rs_sb[0:P, :])

    def norm_to_h(xt, sq, l_next, gbase, tt):
        bk = rr()
        for c in range(8):
            K.A.activation(out=sq[c % 2].all(), in_=xt[:, c * TT:(c + 1) * TT], func=AF.Square)
            K.T.matmul(out=bk.all(), lhsT=ones_b.all(), rhs=sq[c % 2].all(), start=(c == 0), stop=(c == 7))
        rstd_from(bk.all(), D)
        for c in range(8):
            K.V.scalar_tensor_tensor(out=hcol(c, tt * TT, (tt + 1) * TT), in0=xt[:, c * TT:(c + 1) * TT],
                                     scalar=vcol(l_next, gbase + c), in1=rs_sb.all(), op0=ALU.mult, op1=ALU.mult)

    def xs_view(tt):
        ap = xs_d.rearrange("p (c t) -> p c t", c=8)[:, :, tt * TT:(tt + 1) * TT]
        return dram_view(ap, "xs", tt, tt + 1)

    out_toks = []

    def ckpt(name):
        if stop == name:
            raise _Stop()

    try:
        K.S.dma_start(out=cst.all(), in_=cst_in)
        K.S.dma_start(out=vecs.all(), in_=vecs_in)
        K.V.tensor_copy(out=ident_f, in_=cst[:, 0:128])
        K.V.tensor_copy(out=U_f, in_=cst[:, 128:256])
        K.V.tensor_copy(out=ident_b.all(), in_=cst[:, 0:128])
        K.V.memset(ap=ones_b.all(), constant=1.0)
        K.V.memset(ap=ones_f.all(), constant=1.0)
        K.V.memset(ap=epsc.all(), constant=EPS)
        K.V.tensor_copy(out=maskneg_b.all(), in_=cst[:, 338:850])

        A = Arena()
        pos_i = A.alloc(128, S, I32, "posi")
        ang = A.alloc(128, S, F32, "ang")
        nfl = A.alloc(128, S, F32, "nfl")
        n_i = A.alloc(128, S, I32, "ni")
        K.S.dma_start(out=pos_i.all(), in_=pos_in)
        R = slice(64, 96)
        K.V.tensor_copy(out=ang[R, :], in_=pos_i[R, :])
        K.V.tensor_scalar(out=ang[R, :], in0=ang[R, :], scalar1=cst[R, 336:337], scalar2=None, op0=ALU.mult)
        for which, table in ((0, ropeS), (1, ropeC)):
            if which == 1:
                K.V.tensor_scalar(out=ang[R, :], in0=ang[R, :], scalar1=math.pi / 2, scalar2=None, op0=ALU.add)
            K.V.tensor_scalar(out=nfl[R, :], in0=ang[R, :], scalar1=1.0 / (2 * math.pi), scalar2=None, op0=ALU.mult)
            K.V.tensor_copy(out=n_i[R, :], in_=nfl[R, :])
            K.V.tensor_copy(out=nfl[R, :], in_=n_i[R, :])
            K.V.scalar_tensor_tensor(out=table[R, :], in0=nfl[R, :], scalar=-C1_2PI, in1=ang[R, :], op0=ALU.mult, op1=ALU.add)
            K.V.scalar_tensor_tensor(out=table[R, :], in0=nfl[R, :], scalar=-C2_2PI, in1=table[R, :], op0=ALU.mult, op1=ALU.add)
            K.V.tensor_scalar(out=table[R, :], in0=table[R, :], scalar1=-3.14159, scalar2=3.14159, op0=ALU.max, op1=ALU.min)
            K.A.activation(out=table[R, :], in_=table[R, :], func=AF.Sin)
        K.V.tensor_scalar(out=ropeS[R, :], in0=ropeS[R, :], scalar1=cst[R, 337:338], scalar2=None, op0=ALU.mult)
        dbg("ropeC", ropeC[R, :], 32, S)
        dbg("ropeS", ropeS[R, :], 32, S)
        ckpt("setup")

        if not NOALIAS0:
            A = Arena()
        xt = A.alloc(128, 8 * TT, F32, "xt")
        sq = [A.alloc(128, TT, BF16, f"sq{i}") for i in range(2)]
        xin = [A.alloc(128, D, F32, f"xin{i}") for i in range(2)]
        set_rr(range(7))
        for tt in range(NTT):
            for st in range(4):
                xi = xin[(tt * 4 + st) % 2]
                r0 = (tt * 4 + st) * 128
                K.S.dma_start(out=xi.all(), in_=x_in[r0:r0 + 128, :])
                for half in range(2):
                    bk = rr()
                    for j in range(4):
                        c = half * 4 + j
                        K.T.transpose(out=bk[:, j * 128:(j + 1) * 128], in_=xi[:, c * 128:(c + 1) * 128], identity=ident_f)
                    for j in range(4):
                        c = half * 4 + j
                        evac_copy(xt[:, c * TT + st * 128:c * TT + (st + 1) * 128], bk[:, j * 128:(j + 1) * 128])
            dbg("xt0", xt.all(), 128, 8 * TT)
            ckpt("p0a")
            K.S.dma_start(out=xs_view(tt), in_=xt.all().r("p (c t) -> p c t", c=8))
            ckpt("p0b")
            norm_to_h(xt, sq, 0, 0, tt)
            ckpt("p0c")
        dbg("h0", hT.all(), 128, 8 * S, BF16)
        ckpt("phase0")

        for l in range(depth):
            A = Arena()
            ya_in = A.alloc(128, 4 * S, BF16, "ya_in")
            gla_out = A.alloc(128, 4 * S, BF16, "gla_out")
            attn_out = A.alloc(128, 4 * S, BF16, "attn_out")
            mix0 = A.off
            PAD = 16
            ubuf = [A.alloc(128, PAD + S, F32, f"ubuf{i}") for i in range(3)]
            diff = A.alloc(128, S, BF16, "diff")
            ptmp = A.alloc(128, 16, F32, "ptmp")
            set_rr(range(7))
            for i in range(3):
                K.V.memset(ap=ubuf[i][:, 0:PAD], constant=0.0)
            wpool_in = wload(W1, l * D, 8, POOL0, 512)
            wpp = wload(WP, l * 128, 1, 0, 512)
            for g in range(4):
                w = 2 ** (g + 1)
                for tt in range(NTT):
                    bk = rr()
                    for kc in range(8):
                        K.T.matmul(out=bk.all(), lhsT=wpool_in(kc, g * 128, (g + 1) * 128), rhs=hcol(kc, tt * TT, (tt + 1) * TT),
                                   start=(kc == 0), stop=(kc == 7))
                    evac_copy(ubuf[0][:, PAD + tt * TT:PAD + (tt + 1) * TT], bk.all())
                src = 0
                for j in range(g + 1):
                    d = 2 ** j
                    dst = 1 if src != 1 else 2
                    K.V.tensor_tensor(out=ubuf[dst][:, PAD:PAD + S], in0=ubuf[src][:, PAD:PAD + S],
                                      in1=ubuf[src][:, PAD - d:PAD - d + S], op=ALU.add)
                    src = dst
                sfin = ubuf[src]
                K.V.scalar_tensor_tensor(out=diff.all(), in0=sfin[:, PAD:PAD + S], scalar=1.0 / w, in1=ubuf[0][:, PAD:PAD + S],
                                         op0=ALU.mult, op1=ALU.subtract)
                K.V.tensor_tensor(out=ptmp[:, 0:w - 1], in0=sfin[:, PAD:PAD + w - 1], in1=cst[:, 320:320 + w - 1], op=ALU.mult)
                K.V.tensor_tensor(out=diff[:, 0:w - 1], in0=ptmp[:, 0:w - 1], in1=ubuf[0][:, PAD:PAD + w - 1], op=ALU.subtract)
                for tt in range(NTT):
                    bk = rr()
                    K.T.matmul(out=bk.all(), lhsT=wpp(0, g * 128, (g + 1) * 128), rhs=diff[:, tt * TT:(tt + 1) * TT], start=True, stop=True)
                    K.V.tensor_scalar(out=ya_in[:, g * S + tt * TT:g * S + (tt + 1) * TT], in0=bk.all(), scalar1=vcol(l, 32 + g),
                                      scalar2=None, op0=ALU.mult)
            if l == 0:
                dbg("ya_in", ya_in.all(), 128, 4 * S, BF16)
            ckpt("pool")

            A.off = mix0
            qdec = A.alloc(128, 2 * S, BF16, "qdec")
            kinv = A.alloc(128, 2 * S, BF16, "kinv")
            v_tm = A.alloc(128, 16 * 512, BF16, "v_tm")
            kinv_tm = A.alloc(128, 16 * 256, BF16, "kinv_tm")
            a1T = A.alloc(16, S, F32, "a1T")
            la = A.alloc(128, 256, F32, "la")
            Eq = A.alloc(128, 2 * TT, F32, "Eq")
            Ek = A.alloc(128, 2 * TT, F32, "Ek")
            dec = A.alloc(128, 2 * 32, F32, "dec")
            attm = [A.alloc(128, 64, BF16, f"attm{i}") for i in range(2)]
            S_f = [A.alloc(128, 128, F32, f"S_f{h}") for h in range(4)]
            S_t = [A.alloc(128, 128, F32, f"S_t{h}") for h in range(4)]
            S_b = [A.alloc(128, 128, BF16, f"S_b{h}") for h in range(4)]
            osq = A.alloc(128, TT, BF16, "osq")
            otmp = A.alloc(128, TT, F32, "otmp")
            wa2_sb = A.alloc(16, 256, F32, "wa2")
            ba_sb = A.alloc(1, 256, F32, "ba")
            set_rr(range(5))
            K.S.dma_start(out=wa2_sb.all(), in_=WA2[l * 16:(l + 1) * 16, :])
            K.S.dma_start(out=ba_sb.all(), in_=BA[l:l + 1, :])
            wr = wload(W1, l * D, 8, R0, 512)
            for fc in range(4):
                for tt in range(NTT):
                    bk = rr()
                    for kc in range(8):
                        K.T.matmul(out=bk.all(), lhsT=wr(kc, fc * 128, (fc + 1) * 128), rhs=hcol(kc, tt * TT, (tt + 1) * TT),
                                   start=(kc == 0), stop=(kc == 7))
                    K.A.activation(out=gla_out[:, fc * S + tt * TT:fc * S + (tt + 1) * TT], in_=bk.all(), func=AF.Silu)
            wsm = wload(W1, l * D, 8, SM0, 80)
            for tt in range(NTT):
                bk = rr()
                for kc in range(8):
                    K.T.matmul(out=bk[0:16, :], lhsT=wsm(kc, 64, 80), rhs=hcol(kc, tt * TT, (tt + 1) * TT), start=(kc == 0), stop=(kc == 7))
                evac_copy(a1T[0:16, tt * TT:(tt + 1) * TT], bk[0:16, :])
            wv = wload(W1, l * D, 8, V0, 512)
            for st in range(16):
                bk = rr()
                for kc in range(8):
                    K.T.matmul(out=bk.all(), lhsT=hcol(kc, st * 128, (st + 1) * 128), rhs=wv(kc, 0, 512), start=(kc == 0), stop=(kc == 7))
                evac_copy(v_tm[:, st * 512:(st + 1) * 512], bk.all())
            wqk = wload(W1, l * D, 8, QK0, 512)
            for tt in range(NTT):
                for s4 in range(4):
                    st = tt * 4 + s4
                    bz = rr()
                    K.T.matmul(out=bz[:, 0:256], lhsT=a1T[0:16, st * 128:(st + 1) * 128], rhs=wa2_sb.all(), start=True, stop=False)
                    K.T.matmul(out=bz[:, 0:256], lhsT=ones_f[0:1, 0:128], rhs=ba_sb.all(), start=False, stop=True)
                    K.A.activation(out=la.all(), in_=bz[:, 0:256], func=AF.Exp, scale=-1.0)
                    K.A.activation(out=la.all(), in_=la.all(), func=AF.Ln, bias=ones_f[:, 0:1], scale=1.0)
                    bc = rr()
                    for fc in range(2):
                        K.T.matmul(out=bc[:, fc * 128:(fc + 1) * 128], lhsT=la[:, fc * 128:(fc + 1) * 128], rhs=U_f, start=True, stop=True)
                    for fc in range(2):
                        K.A.activation(out=Eq[:, fc * TT + s4 * 128:fc * TT + (s4 + 1) * 128], in_=bc[:, fc * 128:(fc + 1) * 128],
                                       func=AF.Exp, scale=-1.0 / 16.0)
                        K.A.activation(out=Ek[:, fc * TT + s4 * 128:fc * TT + (s4 + 1) * 128], in_=bc[:, fc * 128:(fc + 1) * 128],
                                       func=AF.Exp, scale=1.0 / 16.0)
                        for hf in range(2):
                            n = st * 2 + hf
                            K.V.tensor_copy(out=dec[:, fc * 32 + n:fc * 32 + n + 1],
                                            in_=Eq[:, fc * TT + s4 * 128 + hf * 64 + 63:fc * TT + s4 * 128 + hf * 64 + 64])
                for fc in range(2):
                    bq = rr()
                    for kc in range(8):
                        K.T.matmul(out=bq.all(), lhsT=wqk(kc, fc * 128, (fc + 1) * 128), rhs=hcol(kc, tt * TT, (tt + 1) * TT),
                                   start=(kc == 0), stop=(kc == 7))
                    K.V.scalar_tensor_tensor(out=qdec[:, fc * S + tt * TT:fc * S + (tt + 1) * TT], in0=bq.all(), scalar=0.125,
                                             in1=Eq[:, fc * TT:(fc + 1) * TT], op0=ALU.mult, op1=ALU.mult)
                    bk2 = rr()
                    for kc in range(8):
                        K.T.matmul(out=bk2.all(), lhsT=wqk(kc, 256 + fc * 128, 256 + (fc + 1) * 128), rhs=hcol(kc, tt * TT, (tt + 1) * TT),
                                   start=(kc == 0), stop=(kc == 7))
                    K.V.tensor_tensor(out=kinv[:, fc * S + tt * TT:fc * S + (tt + 1) * TT], in0=bk2.all(), in1=Ek[:, fc * TT:(fc + 1) * TT], op=ALU.mult)
            for st in range(16):
                for fc in range(2):
                    K.T.transpose(out=bankT[:, fc * 128:(fc + 1) * 128], in_=kinv[:, fc * S + st * 128:fc * S + (st + 1) * 128], identity=ident_b.all())
                evac_copy(kinv_tm[:, st * 256:(st + 1) * 256], bankT[:, 0:256])
            if l == 0:
                dbg("qdec", qdec.all(), 128, 2 * S, BF16)
                dbg("kinv", kinv.all(), 128, 2 * S, BF16)
                dbg("dec", dec.all(), 128, 64)
            ckpt("gla1")
            for h in range(4):
                fc, r0 = h // 2, (h % 2) * 64
                P = slice(r0, r0 + 64)
                for tt in range(NTT):
                    bo = banks[5 + (h * NTT + tt) % 2]
                    for c8 in range(8):
                        n = tt * 8 + c8
                        st, hf = n // 2, n % 2
                        t0 = n * 64
                        H = slice(hf * 64, hf * 64 + 64)
                        qv = qdec[P, fc * S + t0:fc * S + t0 + 64]
                        ba_ = rr()
                        K.T.matmul(out=ba_[0:64, 0:64], lhsT=kinv[P, fc * S + t0:fc * S + t0 + 64], rhs=qv, start=True, stop=True)
                        am = attm[n % 2]
                        K.V.tensor_tensor(out=am[H, :], in0=ba_[0:64, 0:64], in1=cst[0:64, 256:320], op=ALU.mult)
                        oc = bo[:, c8 * 64:(c8 + 1) * 64]
                        if n > 0:
                            K.T.matmul(out=oc, lhsT=S_b[h][P, :], rhs=qv, start=True, stop=False)
                        K.T.matmul(out=oc, lhsT=v_tm[H, st * 512 + h * 128:st * 512 + (h + 1) * 128], rhs=am[H, :], start=(n == 0), stop=True)
                        if n < 31:
                            bkv = rr()
                            K.T.matmul(out=bkv[0:64, 0:128], lhsT=kinv_tm[H, st * 256 + h * 64:st * 256 + (h + 1) * 64],
                                       rhs=v_tm[H, st * 512 + h * 128:st * 512 + (h + 1) * 128], start=True, stop=True)
                            dcol = dec[P, fc * 32 + n:fc * 32 + n + 1]
                            if n == 0:
                                K.V.tensor_copy(out=S_t[h][P, :], in_=bkv[0:64, 0:128])
                            else:
                                K.V.tensor_tensor(out=S_t[h][P, :], in0=bkv[0:64, 0:128], in1=S_f[h][P, :], op=ALU.add)
                            K.A.activation(out=S_b[h][P, :], in_=S_t[h][P, :], func=AF.Copy, scale=dcol)
                            K.V.tensor_scalar(out=S_f[h][P, :], in0=S_t[h][P, :], scalar1=dcol, scalar2=None, op0=ALU.mult)
                    K.A.activation(out=osq.all(), in_=bo.all(), func=AF.Square)
                    bs = rr()
                    K.T.matmul(out=bs.all(), lhsT=ones_b.all(), rhs=osq.all(), start=True, stop=True)
                    rstd_from(bs.all(), 128)
                    K.V.tensor_tensor(out=otmp.all(), in0=bo.all(), in1=rs_sb.all(), op=ALU.mult)
                    go = gla_out[:, h * S + tt * TT:h * S + (tt + 1) * TT]
                    K.V.scalar_tensor_tensor(out=go, in0=otmp.all(), scalar=vcol(l, 36 + h), in1=go, op0=ALU.mult, op1=ALU.mult)
            if l == 0:
                dbg("gla_out", gla_out.all(), 128, 4 * S, BF16)
            ckpt("gla")

            A.off = mix0
            cqn = A.alloc(128, 3 * S, BF16, "cqn")
            ckvn = A.alloc(128, 2 * S, BF16, "ckvn")
            krope = A.alloc(128, S, BF16, "krope")
            Qh = [A.alloc(128, S, BF16, f"Qh{i}") for i in range(2)]
            Kh = [A.alloc(128, S, BF16, f"Kh{i}") for i in range(2)]
            Vh = [A.alloc(128, 16 * 128, BF16, f"Vh{i}") for i in range(2)]
            pt = [A.alloc(128, TT, BF16, f"pt{i}") for i in range(3)]
            sqh = A.alloc(128, S, BF16, "sqh")
            rt1 = A.alloc(128, TT, F32, "rt1")
            rt2 = A.alloc(128, TT, F32, "rt2")
            rden = A.alloc(128, TT, F32, "rden")
            nrow = A.alloc(1, S, F32, "nrow")
            kmx = A.alloc(1, 8, F32, "kmx")
            negm = [A.alloc(1, S, BF16, f"negm{i}") for i in range(2)]
            set_rr(range(5))
            for i in range(2):
                K.V.memset(ap=Vh[i].all().r("p (s e) -> p s e", e=128).sub(lambda ap: ap[:, :, 64:128]), constant=1.0)
            for (col0, nch, dstb, gb, nfeat) in ((CQ0, 3, cqn, 40, 384), (CKV0, 2, ckvn, 43, 256)):
                wc = wload(W1, l * D, 8, col0, nch * 128)
                for tt in range(NTT):
                    pb = [rr() for _ in range(nch)]
                    for c in range(nch):
                        for kc in range(8):
                            K.T.matmul(out=pb[c].all(), lhsT=wc(kc, c * 128, (c + 1) * 128), rhs=hcol(kc, tt * TT, (tt + 1) * TT),
                                       start=(kc == 0), stop=(kc == 7))
                    bs = rr()
                    for c in range(nch):
                        mq = pt[c]
                        K.A.activation(out=mq.all(), in_=pb[c].all(), func=AF.Square)
                        K.T.matmul(out=bs.all(), lhsT=ones_b.all(), rhs=mq.all(), start=(c == 0), stop=(c == nch - 1))
                    rstd_from(bs.all(), nfeat)
                    for c in range(nch):
                        K.V.scalar_tensor_tensor(out=dstb[:, c * S + tt * TT:c * S + (tt + 1) * TT], in0=pb[c].all(), scalar=vcol(l, gb + c),
                                                 in1=rs_sb.all(), op0=ALU.mult, op1=ALU.mult)
            wsm = wload(W1, l * D, 8, SM0, 80)
            R = slice(64, 96)
            for tt in range(NTT):
                bk = rr()
                for kc in range(8):
                    K.T.matmul(out=bk[0:64, :], lhsT=wsm(kc, 0, 64), rhs=hcol(kc, tt * TT, (tt + 1) * TT), start=(kc == 0), stop=(kc == 7))
                K.V.tensor_tensor(out=rt1[R, :], in0=bk[0:32, :], in1=ropeC[R, tt * TT:(tt + 1) * TT], op=ALU.mult)
                K.V.tensor_tensor(out=rt2[R, :], in0=bk[32:64, :], in1=ropeS[R, tt * TT:(tt + 1) * TT], op=ALU.mult)
                K.V.tensor_tensor(out=krope[R, tt * TT:(tt + 1) * TT], in0=rt1[R, :], in1=rt2[R, :], op=ALU.add)
            wuq = wload(WUQ, l * 384, 3, 0, 768)
            wuqs = wload(WUQS, l * 384, 3, 0, 768)
            wukv = wload(WUKV, l * 256, 2, 0, 1024)
            SCALE = 96.0 ** -0.5
            def mla_prep(h):
                Q, Kt, V = Qh[h % 2], Kh[h % 2], Vh[h % 2]
                for tt in range(NTT):
                    T = slice(tt * TT, (tt + 1) * TT)
                    bq, bqs, bkk = rr(), rr(), rr()
                    for c in range(3):
                        K.T.matmul(out=bq[0:96, :], lhsT=wuq(c, h * 96, (h + 1) * 96), rhs=cqn[:, c * S + tt * TT:c * S + (tt + 1) * TT],
                                   start=(c == 0), stop=(c == 2))
                    for c in range(3):
                        K.T.matmul(out=bqs[0:96, :], lhsT=wuqs(c, h * 96, (h + 1) * 96), rhs=cqn[:, c * S + tt * TT:c * S + (tt + 1) * TT],
                                   start=(c == 0), stop=(c == 2))
                    for c in range(2):
                        K.T.matmul(out=bkk[0:64, :], lhsT=wukv(c, h * 64, (h + 1) * 64), rhs=ckvn[:, c * S + tt * TT:c * S + (tt + 1) * TT],
                                   start=(c == 0), stop=(c == 1))
                    K.A.copy(out=Q[0:64, T], in_=bq[0:64, :])
                    K.V.tensor_tensor(out=rt1[R, :], in0=bq[R, :], in1=ropeC[R, T], op=ALU.mult)
                    K.V.tensor_tensor(out=rt2[R, :], in0=bqs[R, :], in1=ropeS[R, T], op=ALU.mult)
                    K.V.tensor_tensor(out=Q[R, T], in0=rt1[R, :], in1=rt2[R, :], op=ALU.add)
                    K.A.copy(out=Kt[0:64, T], in_=bkk[0:64, :])
                    K.V.tensor_copy(out=Kt[R, T], in_=krope[R, T])
                for st in range(16):
                    bv = rr()
                    for c in range(2):
                        K.T.matmul(out=bv[:, 0:64], lhsT=ckvn[:, c * S + st * 128:c * S + (st + 1) * 128], rhs=wukv(c, 512 + h * 64, 512 + (h + 1) * 64),
                                   start=(c == 0), stop=(c == 1))
                    evac_copy(V[:, st * 128:st * 128 + 64], bv[:, 0:64])
                K.A.activation(out=sqh[0:96, :], in_=Kt[0:96, :], func=AF.Square)
                for tt in range(NTT):
                    bn = rr()
                    K.T.matmul(out=bn[0:1, :], lhsT=ones_b[0:96, 0:1], rhs=sqh[0:96, tt * TT:(tt + 1) * TT], start=True, stop=True)
                    K.V.tensor_reduce(out=kmx[0:1, tt:tt + 1], in_=bn[0:1, :], axis=AX.X, op=ALU.max)
                K.V.tensor_reduce(out=kmx[0:1, 4:5], in_=kmx[0:1, 0:4], axis=AX.X, op=ALU.max)
                K.A.activation(out=sqh[0:96, :], in_=Q[0:96, :], func=AF.Square)
                for tt in range(NTT):
                    bn = rr()
                    K.T.matmul(out=bn[0:1, :], lhsT=ones_b[0:96, 0:1], rhs=sqh[0:96, tt * TT:(tt + 1) * TT], start=True, stop=True)
                    K.V.tensor_scalar(out=nrow[0:1, tt * TT:(tt + 1) * TT], in0=bn[0:1, :], scalar1=kmx[0:1, 4:5], scalar2=None, op0=ALU.mult)
                K.A.activation(out=nrow.all(), in_=nrow.all(), func=AF.Sqrt)
                K.V.tensor_scalar(out=negm[h % 2].all(), in0=nrow.all(), scalar1=-1.0, scalar2=None, op0=ALU.mult)

            def mla_attend(h):
                Q, Kt, V = Qh[h % 2], Kh[h % 2], Vh[h % 2]
                for qb in range(4):
                    bo = banks[5 + (h * 4 + qb) % 2]
                    nk = 4 * qb + 4
                    for kt in range(nk):
                        r = kt - 4 * qb
                        c0 = max(r, 0) * 128
                        q0 = qb * TT + c0
                        bs_ = rr()
                        K.T.matmul(out=bs_[:, c0:TT], lhsT=Kt[0:96, kt * 128:(kt + 1) * 128], rhs=Q[0:96, q0:(qb + 1) * TT], start=True, stop=False)
                        K.T.matmul(out=bs_[:, c0:TT], lhsT=ones_b[0:1, 0:128], rhs=negm[h % 2][0:1, q0:(qb + 1) * TT], start=False, stop=(r < 0))
                        if r >= 0:
                            K.T.matmul(out=bs_[:, c0:TT], lhsT=ident_b.all(), rhs=maskneg_b[:, 0:TT - c0], start=False, stop=True)
                        p = pt[(h * 64 + qb * 16 + kt) % 3]
                        K.A.activation(out=p[:, c0:TT], in_=bs_[:, c0:TT], func=AF.Exp, scale=SCALE)
                        K.T.matmul(out=bo[:, c0:TT], lhsT=V[:, kt * 128:(kt + 1) * 128], rhs=p[:, c0:TT], start=(kt == 0), stop=(kt == nk - 1))
                    K.V.reciprocal(out=rden[64:128, :], in_=bo[64:128, :])
                    rr0 = (h % 2) * 64
                    K.V.tensor_tensor(out=attn_out[rr0:rr0 + 64, (h // 2) * S + qb * TT:(h // 2) * S + (qb + 1) * TT], in0=bo[0:64, :],
                                      in1=rden[64:128, :], op=ALU.mult)
            mla_prep(0)
            for h in range(8):
                if h + 1 < 8:
                    mla_prep(h + 1)
                mla_attend(h)
            if l == 0:
                dbg("attn_out", attn_out.all(), 128, 4 * S, BF16)
            ckpt("mla")

            A.off = mix0
            merged = A.alloc(128, 8 * S, BF16, "merged")
            sig = [A.alloc(128, TT, F32, f"sig{i}") for i in range(3)]
            acc = [A.alloc(128, TT, F32, f"acc{i}") for i in range(2)]
            yins = (ya_in, gla_out, attn_out)
            set_rr(range(7))
            for m in range(8):
                wg = wload(W1, l * D, 8, G0 + m * 384, 384)
                wy = wload(WABC, l * 1536, 12, m * 128, 128)
                for tt in range(NTT):
                    ac = acc[(m * NTT + tt) % 2]
                    for b in range(3):
                        bg, by = rr(), rr()
                        for kc in range(8):
                            K.T.matmul(out=bg.all(), lhsT=wg(kc, b * 128, (b + 1) * 128), rhs=hcol(kc, tt * TT, (tt + 1) * TT),
                                       start=(kc == 0), stop=(kc == 7))
                        for c in range(4):
                            K.T.matmul(out=by.all(), lhsT=wy(b * 4 + c, 0, 128), rhs=yins[b][:, c * S + tt * TT:c * S + (tt + 1) * TT],
                                       start=(c == 0), stop=(c == 3))
                        K.A.activation(out=sig[b].all(), in_=bg.all(), func=AF.Sigmoid)
                        if b == 0:
                            K.V.tensor_tensor(out=ac.all(), in0=by.all(), in1=sig[b].all(), op=ALU.mult)
                        else:
                            K.V.tensor_tensor(out=sig[b].all(), in0=by.all(), in1=sig[b].all(), op=ALU.mult)
                            if b == 1:
                                K.V.tensor_tensor(out=ac.all(), in0=ac.all(), in1=sig[b].all(), op=ALU.add)
                            else:
                                K.V.tensor_tensor(out=merged[:, m * S + tt * TT:m * S + (tt + 1) * TT], in0=ac.all(), in1=sig[b].all(), op=ALU.add)
            if l == 0:
                dbg("merged", merged.all(), 128, 8 * S, BF16)
            ckpt("merge")

            xt = A.alloc(128, 8 * TT, F32, "xt")
            tt_ = A.alloc(128, 8 * TT, F32, "ttile")
            sq = [A.alloc(128, TT, BF16, f"sq{i}") for i in range(2)]
            wo0 = wload(WO, l * D, 8, 0, 512)
            wo1 = wload(WO, l * D, 8, 512, 512)
            for tt in range(NTT):
                K.S.dma_start(out=xt.all().r("p (c t) -> p c t", c=8), in_=xs_view(tt))
                bsum = banks[6]
                set_rr(range(6))
                for m in range(8):
                    wo_ = wo0 if m < 4 else wo1
                    bk = rr()
                    for kc in range(8):
                        K.T.matmul(out=bk.all(), lhsT=wo_(kc, (m % 4) * 128, (m % 4 + 1) * 128), rhs=merged[:, kc * S + tt * TT:kc * S + (tt + 1) * TT],
                                   start=(kc == 0), stop=(kc == 7))
                    K.V.tensor_copy(out=tt_[:, m * TT:(m + 1) * TT], in_=bk.all())
                    K.A.activation(out=sq[m % 2].all(), in_=bk.all(), func=AF.Square)
                    K.T.matmul(out=bsum.all(), lhsT=ones_b.all(), rhs=sq[m % 2].all(), start=(m == 0), stop=(m == 7))
                rstd_from(bsum.all(), D)
                for m in range(8):
                    K.V.scalar_tensor_tensor(out=tt_[:, m * TT:(m + 1) * TT], in0=tt_[:, m * TT:(m + 1) * TT], scalar=vcol(l, 8 + m),
                                             in1=rs_sb.all(), op0=ALU.mult, op1=ALU.mult)
                    K.V.tensor_tensor(out=xt[:, m * TT:(m + 1) * TT], in0=xt[:, m * TT:(m + 1) * TT], in1=tt_[:, m * TT:(m + 1) * TT], op=ALU.add)
                K.S.dma_start(out=xs_view(tt), in_=xt.all().r("p (c t) -> p c t", c=8))
                norm_to_h(xt, sq, l, 16, tt)
            if l == 0:
                dbg("h2", hT.all(), 128, 8 * S, BF16)
            ckpt("wo")

            A = Arena()
            act = A.alloc(128, 22 * S, BF16, "act")
            xt = A.alloc(128, 8 * TT, F32, "xt")
            ft = A.alloc(128, 8 * TT, F32, "ftile")
            sq = [A.alloc(128, TT, BF16, f"sq{i}") for i in range(2)]
            sg = [A.alloc(128, TT, F32, f"sg{i}") for i in range(2)]
            otile = [K.sbuf(128, D, F32, f"otile{i}", off=ft.off + i * D * 4) for i in range(2)]
            set_rr(range(7))
            for j in range(11):
                wgu = wload(WGU, l * D, 8, j * 512, 512)
                for pp in range(2):
                    jj = 2 * j + pp
                    for tt in range(NTT):
                        bg, bu = rr(), rr()
                        for kc in range(8):
                            K.T.matmul(out=bg.all(), lhsT=wgu(kc, pp * 256, pp * 256 + 128), rhs=hcol(kc, tt * TT, (tt + 1) * TT),
                                       start=(kc == 0), stop=(kc == 7))
                        for kc in range(8):
                            K.T.matmul(out=bu.all(), lhsT=wgu(kc, pp * 256 + 128, pp * 256 + 256), rhs=hcol(kc, tt * TT, (tt + 1) * TT),
                                       start=(kc == 0), stop=(kc == 7))
                        s_ = sg[(jj * NTT + tt) % 2]
                        K.A.activation(out=s_.all(), in_=bg.all(), func=AF.Silu)
                        K.V.tensor_tensor(out=act[:, jj * S + tt * TT:jj * S + (tt + 1) * TT], in0=bu.all(), in1=s_.all(), op=ALU.mult)
            for tt in range(NTT):
                K.S.dma_start(out=xt.all().r("p (c t) -> p c t", c=8), in_=xs_view(tt))
                bsum = banks[6]
                set_rr(range(6))
                for m in range(8):
                    wd = wload(WD, l * DFF, 22, m * 128, 128)
                    bk = rr()
                    for kc in range(22):
                        K.T.matmul(out=bk.all(), lhsT=wd(kc, 0, 128), rhs=act[:, kc * S + tt * TT:kc * S + (tt + 1) * TT], start=(kc == 0), stop=(kc == 21))
                    K.V.tensor_copy(out=ft[:, m * TT:(m + 1) * TT], in_=bk.all())
                    K.A.activation(out=sq[m % 2].all(), in_=bk.all(), func=AF.Square)
                    K.T.matmul(out=bsum.all(), lhsT=ones_b.all(), rhs=sq[m % 2].all(), start=(m == 0), stop=(m == 7))
                rstd_from(bsum.all(), D)
                for m in range(8):
                    K.V.scalar_tensor_tensor(out=ft[:, m * TT:(m + 1) * TT], in0=ft[:, m * TT:(m + 1) * TT], scalar=vcol(l, 24 + m),
                                             in1=rs_sb.all(), op0=ALU.mult, op1=ALU.mult)
                    K.V.tensor_tensor(out=xt[:, m * TT:(m + 1) * TT], in0=xt[:, m * TT:(m + 1) * TT], in1=ft[:, m * TT:(m + 1) * TT], op=ALU.add)
                if l < depth - 1:
                    K.S.dma_start(out=xs_view(tt), in_=xt.all().r("p (c t) -> p c t", c=8))
                    norm_to_h(xt, sq, l + 1, 0, tt)
                else:
                    for s4 in range(4):
                        ot = otile[s4 % 2]
                        for half in range(2):
                            bk = rr()
                            for j in range(4):
                                c = half * 4 + j
                                K.T.transpose(out=bk[:, j * 128:(j + 1) * 128], in_=xt[:, c * TT + s4 * 128:c * TT + (s4 + 1) * 128], identity=ident_f)
                            evac_copy(ot[:, half * 512:(half + 1) * 512], bk.all())
                        r0 = (tt * 4 + s4) * 128
                        i = K.S.dma_start(out=dram_view(out_d[r0:r0 + 128, :], "out", r0, r0 + 128), in_=ot.all())
                        out_toks.append(i.tok)

    except _Stop:
        pass
    K.wait_all("sync", out_toks + list(dbg_out.values()))
    K.emit()
    K.close()
    return nc


def host_prep(inputs):
    f = np.float32
    w_in = np.asarray(inputs["w_in"], f)
    Ld = w_in.shape[0]
    O_POOL, O_Q, O_K, O_V, O_R, O_A1, O_CQ, O_CKV, O_KR, O_G = 0, 512, 768, 1024, 1536, 2048, 2064, 2448, 2704, 2736
    idx = []
    idx += list(range(O_POOL, O_POOL + 512))
    idx += list(range(O_Q, O_Q + 256)) + list(range(O_K, O_K + 256))
    idx += list(range(O_R, O_R + 512))
    idx += list(range(O_V, O_V + 512))
    idx += list(range(O_CQ, O_CQ + 384))
    idx += list(range(O_CKV, O_CKV + 256))
    idx += list(range(O_KR, O_KR + 32))
    idx += list(range(O_KR + 16, O_KR + 32)) + list(range(O_KR, O_KR + 16))
    idx += list(range(O_A1, O_A1 + 16))
    for m in range(8):
        for b in range(3):
            idx += list(range(O_G + b * 1024 + m * 128, O_G + b * 1024 + (m + 1) * 128))
    idx = np.asarray(idx)
    assert idx.shape[0] == W1C
    w1 = np.ascontiguousarray(w_in[:, :, idx]).reshape(Ld * D, W1C)
    wp = np.ascontiguousarray(np.asarray(inputs["w_pool"], f).transpose(0, 2, 1, 3)).reshape(Ld * 128, 512)
    wabc = np.concatenate([np.asarray(inputs["w_a"], f), np.asarray(inputs["w_b"], f), np.asarray(inputs["w_c"], f)], axis=1).reshape(Ld * 1536, D)
    wa2 = np.ascontiguousarray(np.asarray(inputs["w_gla_a2"], f)).reshape(Ld * 16, 256)
    ba = np.ascontiguousarray(np.asarray(inputs["b_gla_a"], f)).reshape(Ld, 256)
    w_uq = np.asarray(inputs["w_mla_uq"], f)
    sw = []
    for h in range(8):
        sw += list(range(h * 96, h * 96 + 64)) + list(range(h * 96 + 80, h * 96 + 96)) + list(range(h * 96 + 64, h * 96 + 80))
    wuq = np.ascontiguousarray(w_uq).reshape(Ld * 384, 768)
    wuqs = np.ascontiguousarray(w_uq[:, :, np.asarray(sw)]).reshape(Ld * 384, 768)
    pk = []
    for h in range(8):
        pk += list(range(h * 128, h * 128 + 64))
    for h in range(8):
        pk += list(range(h * 128 + 64, h * 128 + 128))
    wukv = np.ascontiguousarray(np.asarray(inputs["w_mla_ukv"], f)[:, :, np.asarray(pk)]).reshape(Ld * 256, 1024)
    wo = np.ascontiguousarray(np.asarray(inputs["w_o"], f)).reshape(Ld * D, D)
    pg = []
    for j in range(22):
        pg += list(range(j * 128, (j + 1) * 128)) + list(range(DFF + j * 128, DFF + (j + 1) * 128))
    wgu = np.ascontiguousarray(np.asarray(inputs["w_ffn_gu"], f)[:, :, np.asarray(pg)]).reshape(Ld * D, 2 * DFF)
    wd = np.ascontiguousarray(np.asarray(inputs["w_ffn_down"], f)).reshape(Ld * DFF, D)
    vecs = np.zeros((128, Ld * NV), f)
    for l in range(Ld):
        cols = []
        for name, n in (("norm_pre_mix", 8), ("norm_post_mix", 8), ("norm_pre_ffn", 8), ("norm_post_ffn", 8), ("pool_scale", 4),
                        ("gla_norm", 4), ("mla_q_norm", 3), ("mla_kv_norm", 2)):
            v = np.asarray(inputs[name], f)[l]
            cols.append(v.reshape(n, 128).T)
        vecs[:, l * NV:(l + 1) * NV] = np.concatenate(cols, axis=1)
    cst = np.zeros((128, NCST), f)
    cst[:, 0:128] = np.eye(128, dtype=f)
    s = np.arange(128)
    cst[:, 128:256] = ((s[:, None] // 64 == s[None, :] // 64) & (s[:, None] <= s[None, :])).astype(f)
    cst[:, 256:320] = ((s[:, None] % 64) <= np.arange(64)[None, :]).astype(f)
    cst[:, 320:336] = (1.0 / (np.arange(16) + 1.0)).astype(f)[None, :]
    invf = (10000.0 ** (-np.arange(0, 32, 2, dtype=np.float32) / 32.0)).astype(f)
    cst[64:80, 336] = invf
    cst[80:96, 336] = invf
    cst[64:80, 337] = -1.0
    cst[80:96, 337] = 1.0
    q = np.arange(128)
    cst[:, 338:338 + 128] = np.where(q[None, :] < s[:, None], -30000.0, 0.0).astype(f)
    shared = dict(cst=cst, vecs=vecs, w1=w1, wp=wp, wabc=wabc, wa2=wa2, ba=ba, wuq=wuq, wuqs=wuqs, wukv=wukv, wo=wo, wgu=wgu, wd=wd)
    return shared


def kernel(**inputs):
    x = np.asarray(inputs["x"], np.float32)
    pos = np.asarray(inputs["positions"], np.int32)
    B = x.shape[0]
    shared = host_prep(inputs)
    nc = build_program(L_FULL)
    in_maps = []
    for b in range(B):
        m = dict(shared)
        m["x"] = np.ascontiguousarray(x[b])
        m["pos"] = np.ascontiguousarray(np.broadcast_to(pos[b][None, :], (128, S)))
        in_maps.append(m)
    res = run_bass_kernel_spmd(nc, in_maps, core_ids=list(range(B)))
    return np.stack([np.asarray(r["out"], np.float32) for r in res.results], axis=0)
```

```python
import numpy as np
from contextlib import ExitStack
import concourse.bass as bass
import concourse.mybir as mybir

F32 = mybir.dt.float32
BF16 = mybir.dt.bfloat16
I32 = mybir.dt.int32
AF = mybir.ActivationFunctionType
ALU = mybir.AluOpType
AX = mybir.AxisListType
ESZ = {F32: 4, BF16: 2, I32: 4}

SAME_ENGINE_SYNC = True
SEM_EPOCH = 30000
N_DMA_SEMS = 8


class View:
    def __init__(self, ap, space, p0, p1, b0, b1):
        self.ap, self.space, self.p0, self.p1, self.b0, self.b1 = ap, space, p0, p1, b0, b1

    def r(self, pattern, **kw):
        return View(self.ap.rearrange(pattern, **kw), self.space, self.p0, self.p1, self.b0, self.b1)

    def bc(self, shape):
        return View(self.ap.to_broadcast(shape), self.space, self.p0, self.p1, self.b0, self.b1)

    def bitcast(self, dt):
        return View(self.ap.bitcast(dt), self.space, self.p0, self.p1, self.b0, self.b1)

    def sub(self, fn):
        return View(fn(self.ap), self.space, self.p0, self.p1, self.b0, self.b1)


class Buf:
    def __init__(self, handle, space, P, F, dtype, off):
        self.t, self.space, self.P, self.F, self.dtype, self.off = handle, space, P, F, dtype, off
        self.esz = ESZ[dtype]

    def __getitem__(self, key):
        ps, cs = key
        p0, p1, _ = ps.indices(self.P)
        c0, c1, _ = cs.indices(self.F)
        return View(self.t[p0:p1, c0:c1], self.space, p0, p1,
                    self.off + c0 * self.esz, self.off + c1 * self.esz)

    def all(self):
        return self[:, :]


class DView(View):
    pass


def dram_view(ap, name, lo, hi):
    return View(ap, "d:" + name, 0, 1, lo, hi)


class Ins:
    __slots__ = ("eng", "fn", "kw", "waits", "tok", "inc", "is_dma")


class EngProxy:
    def __init__(self, K, eng):
        self.K, self.eng = K, eng

    def __getattr__(self, fn):
        def call(**kw):
            return self.K._record(self.eng, fn, kw)
        return call


OUT_KEYS = ("out", "accum_out", "ap")


class Kern:
    def __init__(self, nc):
        self.nc = nc
        self.es = ExitStack()
        self.sb_off = 229376 - nc.sbuf_bytes_remaining
        self.sb_end = 229376
        self.streams = {e: [] for e in ("tensor", "vector", "scalar", "gpsimd", "sync")}
        self.sems = {}
        self.cnt = {e: 0 for e in self.streams}
        self.dma_sems = {}
        self.dma_rr = {e: 0 for e in self.streams}
        self.seen = {e: {} for e in self.streams}
        self.recs = {}
        self.T = EngProxy(self, "tensor")
        self.V = EngProxy(self, "vector")
        self.A = EngProxy(self, "scalar")
        self.G = EngProxy(self, "gpsimd")
        self.S = EngProxy(self, "sync")
        self.nsem = 0
        self.nbuf = 0

    def sem(self, name):
        self.nsem += 1
        return self.es.enter_context(self.nc.semaphore(name))

    def sbuf(self, P, F, dtype, name=None, off=None):
        esz = ESZ[dtype]
        nbytes = F * esz
        if off is None:
            off = (self.sb_off + 63) // 64 * 64
            self.sb_off = off + nbytes
            assert self.sb_off <= self.sb_end, (name, self.sb_off)
        self.nbuf += 1
        name = f"{name or 'sb'}_{self.nbuf}"
        h = self.nc.alloc_sbuf_tensor_at(name, [P, F], dtype, offset=off)
        return Buf(h, "sb", P, F, dtype, off)

    def psum_banks(self, n=8):
        banks = []
        for i in range(n):
            h = self.es.enter_context(self.nc.psum_tensor(f"psb{i}", [128, 512], F32))
            banks.append(Buf(h, "ps", 128, 512, F32, i * 2048))
        return banks

    def _tok_compute(self, eng):
        self.cnt[eng] += 1
        c = self.cnt[eng]
        ep = (c - 1) // SEM_EPOCH
        lst = self.sems.setdefault(eng, [])
        while len(lst) <= ep:
            lst.append(self.sem(f"s_{eng}_{len(lst)}"))
        return (lst[ep], c - ep * SEM_EPOCH)

    def _record(self, eng, fn, kw):
        ins = Ins()
        ins.eng, ins.fn, ins.kw, ins.is_dma = eng, fn, kw, fn.startswith("dma_start")
        reads, writes = [], []
        for k, v in list(kw.items()):
            if isinstance(v, View):
                if v.space == "ps":
                    bb = (v.b0 // 2048) * 2048
                    v = View(v.ap, "ps", (v.p0 // 32) * 32, ((v.p1 + 31) // 32) * 32, bb, bb + 2048)
                    kw[k] = v
                (writes if k in OUT_KEYS else reads).append(v)
        deps = {}

        def add_dep(tok):
            s, val = tok
            if deps.get(id(s), (None, 0))[1] < val:
                deps[id(s)] = (s, val)

        for v, is_w in [(x, False) for x in reads] + [(x, True) for x in writes]:
            lst = self.recs.setdefault(v.space, [])
            for r in lst:
                if r[0] < v.p1 and v.p0 < r[1] and r[2] < v.b1 and v.b0 < r[3] and (is_w or r[4] or (v.space == "ps" and r[5] != eng)):
                    if r[5] == eng and not r[6] and not ins.is_dma:
                        if eng == "tensor" or not SAME_ENGINE_SYNC:
                            continue
                    add_dep(r[7])
        if ins.is_dma:
            pool = self.dma_sems.setdefault(eng, [])
            if len(pool) < N_DMA_SEMS:
                pool.append([self.sem(f"d_{eng}_{len(pool)}"), 0])
            i = self.dma_rr[eng] % len(pool) if len(pool) == N_DMA_SEMS else len(pool) - 1
            self.dma_rr[eng] += 1
            ent = pool[i]
            if ent[1] > 0:
                add_dep((ent[0], ent[1]))
            ent[1] += 16
            ins.tok, ins.inc = (ent[0], ent[1]), 16
        else:
            ins.tok, ins.inc = self._tok_compute(eng), 1
        seen = self.seen[eng]
        ins.waits = []
        for s, val in deps.values():
            if seen.get(id(s), 0) < val:
                seen[id(s)] = val
                ins.waits.append((s, val))
        for v, is_w in [(x, False) for x in reads] + [(x, True) for x in writes]:
            lst = self.recs[v.space]
            new = []
            for r in lst:
                covered = v.p0 <= r[0] and r[1] <= v.p1 and v.b0 <= r[2] and r[3] <= v.b1
                if covered and (is_w or (not r[4] and ((r[5] == eng and not r[6] and not ins.is_dma) or v.space == "ps"))):
                    continue
                new.append(r)
            new.append([v.p0, v.p1, v.b0, v.b1, is_w, eng, ins.is_dma, ins.tok])
            self.recs[v.space] = new
        self.streams[eng].append(ins)
        return ins

    def wait_all(self, eng, toks):
        ins = Ins()
        ins.eng, ins.fn, ins.kw, ins.is_dma = eng, None, {}, False
        ins.waits = list(toks)
        ins.tok, ins.inc = None, 0
        self.streams[eng].append(ins)

    def emit(self):
        nc = self.nc
        block = self.es.enter_context(nc.Block())

        def run(engname):
            def body(e):
                for ins in self.streams[engname]:
                    for s, val in ins.waits:
                        e.wait_ge(s, val)
                    if ins.fn is None:
                        continue
                    kw = {k: (v.ap if isinstance(v, View) else v) for k, v in ins.kw.items()}
                    r = getattr(e, ins.fn)(**kw)
                    r.then_inc(ins.tok[0], ins.inc)
            return body

        block.tensor(run("tensor"))
        block.vector(run("vector"))
        block.scalar(run("scalar"))
        block.gpsimd(run("gpsimd"))
        block.sync(run("sync"))

    def close(self):
        self.es.close()


import math
from concourse.bass_utils import run_bass_kernel_spmd

D = 1024
S = 2048
L_FULL = 4
DFF = 2816
NV = 45
NCST = 850
EPS = 1e-6
TT = 512
NTT = S // TT
W1C = 5840
POOL0, QK0, R0, V0, CQ0, CKV0, SM0, G0 = 0, 512, 1024, 1536, 2048, 2432, 2688, 2768
WSLOT = 4096
NSLOT = 3
C1_2PI = 6.28125
C2_2PI = 2.0 * math.pi - 6.28125
import os
NOALIAS0 = bool(int(os.environ.get('NOALIAS0', '0')))


class _Stop(Exception):
    pass


def build_program(depth=L_FULL, debug=(), stop=None):
    nc = bass.Bass("TRN2", target_bir_lowering=False)
    K = Kern(nc)

    def din(name, shape, dt=F32):
        return nc.dram_tensor(name, shape, dt, kind="ExternalInput").ap()

    x_in = din("x", [S, D])
    pos_in = din("pos", [128, S], I32)
    cst_in = din("cst", [128, NCST])
    vecs_in = din("vecs", [128, L_FULL * NV])
    W1 = din("w1", [L_FULL * D, W1C])
    WP = din("wp", [L_FULL * 128, 512])
    WABC = din("wabc", [L_FULL * 1536, D])
    WA2 = din("wa2", [L_FULL * 16, 256])
    BA = din("ba", [L_FULL, 256])
    WUQ = din("wuq", [L_FULL * 384, 768])
    WUQS = din("wuqs", [L_FULL * 384, 768])
    WUKV = din("wukv", [L_FULL * 256, 1024])
    WO = din("wo", [L_FULL * D, D])
    WGU = din("wgu", [L_FULL * D, 2 * DFF])
    WD = din("wd", [L_FULL * DFF, D])
    out_d = nc.dram_tensor("out", [S, D], F32, kind="ExternalOutput").ap()
    xs_d = nc.dram_tensor("xs", [128, 8 * S], F32, kind="Internal").ap()
    dbg_out = {}

    banks = []
    for i in range(7):
        h = K.es.enter_context(nc.psum_tensor(f"psb{i}", [128, 512], F32))
        banks.append(Buf(h, "ps", 128, 512, F32, i * 2048))
    hb = K.es.enter_context(nc.psum_tensor("psb7", [128, 1024], BF16))
    bankT = Buf(hb, "ps", 128, 1024, BF16, 7 * 2048)
    rr_state = {"list": list(range(7)), "i": 0}

    def set_rr(lst):
        rr_state["list"], rr_state["i"] = list(lst), 0

    def rr():
        b = banks[rr_state["list"][rr_state["i"] % len(rr_state["list"])]]
        rr_state["i"] += 1
        return b

    cst = K.sbuf(128, NCST, F32, "cst")
    vecs = K.sbuf(128, L_FULL * NV, F32, "vecs")
    ident_b = K.sbuf(128, 128, BF16, "identb")
    ones_b = K.sbuf(128, 128, BF16, "onesb")
    ones_f = K.sbuf(128, 128, F32, "onesf")
    maskneg_b = K.sbuf(128, 512, BF16, "maskneg")
    epsc = K.sbuf(128, 1, F32, "eps")
    ropeC = K.sbuf(128, S, F32, "ropeC")
    ropeS = K.sbuf(128, S, F32, "ropeS")
    hT = K.sbuf(128, 8 * S, BF16, "hT")
    wslots = [K.sbuf(128, WSLOT, BF16, f"wslot{i}") for i in range(NSLOT)]
    rs_sb = K.sbuf(128, 512, F32, "rs")
    ident_fb = K.sbuf(128, 128, F32, "identf")
    U_fb = K.sbuf(128, 128, F32, "Uf")
    ident_f = ident_fb.all()
    U_f = U_fb.all()
    arena0 = K.sb_off
    ARENA_END = K.sb_end
    gmask = cst[:, 256:320]
    invcnt = cst[:, 320:336]
    invf = cst[:, 336:337]
    sgn = cst[:, 337:338]

    class Arena:
        def __init__(self):
            self.off = arena0

        def alloc(self, P, F, dt, name):
            b = K.sbuf(P, F, dt, name, off=(self.off + 63) // 64 * 64)
            self.off = b.off + F * ESZ[dt]
            assert self.off <= ARENA_END, (name, self.off, ARENA_END)
            return b

    ws_i = [0]

    def wload(W2d, row0, nk, c0, ncols):
        assert nk * ncols <= WSLOT
        slot = wslots[ws_i[0] % NSLOT]
        ws_i[0] += 1
        dst = slot[:, 0:nk * ncols]
        src = W2d[row0:row0 + nk * 128, c0:c0 + ncols].rearrange("(k p) n -> p k n", p=128)
        K.G.dma_start(out=dst.r("p (k n) -> p k n", k=nk), in_=src)

        def w(k, a, b):
            return slot[:, k * ncols + a:k * ncols + b]
        return w

    evac_i = [0]

    def evac_copy(out, in_):
        evac_i[0] += 1
        if evac_i[0] % 2:
            K.A.copy(out=out, in_=in_)
        else:
            K.V.tensor_copy(out=out, in_=in_)

    def hcol(c, t0, t1):
        return hT[:, c * S + t0:c * S + t1]

    def vcol(l, j):
        return vecs[:, l * NV + j:l * NV + j + 1]

    def dbg(name, view, P, F, dt=F32):
        if name not in debug:
            return
        d = nc.dram_tensor("dbg_" + name, [P, F], dt, kind="ExternalOutput").ap()
        i = K.S.dma_start(out=dram_view(d, "dbg_" + name, 0, 1), in_=view)
        dbg_out[name] = i.tok

    def rstd_from(bank_view, n, P=128):
        K.A.activation(out=rs_sb[0:P, :], in_=bank_view, func=AF.Sqrt, bias=epsc[0:P, :], scale=1.0 / n)
        K.V.reciprocal(out=rs_sb[0:P, :], in_=rs_sb[0:P, :])

    def norm_to_h(xt, sq, l_next, gbase, tt):
        bk = rr()
        for c in range(8):
            K.A.activation(out=sq[c % 2].all(), in_=xt[:, c * TT:(c + 1) * TT], func=AF.Square)
            K.T.matmul(out=bk.all(), lhsT=ones_b.all(), rhs=sq[c % 2].all(), start=(c == 0), stop=(c == 7))
        rstd_from(bk.all(), D)
        for c in range(8):
            K.V.scalar_tensor_tensor(out=hcol(c, tt * TT, (tt + 1) * TT), in0=xt[:, c * TT:(c + 1) * TT],
                                     scalar=vcol(l_next, gbase + c), in1=rs_sb.all(), op0=ALU.mult, op1=ALU.mult)

    def xs_view(tt):
        ap = xs_d.rearrange("p (c t) -> p c t", c=8)[:, :, tt * TT:(tt + 1) * TT]
        return dram_view(ap, "xs", tt, tt + 1)

    out_toks = []

    def ckpt(name):
        if stop == name:
            raise _Stop()

    try:
        K.S.dma_start(out=cst.all(), in_=cst_in)
        K.S.dma_start(out=vecs.all(), in_=vecs_in)
        K.V.tensor_copy(out=ident_f, in_=cst[:, 0:128])
        K.V.tensor_copy(out=U_f, in_=cst[:, 128:256])
        K.V.tensor_copy(out=ident_b.all(), in_=cst[:, 0:128])
        K.V.memset(ap=ones_b.all(), constant=1.0)
        K.V.memset(ap=ones_f.all(), constant=1.0)
        K.V.memset(ap=epsc.all(), constant=EPS)
        K.V.tensor_copy(out=maskneg_b.all(), in_=cst[:, 338:850])

        A = Arena()
        pos_i = A.alloc(128, S, I32, "posi")
        ang = A.alloc(128, S, F32, "ang")
        nfl = A.alloc(128, S, F32, "nfl")
        n_i = A.alloc(128, S, I32, "ni")
        K.S.dma_start(out=pos_i.all(), in_=pos_in)
        R = slice(64, 96)
        K.V.tensor_copy(out=ang[R, :], in_=pos_i[R, :])
        K.V.tensor_scalar(out=ang[R, :], in0=ang[R, :], scalar1=cst[R, 336:337], scalar2=None, op0=ALU.mult)
        for which, table in ((0, ropeS), (1, ropeC)):
            if which == 1:
                K.V.tensor_scalar(out=ang[R, :], in0=ang[R, :], scalar1=math.pi / 2, scalar2=None, op0=ALU.add)
            K.V.tensor_scalar(out=nfl[R, :], in0=ang[R, :], scalar1=1.0 / (2 * math.pi), scalar2=None, op0=ALU.mult)
            K.V.tensor_copy(out=n_i[R, :], in_=nfl[R, :])
            K.V.tensor_copy(out=nfl[R, :], in_=n_i[R, :])
            K.V.scalar_tensor_tensor(out=table[R, :], in0=nfl[R, :], scalar=-C1_2PI, in1=ang[R, :], op0=ALU.mult, op1=ALU.add)
            K.V.scalar_tensor_tensor(out=table[R, :], in0=nfl[R, :], scalar=-C2_2PI, in1=table[R, :], op0=ALU.mult, op1=ALU.add)
            K.V.tensor_scalar(out=table[R, :], in0=table[R, :], scalar1=-3.14159, scalar2=3.14159, op0=ALU.max, op1=ALU.min)
            K.A.activation(out=table[R, :], in_=table[R, :], func=AF.Sin)
        K.V.tensor_scalar(out=ropeS[R, :], in0=ropeS[R, :], scalar1=cst[R, 337:338], scalar2=None, op0=ALU.mult)
        dbg("ropeC", ropeC[R, :], 32, S)
        dbg("ropeS", ropeS[R, :], 32, S)
        ckpt("setup")

        if not NOALIAS0:
            A = Arena()
        xt = A.alloc(128, 8 * TT, F32, "xt")
        sq = [A.alloc(128, TT, BF16, f"sq{i}") for i in range(2)]
        xin = [A.alloc(128, D, F32, f"xin{i}") for i in range(2)]
        set_rr(range(7))
        for tt in range(NTT):
            for st in range(4):
                xi = xin[(tt * 4 + st) % 2]
                r0 = (tt * 4 + st) * 128
                K.S.dma_start(out=xi.all(), in_=x_in[r0:r0 + 128, :])
                for half in range(2):
                    bk = rr()
                    for j in range(4):
                        c = half * 4 + j
                        K.T.transpose(out=bk[:, j * 128:(j + 1) * 128], in_=xi[:, c * 128:(c + 1) * 128], identity=ident_f)
                    for j in range(4):
                        c = half * 4 + j
                        evac_copy(xt[:, c * TT + st * 128:c * TT + (st + 1) * 128], bk[:, j * 128:(j + 1) * 128])
            dbg("xt0", xt.all(), 128, 8 * TT)
            ckpt("p0a")
            K.S.dma_start(out=xs_view(tt), in_=xt.all().r("p (c t) -> p c t", c=8))
            ckpt("p0b")
            norm_to_h(xt, sq, 0, 0, tt)
            ckpt("p0c")
        dbg("h0", hT.all(), 128, 8 * S, BF16)
        ckpt("phase0")

        for l in range(depth):
            A = Arena()
            ya_in = A.alloc(128, 4 * S, BF16, "ya_in")
            gla_out = A.alloc(128, 4 * S, BF16, "gla_out")
            attn_out = A.alloc(128, 4 * S, BF16, "attn_out")
            mix0 = A.off
            PAD = 16
            ubuf = [A.alloc(128, PAD + S, F32, f"ubuf{i}") for i in range(3)]
            diff = A.alloc(128, S, BF16, "diff")
            ptmp = A.alloc(128, 16, F32, "ptmp")
            set_rr(range(7))
            for i in range(3):
                K.V.memset(ap=ubuf[i][:, 0:PAD], constant=0.0)
            wpool_in = wload(W1, l * D, 8, POOL0, 512)
            wpp = wload(WP, l * 128, 1, 0, 512)
            for g in range(4):
                w = 2 ** (g + 1)
                for tt in range(NTT):
                    bk = rr()
                    for kc in range(8):
                        K.T.matmul(out=bk.all(), lhsT=wpool_in(kc, g * 128, (g + 1) * 128), rhs=hcol(kc, tt * TT, (tt + 1) * TT),
                                   start=(kc == 0), stop=(kc == 7))
                    evac_copy(ubuf[0][:, PAD + tt * TT:PAD + (tt + 1) * TT], bk.all())
                src = 0
                for j in range(g + 1):
                    d = 2 ** j
                    dst = 1 if src != 1 else 2
                    K.V.tensor_tensor(out=ubuf[dst][:, PAD:PAD + S], in0=ubuf[src][:, PAD:PAD + S],
                                      in1=ubuf[src][:, PAD - d:PAD - d + S], op=ALU.add)
                    src = dst
                sfin = ubuf[src]
                K.V.scalar_tensor_tensor(out=diff.all(), in0=sfin[:, PAD:PAD + S], scalar=1.0 / w, in1=ubuf[0][:, PAD:PAD + S],
                                         op0=ALU.mult, op1=ALU.subtract)
                K.V.tensor_tensor(out=ptmp[:, 0:w - 1], in0=sfin[:, PAD:PAD + w - 1], in1=cst[:, 320:320 + w - 1], op=ALU.mult)
                K.V.tensor_tensor(out=diff[:, 0:w - 1], in0=ptmp[:, 0:w - 1], in1=ubuf[0][:, PAD:PAD + w - 1], op=ALU.subtract)
                for tt in range(NTT):
                    bk = rr()
                    K.T.matmul(out=bk.all(), lhsT=wpp(0, g * 128, (g + 1) * 128), rhs=diff[:, tt * TT:(tt + 1) * TT], start=True, stop=True)
                    K.V.tensor_scalar(out=ya_in[:, g * S + tt * TT:g * S + (tt + 1) * TT], in0=bk.all(), scalar1=vcol(l, 32 + g),
                                      scalar2=None, op0=ALU.mult)
            if l == 0:
                dbg("ya_in", ya_in.all(), 128, 4 * S, BF16)
            ckpt("pool")

            A.off = mix0
            qdec = A.alloc(128, 2 * S, BF16, "qdec")
            kinv = A.alloc(128, 2 * S, BF16, "kinv")
            v_tm = A.alloc(128, 16 * 512, BF16, "v_tm")
            kinv_tm = A.alloc(128, 16 * 256, BF16, "kinv_tm")
            a1T = A.alloc(16, S, F32, "a1T")
            la = A.alloc(128, 256, F32, "la")
            Eq = A.alloc(128, 2 * TT, F32, "Eq")
            Ek = A.alloc(128, 2 * TT, F32, "Ek")
            dec = A.alloc(128, 2 * 32, F32, "dec")
            attm = [A.alloc(128, 64, BF16, f"attm{i}") for i in range(2)]
            S_f = [A.alloc(128, 128, F32, f"S_f{h}") for h in range(4)]
            S_t = [A.alloc(128, 128, F32, f"S_t{h}") for h in range(4)]
            S_b = [A.alloc(128, 128, BF16, f"S_b{h}") for h in range(4)]
            osq = A.alloc(128, TT, BF16, "osq")
            otmp = A.alloc(128, TT, F32, "otmp")
            wa2_sb = A.alloc(16, 256, F32, "wa2")
            ba_sb = A.alloc(1, 256, F32, "ba")
            set_rr(range(5))
            K.S.dma_start(out=wa2_sb.all(), in_=WA2[l * 16:(l + 1) * 16, :])
            K.S.dma_start(out=ba_sb.all(), in_=BA[l:l + 1, :])
            wr = wload(W1, l * D, 8, R0, 512)
            for fc in range(4):
                for tt in range(NTT):
                    bk = rr()
                    for kc in range(8):
                        K.T.matmul(out=bk.all(), lhsT=wr(kc, fc * 128, (fc + 1) * 128), rhs=hcol(kc, tt * TT, (tt + 1) * TT),
                                   start=(kc == 0), stop=(kc == 7))
                    K.A.activation(out=gla_out[:, fc * S + tt * TT:fc * S + (tt + 1) * TT], in_=bk.all(), func=AF.Silu)
            wsm = wload(W1, l * D, 8, SM0, 80)
            for tt in range(NTT):
                bk = rr()
                for kc in range(8):
                    K.T.matmul(out=bk[0:16, :], lhsT=wsm(kc, 64, 80), rhs=hcol(kc, tt * TT, (tt + 1) * TT), start=(kc == 0), stop=(kc == 7))
                evac_copy(a1T[0:16, tt * TT:(tt + 1) * TT], bk[0:16, :])
            wv = wload(W1, l * D, 8, V0, 512)
            for st in range(16):
                bk = rr()
                for kc in range(8):
                    K.T.matmul(out=bk.all(), lhsT=hcol(kc, st * 128, (st + 1) * 128), rhs=wv(kc, 0, 512), start=(kc == 0), stop=(kc == 7))
                evac_copy(v_tm[:, st * 512:(st + 1) * 512], bk.all())
            wqk = wload(W1, l * D, 8, QK0, 512)
            for tt in range(NTT):
                for s4 in range(4):
                    st = tt * 4 + s4
                    bz = rr()
                    K.T.matmul(out=bz[:, 0:256], lhsT=a1T[0:16, st * 128:(st + 1) * 128], rhs=wa2_sb.all(), start=True, stop=False)
                    K.T.matmul(out=bz[:, 0:256], lhsT=ones_f[0:1, 0:128], rhs=ba_sb.all(), start=False, stop=True)
                    K.A.activation(out=la.all(), in_=bz[:, 0:256], func=AF.Exp, scale=-1.0)
                    K.A.activation(out=la.all(), in_=la.all(), func=AF.Ln, bias=ones_f[:, 0:1], scale=1.0)
                    bc = rr()
                    for fc in range(2):
                        K.T.matmul(out=bc[:, fc * 128:(fc + 1) * 128], lhsT=la[:, fc * 128:(fc + 1) * 128], rhs=U_f, start=True, stop=True)
                    for fc in range(2):
                        K.A.activation(out=Eq[:, fc * TT + s4 * 128:fc * TT + (s4 + 1) * 128], in_=bc[:, fc * 128:(fc + 1) * 128],
                                       func=AF.Exp, scale=-1.0 / 16.0)
                        K.A.activation(out=Ek[:, fc * TT + s4 * 128:fc * TT + (s4 + 1) * 128], in_=bc[:, fc * 128:(fc + 1) * 128],
                                       func=AF.Exp, scale=1.0 / 16.0)
                        for hf in range(2):
                            n = st * 2 + hf
                            K.V.tensor_copy(out=dec[:, fc * 32 + n:fc * 32 + n + 1],
                                            in_=Eq[:, fc * TT + s4 * 128 + hf * 64 + 63:fc * TT + s4 * 128 + hf * 64 + 64])
                for fc in range(2):
                    bq = rr()
                    for kc in range(8):
                        K.T.matmul(out=bq.all(), lhsT=wqk(kc, fc * 128, (fc + 1) * 128), rhs=hcol(kc, tt * TT, (tt + 1) * TT),
                                   start=(kc == 0), stop=(kc == 7))
                    K.V.scalar_tensor_tensor(out=qdec[:, fc * S + tt * TT:fc * S + (tt + 1) * TT], in0=bq.all(), scalar=0.125,
                                             in1=Eq[:, fc * TT:(fc + 1) * TT], op0=ALU.mult, op1=ALU.mult)
                    bk2 = rr()
                    for kc in range(8):
                        K.T.matmul(out=bk2.all(), lhsT=wqk(kc, 256 + fc * 128, 256 + (fc + 1) * 128), rhs=hcol(kc, tt * TT, (tt + 1) * TT),
                                   start=(kc == 0), stop=(kc == 7))
                    K.V.tensor_tensor(out=kinv[:, fc * S + tt * TT:fc * S + (tt + 1) * TT], in0=bk2.all(), in1=Ek[:, fc * TT:(fc + 1) * TT], op=ALU.mult)
            for st in range(16):
                for fc in range(2):
                    K.T.transpose(out=bankT[:, fc * 128:(fc + 1) * 128], in_=kinv[:, fc * S + st * 128:fc * S + (st + 1) * 128], identity=ident_b.all())
                evac_copy(kinv_tm[:, st * 256:(st + 1) * 256], bankT[:, 0:256])
            if l == 0:
                dbg("qdec", qdec.all(), 128, 2 * S, BF16)
                dbg("kinv", kinv.all(), 128, 2 * S, BF16)
                dbg("dec", dec.all(), 128, 64)
            ckpt("gla1")
            for h in range(4):
                fc, r0 = h // 2, (h % 2) * 64
                P = slice(r0, r0 + 64)
                for tt in range(NTT):
                    bo = banks[5 + (h * NTT + tt) % 2]
                    for c8 in range(8):
                        n = tt * 8 + c8
                        st, hf = n // 2, n % 2
                        t0 = n * 64
                        H = slice(hf * 64, hf * 64 + 64)
                        qv = qdec[P, fc * S + t0:fc * S + t0 + 64]
                        ba_ = rr()
                        K.T.matmul(out=ba_[0:64, 0:64], lhsT=kinv[P, fc * S + t0:fc * S + t0 + 64], rhs=qv, start=True, stop=True)
                        am = attm[n % 2]
                        K.V.tensor_tensor(out=am[H, :], in0=ba_[0:64, 0:64], in1=cst[0:64, 256:320], op=ALU.mult)
                        oc = bo[:, c8 * 64:(c8 + 1) * 64]
                        if n > 0:
                            K.T.matmul(out=oc, lhsT=S_b[h][P, :], rhs=qv, start=True, stop=False)
                        K.T.matmul(out=oc, lhsT=v_tm[H, st * 512 + h * 128:st * 512 + (h + 1) * 128], rhs=am[H, :], start=(n == 0), stop=True)
                        if n < 31:
                            bkv = rr()
                            K.T.matmul(out=bkv[0:64, 0:128], lhsT=kinv_tm[H, st * 256 + h * 64:st * 256 + (h + 1) * 64],
                                       rhs=v_tm[H, st * 512 + h * 128:st * 512 + (h + 1) * 128], start=True, stop=True)
                            dcol = dec[P, fc * 32 + n:fc * 32 + n + 1]
                            if n == 0:
                                K.V.tensor_copy(out=S_t[h][P, :], in_=bkv[0:64, 0:128])
                            else:
                                K.V.tensor_tensor(out=S_t[h][P, :], in0=bkv[0:64, 0:128], in1=S_f[h][P, :], op=ALU.add)
                            K.A.activation(out=S_b[h][P, :], in_=S_t[h][P, :], func=AF.Copy, scale=dcol)
                            K.V.tensor_scalar(out=S_f[h][P, :], in0=S_t[h][P, :], scalar1=dcol, scalar2=None, op0=ALU.mult)
                    K.A.activation(out=osq.all(), in_=bo.all(), func=AF.Square)
                    bs = rr()
                    K.T.matmul(out=bs.all(), lhsT=ones_b.all(), rhs=osq.all(), start=True, stop=True)
                    rstd_from(bs.all(), 128)
                    K.V.tensor_tensor(out=otmp.all(), in0=bo.all(), in1=rs_sb.all(), op=ALU.mult)
                    go = gla_out[:, h * S + tt * TT:h * S + (tt + 1) * TT]
                    K.V.scalar_tensor_tensor(out=go, in0=otmp.all(), scalar=vcol(l, 36 + h), in1=go, op0=ALU.mult, op1=ALU.mult)
            if l == 0:
                dbg("gla_out", gla_out.all(), 128, 4 * S, BF16)
            ckpt("gla")

            A.off = mix0
            cqn = A.alloc(128, 3 * S, BF16, "cqn")
            ckvn = A.alloc(128, 2 * S, BF16, "ckvn")
            krope = A.alloc(128, S, BF16, "krope")
            Qh = [A.alloc(128, S, BF16, f"Qh{i}") for i in range(2)]
            Kh = [A.alloc(128, S, BF16, f"Kh{i}") for i in range(2)]
            Vh = [A.alloc(128, 16 * 128, BF16, f"Vh{i}") for i in range(2)]
            pt = [A.alloc(128, TT, BF16, f"pt{i}") for i in range(3)]
            sqh = A.alloc(128, S, BF16, "sqh")
            rt1 = A.alloc(128, TT, F32, "rt1")
            rt2 = A.alloc(128, TT, F32, "rt2")
            rden = A.alloc(128, TT, F32, "rden")
            nrow = A.alloc(1, S, F32, "nrow")
            kmx = A.alloc(1, 8, F32, "kmx")
            negm = [A.alloc(1, S, BF16, f"negm{i}") for i in range(2)]
            set_rr(range(5))
            for i in range(2):
                K.V.memset(ap=Vh[i].all().r("p (s e) -> p s e", e=128).sub(lambda ap: ap[:, :, 64:128]), constant=1.0)
            for (col0, nch, dstb, gb, nfeat) in ((CQ0, 3, cqn, 40, 384), (CKV0, 2, ckvn, 43, 256)):
                wc = wload(W1, l * D, 8, col0, nch * 128)
                for tt in range(NTT):
                    pb = [rr() for _ in range(nch)]
                    for c in range(nch):
                        for kc in range(8):
                            K.T.matmul(out=pb[c].all(), lhsT=wc(kc, c * 128, (c + 1) * 128), rhs=hcol(kc, tt * TT, (tt + 1) * TT),
                                       start=(kc == 0), stop=(kc == 7))
                    bs = rr()
                    for c in range(nch):
                        mq = pt[c]
                        K.A.activation(out=mq.all(), in_=pb[c].all(), func=AF.Square)
                        K.T.matmul(out=bs.all(), lhsT=ones_b.all(), rhs=mq.all(), start=(c == 0), stop=(c == nch - 1))
                    rstd_from(bs.all(), nfeat)
                    for c in range(nch):
                        K.V.scalar_tensor_tensor(out=dstb[:, c * S + tt * TT:c * S + (tt + 1) * TT], in0=pb[c].all(), scalar=vcol(l, gb + c),
                                                 in1=rs_sb.all(), op0=ALU.mult, op1=ALU.mult)
            wsm = wload(W1, l * D, 8, SM0, 80)
            R = slice(64, 96)
            for tt in range(NTT):
                bk = rr()
                for kc in range(8):
                    K.T.matmul(out=bk[0:64, :], lhsT=wsm(kc, 0, 64), rhs=hcol(kc, tt * TT, (tt + 1) * TT), start=(kc == 0), stop=(kc == 7))
                K.V.tensor_tensor(out=rt1[R, :], in0=bk[0:32, :], in1=ropeC[R, tt * TT:(tt + 1) * TT], op=ALU.mult)
                K.V.tensor_tensor(out=rt2[R, :], in0=bk[32:64, :], in1=ropeS[R, tt * TT:(tt + 1) * TT], op=ALU.mult)
                K.V.tensor_tensor(out=krope[R, tt * TT:(tt + 1) * TT], in0=rt1[R, :], in1=rt2[R, :], op=ALU.add)
            wuq = wload(WUQ, l * 384, 3, 0, 768)
            wuqs = wload(WUQS, l * 384, 3, 0, 768)
            wukv = wload(WUKV, l * 256, 2, 0, 1024)
            SCALE = 96.0 ** -0.5
            prr = [0]

            def pbank():
                prr[0] += 1
                return banks[3 + prr[0] % 2]

            def mla_prep(h):
                Q, Kt, V = Qh[h % 2], Kh[h % 2], Vh[h % 2]
                for tt in range(NTT):
                    T = slice(tt * TT, (tt + 1) * TT)
                    bq = pbank()
                    for c in range(3):
                        K.T.matmul(out=bq[0:96, :], lhsT=wuq(c, h * 96, (h + 1) * 96), rhs=cqn[:, c * S + tt * TT:c * S + (tt + 1) * TT],
                                   start=(c == 0), stop=(c == 2))
                    K.A.copy(out=Q[0:64, T], in_=bq[0:64, :])
                    K.V.tensor_tensor(out=rt1[R, :], in0=bq[R, :], in1=ropeC[R, T], op=ALU.mult)
                    bqs = pbank()
                    for c in range(3):
                        K.T.matmul(out=bqs[0:96, :], lhsT=wuqs(c, h * 96, (h + 1) * 96), rhs=cqn[:, c * S + tt * TT:c * S + (tt + 1) * TT],
                                   start=(c == 0), stop=(c == 2))
                    K.V.tensor_tensor(out=rt2[R, :], in0=bqs[R, :], in1=ropeS[R, T], op=ALU.mult)
                    K.V.tensor_tensor(out=Q[R, T], in0=rt1[R, :], in1=rt2[R, :], op=ALU.add)
                    yield
                    bkk = pbank()
                    for c in range(2):
                        K.T.matmul(out=bkk[0:64, :], lhsT=wukv(c, h * 64, (h + 1) * 64), rhs=ckvn[:, c * S + tt * TT:c * S + (tt + 1) * TT],
                                   start=(c == 0), stop=(c == 1))
                    K.A.copy(out=Kt[0:64, T], in_=bkk[0:64, :])
                    K.V.tensor_copy(out=Kt[R, T], in_=krope[R, T])
                    yield
                for st in range(16):
                    bv = pbank()
                    for c in range(2):
                        K.T.matmul(out=bv[:, 0:64], lhsT=ckvn[:, c * S + st * 128:c * S + (st + 1) * 128], rhs=wukv(c, 512 + h * 64, 512 + (h + 1) * 64),
                                   start=(c == 0), stop=(c == 1))
                    evac_copy(V[:, st * 128:st * 128 + 64], bv[:, 0:64])
                    if st % 4 == 3:
                        yield
                K.A.activation(out=sqh[0:96, :], in_=Kt[0:96, :], func=AF.Square)
                for tt in range(NTT):
                    bn = pbank()
                    K.T.matmul(out=bn[0:1, :], lhsT=ones_b[0:96, 0:1], rhs=sqh[0:96, tt * TT:(tt + 1) * TT], start=True, stop=True)
                    K.V.tensor_reduce(out=kmx[0:1, tt:tt + 1], in_=bn[0:1, :], axis=AX.X, op=ALU.max)
                K.V.tensor_reduce(out=kmx[0:1, 4:5], in_=kmx[0:1, 0:4], axis=AX.X, op=ALU.max)
                yield
                K.A.activation(out=sqh[0:96, :], in_=Q[0:96, :], func=AF.Square)
                for tt in range(NTT):
                    bn = pbank()
                    K.T.matmul(out=bn[0:1, :], lhsT=ones_b[0:96, 0:1], rhs=sqh[0:96, tt * TT:(tt + 1) * TT], start=True, stop=True)
                    K.V.tensor_scalar(out=nrow[0:1, tt * TT:(tt + 1) * TT], in0=bn[0:1, :], scalar1=kmx[0:1, 4:5], scalar2=None, op0=ALU.mult)
                K.A.activation(out=nrow.all(), in_=nrow.all(), func=AF.Sqrt)
                K.V.tensor_scalar(out=negm[h % 2].all(), in0=nrow.all(), scalar1=-1.0, scalar2=None, op0=ALU.mult)
                yield

            pt_i = [0]

            def mla_attend(h, gen):
                Q, Kt, V = Qh[h % 2], Kh[h % 2], Vh[h % 2]
                iters = [(qb, kt) for qb in range(4) for kt in range(4 * qb + 4)]
                sbank = {}
                LOOK = 2

                def score(i):
                    qb, kt = iters[i]
                    r = kt - 4 * qb
                    c0 = max(r, 0) * 128
                    q0 = qb * TT + c0
                    bs_ = rr()
                    sbank[i] = bs_
                    K.T.matmul(out=bs_[:, c0:TT], lhsT=Kt[0:96, kt * 128:(kt + 1) * 128], rhs=Q[0:96, q0:(qb + 1) * TT], start=True, stop=False)
                    K.T.matmul(out=bs_[:, c0:TT], lhsT=ones_b[0:1, 0:128], rhs=negm[h % 2][0:1, q0:(qb + 1) * TT], start=False, stop=(r < 0))
                    if r >= 0:
                        K.T.matmul(out=bs_[:, c0:TT], lhsT=ident_b.all(), rhs=maskneg_b[:, 0:TT - c0], start=False, stop=True)

                def exp_pv(i):
                    qb, kt = iters[i]
                    nk = 4 * qb + 4
                    r = kt - 4 * qb
                    c0 = max(r, 0) * 128
                    bo = banks[5 + (h * 4 + qb) % 2]
                    bs_ = sbank.pop(i)
                    p = pt[pt_i[0] % 3]
                    pt_i[0] += 1
                    K.A.activation(out=p[:, c0:TT], in_=bs_[:, c0:TT], func=AF.Exp, scale=SCALE)
                    K.T.matmul(out=bo[:, c0:TT], lhsT=V[:, kt * 128:(kt + 1) * 128], rhs=p[:, c0:TT], start=(kt == 0), stop=(kt == nk - 1))
                    if kt == nk - 1:
                        K.V.reciprocal(out=rden[64:128, :], in_=bo[64:128, :])
                        rr0 = (h % 2) * 64
                        K.V.tensor_tensor(out=attn_out[rr0:rr0 + 64, (h // 2) * S + qb * TT:(h // 2) * S + (qb + 1) * TT], in0=bo[0:64, :],
                                          in1=rden[64:128, :], op=ALU.mult)

                for i in range(len(iters) + LOOK):
                    if i < len(iters):
                        score(i)
                    if i - LOOK >= 0:
                        exp_pv(i - LOOK)
                    if gen is not None and i % 2 == 1:
                        next(gen, None)
                if gen is not None:
                    for _ in gen:
                        pass

            set_rr(range(3))
            for _ in mla_prep(0):
                pass
            for h in range(8):
                mla_attend(h, mla_prep(h + 1) if h + 1 < 8 else None)
            if l == 0:
                dbg("attn_out", attn_out.all(), 128, 4 * S, BF16)
            ckpt("mla")

            A.off = mix0
            merged = A.alloc(128, 8 * S, BF16, "merged")
            sig = [A.alloc(128, TT, F32, f"sig{i}") for i in range(3)]
            acc = [A.alloc(128, TT, F32, f"acc{i}") for i in range(2)]
            yins = (ya_in, gla_out, attn_out)
            set_rr(range(7))
            for m in range(8):
                wg = wload(W1, l * D, 8, G0 + m * 384, 384)
                wy = wload(WABC, l * 1536, 12, m * 128, 128)
                for tt in range(NTT):
                    ac = acc[(m * NTT + tt) % 2]
                    for b in range(3):
                        bg, by = rr(), rr()
                        for kc in range(8):
                            K.T.matmul(out=bg.all(), lhsT=wg(kc, b * 128, (b + 1) * 128), rhs=hcol(kc, tt * TT, (tt + 1) * TT),
                                       start=(kc == 0), stop=(kc == 7))
                        for c in range(4):
                            K.T.matmul(out=by.all(), lhsT=wy(b * 4 + c, 0, 128), rhs=yins[b][:, c * S + tt * TT:c * S + (tt + 1) * TT],
                                       start=(c == 0), stop=(c == 3))
                        K.A.activation(out=sig[b].all(), in_=bg.all(), func=AF.Sigmoid)
                        if b == 0:
                            K.V.tensor_tensor(out=ac.all(), in0=by.all(), in1=sig[b].all(), op=ALU.mult)
                        else:
                            K.V.tensor_tensor(out=sig[b].all(), in0=by.all(), in1=sig[b].all(), op=ALU.mult)
                            if b == 1:
                                K.V.tensor_tensor(out=ac.all(), in0=ac.all(), in1=sig[b].all(), op=ALU.add)
                            else:
                                K.V.tensor_tensor(out=merged[:, m * S + tt * TT:m * S + (tt + 1) * TT], in0=ac.all(), in1=sig[b].all(), op=ALU.add)
            if l == 0:
                dbg("merged", merged.all(), 128, 8 * S, BF16)
            ckpt("merge")

            xt = A.alloc(128, 8 * TT, F32, "xt")
            tt_ = A.alloc(128, 8 * TT, F32, "ttile")
            sq = [A.alloc(128, TT, BF16, f"sq{i}") for i in range(2)]
            wo0 = wload(WO, l * D, 8, 0, 512)
            wo1 = wload(WO, l * D, 8, 512, 512)
            for tt in range(NTT):
                K.S.dma_start(out=xt.all().r("p (c t) -> p c t", c=8), in_=xs_view(tt))
                bsum = banks[6]
                set_rr(range(6))
                for m in range(8):
                    wo_ = wo0 if m < 4 else wo1
                    bk = rr()
                    for kc in range(8):
                        K.T.matmul(out=bk.all(), lhsT=wo_(kc, (m % 4) * 128, (m % 4 + 1) * 128), rhs=merged[:, kc * S + tt * TT:kc * S + (tt + 1) * TT],
                                   start=(kc == 0), stop=(kc == 7))
                    K.V.tensor_copy(out=tt_[:, m * TT:(m + 1) * TT], in_=bk.all())
                    K.A.activation(out=sq[m % 2].all(), in_=bk.all(), func=AF.Square)
                    if m > 0:
                        K.T.matmul(out=bsum.all(), lhsT=ones_b.all(), rhs=sq[(m - 1) % 2].all(), start=(m == 1), stop=False)
                K.T.matmul(out=bsum.all(), lhsT=ones_b.all(), rhs=sq[7 % 2].all(), start=False, stop=True)
                rstd_from(bsum.all(), D)
                for m in range(8):
                    K.V.scalar_tensor_tensor(out=tt_[:, m * TT:(m + 1) * TT], in0=tt_[:, m * TT:(m + 1) * TT], scalar=vcol(l, 8 + m),
                                             in1=rs_sb.all(), op0=ALU.mult, op1=ALU.mult)
                    K.V.tensor_tensor(out=xt[:, m * TT:(m + 1) * TT], in0=xt[:, m * TT:(m + 1) * TT], in1=tt_[:, m * TT:(m + 1) * TT], op=ALU.add)
                K.S.dma_start(out=xs_view(tt), in_=xt.all().r("p (c t) -> p c t", c=8))
                norm_to_h(xt, sq, l, 16, tt)
            if l == 0:
                dbg("h2", hT.all(), 128, 8 * S, BF16)
            ckpt("wo")

            A = Arena()
            act = A.alloc(128, 22 * S, BF16, "act")
            xt = A.alloc(128, 8 * TT, F32, "xt")
            ft = A.alloc(128, 8 * TT, F32, "ftile")
            sq = [A.alloc(128, TT, BF16, f"sq{i}") for i in range(2)]
            sg = [A.alloc(128, TT, F32, f"sg{i}") for i in range(2)]
            otile = [K.sbuf(128, D, F32, f"otile{i}", off=ft.off + i * D * 4) for i in range(2)]
            set_rr(range(7))
            for j in range(11):
                wgu = wload(WGU, l * D, 8, j * 512, 512)
                for pp in range(2):
                    jj = 2 * j + pp
                    for tt in range(NTT):
                        bg, bu = rr(), rr()
                        for kc in range(8):
                            K.T.matmul(out=bg.all(), lhsT=wgu(kc, pp * 256, pp * 256 + 128), rhs=hcol(kc, tt * TT, (tt + 1) * TT),
                                       start=(kc == 0), stop=(kc == 7))
                        for kc in range(8):
                            K.T.matmul(out=bu.all(), lhsT=wgu(kc, pp * 256 + 128, pp * 256 + 256), rhs=hcol(kc, tt * TT, (tt + 1) * TT),
                                       start=(kc == 0), stop=(kc == 7))
                        s_ = sg[(jj * NTT + tt) % 2]
                        K.A.activation(out=s_.all(), in_=bg.all(), func=AF.Silu)
                        K.V.tensor_tensor(out=act[:, jj * S + tt * TT:jj * S + (tt + 1) * TT], in0=bu.all(), in1=s_.all(), op=ALU.mult)
            for tt in range(NTT):
                K.S.dma_start(out=xt.all().r("p (c t) -> p c t", c=8), in_=xs_view(tt))
                bsum = banks[6]
                set_rr(range(6))
                for m in range(8):
                    wd = wload(WD, l * DFF, 22, m * 128, 128)
                    bk = rr()
                    for kc in range(22):
                        K.T.matmul(out=bk.all(), lhsT=wd(kc, 0, 128), rhs=act[:, kc * S + tt * TT:kc * S + (tt + 1) * TT], start=(kc == 0), stop=(kc == 21))
                    K.V.tensor_copy(out=ft[:, m * TT:(m + 1) * TT], in_=bk.all())
                    K.A.activation(out=sq[m % 2].all(), in_=bk.all(), func=AF.Square)
                    if m > 0:
                        K.T.matmul(out=bsum.all(), lhsT=ones_b.all(), rhs=sq[(m - 1) % 2].all(), start=(m == 1), stop=False)
                K.T.matmul(out=bsum.all(), lhsT=ones_b.all(), rhs=sq[7 % 2].all(), start=False, stop=True)
                rstd_from(bsum.all(), D)
                for m in range(8):
                    K.V.scalar_tensor_tensor(out=ft[:, m * TT:(m + 1) * TT], in0=ft[:, m * TT:(m + 1) * TT], scalar=vcol(l, 24 + m),
                                             in1=rs_sb.all(), op0=ALU.mult, op1=ALU.mult)
                    K.V.tensor_tensor(out=xt[:, m * TT:(m + 1) * TT], in0=xt[:, m * TT:(m + 1) * TT], in1=ft[:, m * TT:(m + 1) * TT], op=ALU.add)
                if l < depth - 1:
                    K.S.dma_start(out=xs_view(tt), in_=xt.all().r("p (c t) -> p c t", c=8))
                    norm_to_h(xt, sq, l + 1, 0, tt)
                else:
                    for s4 in range(4):
                        ot = otile[s4 % 2]
                        for half in range(2):
                            bk = rr()
                            for j in range(4):
                                c = half * 4 + j
                                K.T.transpose(out=bk[:, j * 128:(j + 1) * 128], in_=xt[:, c * TT + s4 * 128:c * TT + (s4 + 1) * 128], identity=ident_f)
                            evac_copy(ot[:, half * 512:(half + 1) * 512], bk.all())
                        r0 = (tt * 4 + s4) * 128
                        i = K.S.dma_start(out=dram_view(out_d[r0:r0 + 128, :], "out", r0, r0 + 128), in_=ot.all())
                        out_toks.append(i.tok)

    except _Stop:
        pass
    K.wait_all("sync", out_toks + list(dbg_out.values()))
    K.emit()
    K.close()
    return nc


def host_prep(inputs):
    f = np.float32
    w_in = np.asarray(inputs["w_in"], f)
    Ld = w_in.shape[0]
    O_POOL, O_Q, O_K, O_V, O_R, O_A1, O_CQ, O_CKV, O_KR, O_G = 0, 512, 768, 1024, 1536, 2048, 2064, 2448, 2704, 2736
    idx = []
    idx += list(range(O_POOL, O_POOL + 512))
    idx += list(range(O_Q, O_Q + 256)) + list(range(O_K, O_K + 256))
    idx += list(range(O_R, O_R + 512))
    idx += list(range(O_V, O_V + 512))
    idx += list(range(O_CQ, O_CQ + 384))
    idx += list(range(O_CKV, O_CKV + 256))
    idx += list(range(O_KR, O_KR + 32))
    idx += list(range(O_KR + 16, O_KR + 32)) + list(range(O_KR, O_KR + 16))
    idx += list(range(O_A1, O_A1 + 16))
    for m in range(8):
        for b in range(3):
            idx += list(range(O_G + b * 1024 + m * 128, O_G + b * 1024 + (m + 1) * 128))
    idx = np.asarray(idx)
    assert idx.shape[0] == W1C
    w1 = np.ascontiguousarray(w_in[:, :, idx]).reshape(Ld * D, W1C)
    wp = np.ascontiguousarray(np.asarray(inputs["w_pool"], f).transpose(0, 2, 1, 3)).reshape(Ld * 128, 512)
    wabc = np.concatenate([np.asarray(inputs["w_a"], f), np.asarray(inputs["w_b"], f), np.asarray(inputs["w_c"], f)], axis=1).reshape(Ld * 1536, D)
    wa2 = np.ascontiguousarray(np.asarray(inputs["w_gla_a2"], f)).reshape(Ld * 16, 256)
    ba = np.ascontiguousarray(np.asarray(inputs["b_gla_a"], f)).reshape(Ld, 256)
    w_uq = np.asarray(inputs["w_mla_uq"], f)
    sw = []
    for h in range(8):
        sw += list(range(h * 96, h * 96 + 64)) + list(range(h * 96 + 80, h * 96 + 96)) + list(range(h * 96 + 64, h * 96 + 80))
    wuq = np.ascontiguousarray(w_uq).reshape(Ld * 384, 768)
    wuqs = np.ascontiguousarray(w_uq[:, :, np.asarray(sw)]).reshape(Ld * 384, 768)
    pk = []
    for h in range(8):
        pk += list(range(h * 128, h * 128 + 64))
    for h in range(8):
        pk += list(range(h * 128 + 64, h * 128 + 128))
    wukv = np.ascontiguousarray(np.asarray(inputs["w_mla_ukv"], f)[:, :, np.asarray(pk)]).reshape(Ld * 256, 1024)
    wo = np.ascontiguousarray(np.asarray(inputs["w_o"], f)).reshape(Ld * D, D)
    pg = []
    for j in range(22):
        pg += list(range(j * 128, (j + 1) * 128)) + list(range(DFF + j * 128, DFF + (j + 1) * 128))
    wgu = np.ascontiguousarray(np.asarray(inputs["w_ffn_gu"], f)[:, :, np.asarray(pg)]).reshape(Ld * D, 2 * DFF)
    wd = np.ascontiguousarray(np.asarray(inputs["w_ffn_down"], f)).reshape(Ld * DFF, D)
    vecs = np.zeros((128, Ld * NV), f)
    for l in range(Ld):
        cols = []
        for name, n in (("norm_pre_mix", 8), ("norm_post_mix", 8), ("norm_pre_ffn", 8), ("norm_post_ffn", 8), ("pool_scale", 4),
                        ("gla_norm", 4), ("mla_q_norm", 3), ("mla_kv_norm", 2)):
            v = np.asarray(inputs[name], f)[l]
            cols.append(v.reshape(n, 128).T)
        vecs[:, l * NV:(l + 1) * NV] = np.concatenate(cols, axis=1)
    cst = np.zeros((128, NCST), f)
    cst[:, 0:128] = np.eye(128, dtype=f)
    s = np.arange(128)
    cst[:, 128:256] = ((s[:, None] // 64 == s[None, :] // 64) & (s[:, None] <= s[None, :])).astype(f)
    cst[:, 256:320] = ((s[:, None] % 64) <= np.arange(64)[None, :]).astype(f)
    cst[:, 320:336] = (1.0 / (np.arange(16) + 1.0)).astype(f)[None, :]
    invf = (10000.0 ** (-np.arange(0, 32, 2, dtype=np.float32) / 32.0)).astype(f)
    cst[64:80, 336] = invf
    cst[80:96, 336] = invf
    cst[64:80, 337] = -1.0
    cst[80:96, 337] = 1.0
    q = np.arange(128)
    cst[:, 338:338 + 128] = np.where(q[None, :] < s[:, None], -30000.0, 0.0).astype(f)
    shared = dict(cst=cst, vecs=vecs, w1=w1, wp=wp, wabc=wabc, wa2=wa2, ba=ba, wuq=wuq, wuqs=wuqs, wukv=wukv, wo=wo, wgu=wgu, wd=wd)
    return shared


def kernel(**inputs):
    x = np.asarray(inputs["x"], np.float32)
    pos = np.asarray(inputs["positions"], np.int32)
    B = x.shape[0]
    shared = host_prep(inputs)
    nc = build_program(L_FULL)
    in_maps = []
    for b in range(B):
        m = dict(shared)
        m["x"] = np.ascontiguousarray(x[b])
        m["pos"] = np.ascontiguousarray(np.broadcast_to(pos[b][None, :], (128, S)))
        in_maps.append(m)
    res = run_bass_kernel_spmd(nc, in_maps, core_ids=list(range(B)))
    return np.stack([np.asarray(r["out"], np.float32) for r in res.results], axis=0)
```

```python
import numpy as np
from contextlib import ExitStack
import concourse.bass as bass
import concourse.mybir as mybir

F32 = mybir.dt.float32
BF16 = mybir.dt.bfloat16
I32 = mybir.dt.int32
AF = mybir.ActivationFunctionType
ALU = mybir.AluOpType
AX = mybir.AxisListType
ESZ = {F32: 4, BF16: 2, I32: 4}

SAME_ENGINE_SYNC = True
SEM_EPOCH = 30000
N_DMA_SEMS = 8


class View:
    def __init__(self, ap, space, p0, p1, b0, b1):
        self.ap, self.space, self.p0, self.p1, self.b0, self.b1 = ap, space, p0, p1, b0, b1

    def r(self, pattern, **kw):
        return View(self.ap.rearrange(pattern, **kw), self.space, self.p0, self.p1, self.b0, self.b1)

    def bc(self, shape):
        return View(self.ap.to_broadcast(shape), self.space, self.p0, self.p1, self.b0, self.b1)

    def bitcast(self, dt):
        return View(self.ap.bitcast(dt), self.space, self.p0, self.p1, self.b0, self.b1)

    def sub(self, fn):
        return View(fn(self.ap), self.space, self.p0, self.p1, self.b0, self.b1)


class Buf:
    def __init__(self, handle, space, P, F, dtype, off):
        self.t, self.space, self.P, self.F, self.dtype, self.off = handle, space, P, F, dtype, off
        self.esz = ESZ[dtype]

    def __getitem__(self, key):
        ps, cs = key
        p0, p1, _ = ps.indices(self.P)
        c0, c1, _ = cs.indices(self.F)
        return View(self.t[p0:p1, c0:c1], self.space, p0, p1,
                    self.off + c0 * self.esz, self.off + c1 * self.esz)

    def all(self):
        return self[:, :]


class DView(View):
    pass


def dram_view(ap, name, lo, hi):
    return View(ap, "d:" + name, 0, 1, lo, hi)


class Ins:
    __slots__ = ("eng", "fn", "kw", "waits", "tok", "inc", "is_dma")


class EngProxy:
    def __init__(self, K, eng):
        self.K, self.eng = K, eng

    def __getattr__(self, fn):
        def call(**kw):
            return self.K._record(self.eng, fn, kw)
        return call


OUT_KEYS = ("out", "accum_out", "ap")


class Kern:
    def __init__(self, nc):
        self.nc = nc
        self.es = ExitStack()
        self.sb_off = 229376 - nc.sbuf_bytes_remaining
        self.sb_end = 229376
        self.streams = {e: [] for e in ("tensor", "vector", "scalar", "gpsimd", "sync")}
        self.sems = {}
        self.cnt = {e: 0 for e in self.streams}
        self.dma_sems = {}
        self.dma_rr = {e: 0 for e in self.streams}
        self.seen = {e: {} for e in self.streams}
        self.recs = {}
        self.T = EngProxy(self, "tensor")
        self.V = EngProxy(self, "vector")
        self.A = EngProxy(self, "scalar")
        self.G = EngProxy(self, "gpsimd")
        self.S = EngProxy(self, "sync")
        self.nsem = 0
        self.nbuf = 0

    def sem(self, name):
        self.nsem += 1
        return self.es.enter_context(self.nc.semaphore(name))

    def sbuf(self, P, F, dtype, name=None, off=None):
        esz = ESZ[dtype]
        nbytes = F * esz
        if off is None:
            off = (self.sb_off + 63) // 64 * 64
            self.sb_off = off + nbytes
            assert self.sb_off <= self.sb_end, (name, self.sb_off)
        self.nbuf += 1
        name = f"{name or 'sb'}_{self.nbuf}"
        h = self.nc.alloc_sbuf_tensor_at(name, [P, F], dtype, offset=off)
        return Buf(h, "sb", P, F, dtype, off)

    def psum_banks(self, n=8):
        banks = []
        for i in range(n):
            h = self.es.enter_context(self.nc.psum_tensor(f"psb{i}", [128, 512], F32))
            banks.append(Buf(h, "ps", 128, 512, F32, i * 2048))
        return banks

    def _tok_compute(self, eng):
        self.cnt[eng] += 1
        c = self.cnt[eng]
        ep = (c - 1) // SEM_EPOCH
        lst = self.sems.setdefault(eng, [])
        while len(lst) <= ep:
            lst.append(self.sem(f"s_{eng}_{len(lst)}"))
        return (lst[ep], c - ep * SEM_EPOCH)

    def _record(self, eng, fn, kw):
        ins = Ins()
        ins.eng, ins.fn, ins.kw, ins.is_dma = eng, fn, kw, fn.startswith("dma_start")
        reads, writes = [], []
        for k, v in list(kw.items()):
            if isinstance(v, View):
                if v.space == "ps":
                    bb = (v.b0 // 2048) * 2048
                    v = View(v.ap, "ps", (v.p0 // 32) * 32, ((v.p1 + 31) // 32) * 32, bb, bb + 2048)
                    kw[k] = v
                (writes if k in OUT_KEYS else reads).append(v)
        deps = {}

        def add_dep(tok):
            s, val = tok
            if deps.get(id(s), (None, 0))[1] < val:
                deps[id(s)] = (s, val)

        for v, is_w in [(x, False) for x in reads] + [(x, True) for x in writes]:
            lst = self.recs.setdefault(v.space, [])
            for r in lst:
                if r[0] < v.p1 and v.p0 < r[1] and r[2] < v.b1 and v.b0 < r[3] and (is_w or r[4] or (v.space == "ps" and r[5] != eng)):
                    if r[5] == eng and not r[6] and not ins.is_dma:
                        if eng == "tensor" or not SAME_ENGINE_SYNC:
                            continue
                    add_dep(r[7])
        if ins.is_dma:
            pool = self.dma_sems.setdefault(eng, [])
            if len(pool) < N_DMA_SEMS:
                pool.append([self.sem(f"d_{eng}_{len(pool)}"), 0])
            i = self.dma_rr[eng] % len(pool) if len(pool) == N_DMA_SEMS else len(pool) - 1
            self.dma_rr[eng] += 1
            ent = pool[i]
            if ent[1] > 0:
                add_dep((ent[0], ent[1]))
            ent[1] += 16
            ins.tok, ins.inc = (ent[0], ent[1]), 16
        else:
            ins.tok, ins.inc = self._tok_compute(eng), 1
        seen = self.seen[eng]
        ins.waits = []
        for s, val in deps.values():
            if seen.get(id(s), 0) < val:
                seen[id(s)] = val
                ins.waits.append((s, val))
        for v, is_w in [(x, False) for x in reads] + [(x, True) for x in writes]:
            lst = self.recs[v.space]
            new = []
            for r in lst:
                covered = v.p0 <= r[0] and r[1] <= v.p1 and v.b0 <= r[2] and r[3] <= v.b1
                if covered and (is_w or (not r[4] and ((r[5] == eng and not r[6] and not ins.is_dma) or v.space == "ps"))):
                    continue
                new.append(r)
            new.append([v.p0, v.p1, v.b0, v.b1, is_w, eng, ins.is_dma, ins.tok])
            self.recs[v.space] = new
        self.streams[eng].append(ins)
        return ins

    def wait_all(self, eng, toks):
        ins = Ins()
        ins.eng, ins.fn, ins.kw, ins.is_dma = eng, None, {}, False
        ins.waits = list(toks)
        ins.tok, ins.inc = None, 0
        self.streams[eng].append(ins)

    def emit(self):
        nc = self.nc
        block = self.es.enter_context(nc.Block())

        def run(engname):
            def body(e):
                for ins in self.streams[engname]:
                    for s, val in ins.waits:
                        e.wait_ge(s, val)
                    if ins.fn is None:
                        continue
                    kw = {k: (v.ap if isinstance(v, View) else v) for k, v in ins.kw.items()}
                    r = getattr(e, ins.fn)(**kw)
                    r.then_inc(ins.tok[0], ins.inc)
            return body

        block.tensor(run("tensor"))
        block.vector(run("vector"))
        block.scalar(run("scalar"))
        block.gpsimd(run("gpsimd"))
        block.sync(run("sync"))

    def close(self):
        self.es.close()


import math
from concourse.bass_utils import run_bass_kernel_spmd

D = 1024
S = 2048
L_FULL = 4
DFF = 2816
NV = 45
NCST = 850
EPS = 1e-6
TT = 512
NTT = S // TT
W1C = 5840
POOL0, QK0, R0, V0, CQ0, CKV0, SM0, G0 = 0, 512, 1024, 1536, 2048, 2432, 2688, 2768
WSLOT = 4096
NSLOT = 3
C1_2PI = 6.28125
C2_2PI = 2.0 * math.pi - 6.28125
import os
NOALIAS0 = bool(int(os.environ.get('NOALIAS0', '0')))


class _Stop(Exception):
    pass


def build_program(depth=L_FULL, debug=(), stop=None):
    nc = bass.Bass("TRN2", target_bir_lowering=False)
    K = Kern(nc)

    def din(name, shape, dt=F32):
        return nc.dram_tensor(name, shape, dt, kind="ExternalInput").ap()

    x_in = din("x", [S, D])
    pos_in = din("pos", [128, S], I32)
    cst_in = din("cst", [128, NCST])
    vecs_in = din("vecs", [128, L_FULL * NV])
    W1 = din("w1", [L_FULL * D, W1C])
    WP = din("wp", [L_FULL * 128, 512])
    WABC = din("wabc", [L_FULL * 1536, D])
    WA2 = din("wa2", [L_FULL * 16, 256])
    BA = din("ba", [L_FULL, 256])
    WUQ = din("wuq", [L_FULL * 384, 768])
    WUQS = din("wuqs", [L_FULL * 384, 768])
    WUKV = din("wukv", [L_FULL * 256, 1024])
    WO = din("wo", [L_FULL * D, D])
    WGU = din("wgu", [L_FULL * D, 2 * DFF])
    WD = din("wd", [L_FULL * DFF, D])
    out_d = nc.dram_tensor("out", [S, D], F32, kind="ExternalOutput").ap()
    xs_d = nc.dram_tensor("xs", [128, 8 * S], F32, kind="Internal").ap()
    dbg_out = {}

    banks = []
    for i in range(7):
        h = K.es.enter_context(nc.psum_tensor(f"psb{i}", [128, 512], F32))
        banks.append(Buf(h, "ps", 128, 512, F32, i * 2048))
    hb = K.es.enter_context(nc.psum_tensor("psb7", [128, 1024], BF16))
    bankT = Buf(hb, "ps", 128, 1024, BF16, 7 * 2048)
    rr_state = {"list": list(range(7)), "i": 0}

    def set_rr(lst):
        rr_state["list"], rr_state["i"] = list(lst), 0

    def rr():
        b = banks[rr_state["list"][rr_state["i"] % len(rr_state["list"])]]
        rr_state["i"] += 1
        return b

    cst = K.sbuf(128, NCST, F32, "cst")
    vecs = K.sbuf(128, L_FULL * NV, F32, "vecs")
    ident_b = K.sbuf(128, 128, BF16, "identb")
    ones_b = K.sbuf(128, 128, BF16, "onesb")
    ones_f = K.sbuf(128, 128, F32, "onesf")
    maskneg_b = K.sbuf(128, 512, BF16, "maskneg")
    epsc = K.sbuf(128, 1, F32, "eps")
    ropeC = K.sbuf(128, S, F32, "ropeC")
    ropeS = K.sbuf(128, S, F32, "ropeS")
    hT = K.sbuf(128, 8 * S, BF16, "hT")
    wslots = [K.sbuf(128, WSLOT, BF16, f"wslot{i}") for i in range(NSLOT)]
    rs_sb = K.sbuf(128, 512, F32, "rs")
    ident_fb = K.sbuf(128, 128, F32, "identf")
    U_fb = K.sbuf(128, 128, F32, "Uf")
    ident_f = ident_fb.all()
    U_f = U_fb.all()
    arena0 = K.sb_off
    ARENA_END = K.sb_end
    gmask = cst[:, 256:320]
    invcnt = cst[:, 320:336]
    invf = cst[:, 336:337]
    sgn = cst[:, 337:338]

    class Arena:
        def __init__(self):
            self.off = arena0

        def alloc(self, P, F, dt, name):
            b = K.sbuf(P, F, dt, name, off=(self.off + 63) // 64 * 64)
            self.off = b.off + F * ESZ[dt]
            assert self.off <= ARENA_END, (name, self.off, ARENA_END)
            return b

    ws_i = [0]

    def wload(W2d, row0, nk, c0, ncols):
        assert nk * ncols <= WSLOT
        slot = wslots[ws_i[0] % NSLOT]
        ws_i[0] += 1
        dst = slot[:, 0:nk * ncols]
        src = W2d[row0:row0 + nk * 128, c0:c0 + ncols].rearrange("(k p) n -> p k n", p=128)
        K.G.dma_start(out=dst.r("p (k n) -> p k n", k=nk), in_=src)

        def w(k, a, b):
            return slot[:, k * ncols + a:k * ncols + b]
        return w

    evac_i = [0]

    def evac_copy(out, in_):
        evac_i[0] += 1
        if evac_i[0] % 2:
            K.A.copy(out=out, in_=in_)
        else:
            K.V.tensor_copy(out=out, in_=in_)

    def hcol(c, t0, t1):
        return hT[:, c * S + t0:c * S + t1]

    def vcol(l, j):
        return vecs[:, l * NV + j:l * NV + j + 1]

    def dbg(name, view, P, F, dt=F32):
        if name not in debug:
            return
        d = nc.dram_tensor("dbg_" + name, [P, F], dt, kind="ExternalOutput").ap()
        i = K.S.dma_start(out=dram_view(d, "dbg_" + name, 0, 1), in_=view)
        dbg_out[name] = i.tok

    def rstd_from(bank_view, n, P=128):
        K.A.activation(out=rs_sb[0:P, :], in_=bank_view, func=AF.Sqrt, bias=epsc[0:P, :], scale=1.0 / n)
        K.V.reciprocal(out=rs_sb[0:P, :], in_=rs_sb[0:P, :])

    def norm_to_h(xt, sq, l_next, gbase, tt):
        bk = rr()
        for c in range(8):
            K.A.activation(out=sq[c % 2].all(), in_=xt[:, c * TT:(c + 1) * TT], func=AF.Square)
            K.T.matmul(out=bk.all(), lhsT=ones_b.all(), rhs=sq[c % 2].all(), start=(c == 0), stop=(c == 7))
        rstd_from(bk.all(), D)
        for c in range(8):
            K.V.scalar_tensor_tensor(out=hcol(c, tt * TT, (tt + 1) * TT), in0=xt[:, c * TT:(c + 1) * TT],
                                     scalar=vcol(l_next, gbase + c), in1=rs_sb.all(), op0=ALU.mult, op1=ALU.mult)

    def xs_view(tt):
        ap = xs_d.rearrange("p (c t) -> p c t", c=8)[:, :, tt * TT:(tt + 1) * TT]
        return dram_view(ap, "xs", tt, tt + 1)

    out_toks = []

    def ckpt(name):
        if stop == name:
            raise _Stop()

    try:
        K.S.dma_start(out=cst.all(), in_=cst_in)
        K.S.dma_start(out=vecs.all(), in_=vecs_in)
        K.V.tensor_copy(out=ident_f, in_=cst[:, 0:128])
        K.V.tensor_copy(out=U_f, in_=cst[:, 128:256])
        K.V.tensor_copy(out=ident_b.all(), in_=cst[:, 0:128])
        K.V.memset(ap=ones_b.all(), constant=1.0)
        K.V.memset(ap=ones_f.all(), constant=1.0)
        K.V.memset(ap=epsc.all(), constant=EPS)
        K.V.tensor_copy(out=maskneg_b.all(), in_=cst[:, 338:850])

        A = Arena()
        pos_i = A.alloc(128, S, I32, "posi")
        ang = A.alloc(128, S, F32, "ang")
        nfl = A.alloc(128, S, F32, "nfl")
        n_i = A.alloc(128, S, I32, "ni")
        K.S.dma_start(out=pos_i.all(), in_=pos_in)
        R = slice(64, 96)
        K.V.tensor_copy(out=ang[R, :], in_=pos_i[R, :])
        K.V.tensor_scalar(out=ang[R, :], in0=ang[R, :], scalar1=cst[R, 336:337], scalar2=None, op0=ALU.mult)
        for which, table in ((0, ropeS), (1, ropeC)):
            if which == 1:
                K.V.tensor_scalar(out=ang[R, :], in0=ang[R, :], scalar1=math.pi / 2, scalar2=None, op0=ALU.add)
            K.V.tensor_scalar(out=nfl[R, :], in0=ang[R, :], scalar1=1.0 / (2 * math.pi), scalar2=None, op0=ALU.mult)
            K.V.tensor_copy(out=n_i[R, :], in_=nfl[R, :])
            K.V.tensor_copy(out=nfl[R, :], in_=n_i[R, :])
            K.V.scalar_tensor_tensor(out=table[R, :], in0=nfl[R, :], scalar=-C1_2PI, in1=ang[R, :], op0=ALU.mult, op1=ALU.add)
            K.V.scalar_tensor_tensor(out=table[R, :], in0=nfl[R, :], scalar=-C2_2PI, in1=table[R, :], op0=ALU.mult, op1=ALU.add)
            K.V.tensor_scalar(out=table[R, :], in0=table[R, :], scalar1=-3.14159, scalar2=3.14159, op0=ALU.max, op1=ALU.min)
            K.A.activation(out=table[R, :], in_=table[R, :], func=AF.Sin)
        K.V.tensor_scalar(out=ropeS[R, :], in0=ropeS[R, :], scalar1=cst[R, 337:338], scalar2=None, op0=ALU.mult)
        dbg("ropeC", ropeC[R, :], 32, S)
        dbg("ropeS", ropeS[R, :], 32, S)
        ckpt("setup")

        if not NOALIAS0:
            A = Arena()
        xt = A.alloc(128, 8 * TT, F32, "xt")
        sq = [A.alloc(128, TT, BF16, f"sq{i}") for i in range(2)]
        xin = [A.alloc(128, D, F32, f"xin{i}") for i in range(2)]
        set_rr(range(7))
        for tt in range(NTT):
            for st in range(4):
                xi = xin[(tt * 4 + st) % 2]
                r0 = (tt * 4 + st) * 128
                K.S.dma_start(out=xi.all(), in_=x_in[r0:r0 + 128, :])
                for half in range(2):
                    bk = rr()
                    for j in range(4):
                        c = half * 4 + j
                        K.T.transpose(out=bk[:, j * 128:(j + 1) * 128], in_=xi[:, c * 128:(c + 1) * 128], identity=ident_f)
                    for j in range(4):
                        c = half * 4 + j
                        evac_copy(xt[:, c * TT + st * 128:c * TT + (st + 1) * 128], bk[:, j * 128:(j + 1) * 128])
            dbg("xt0", xt.all(), 128, 8 * TT)
            ckpt("p0a")
            K.S.dma_start(out=xs_view(tt), in_=xt.all().r("p (c t) -> p c t", c=8))
            ckpt("p0b")
            norm_to_h(xt, sq, 0, 0, tt)
            ckpt("p0c")
        dbg("h0", hT.all(), 128, 8 * S, BF16)
        ckpt("phase0")

        for l in range(depth):
            A = Arena()
            ya_in = A.alloc(128, 4 * S, BF16, "ya_in")
            gla_out = A.alloc(128, 4 * S, BF16, "gla_out")
            attn_out = A.alloc(128, 4 * S, BF16, "attn_out")
            mix0 = A.off
            PAD = 16
            ubuf = [A.alloc(128, PAD + S, F32, f"ubuf{i}") for i in range(3)]
            diff = A.alloc(128, S, BF16, "diff")
            ptmp = A.alloc(128, 16, F32, "ptmp")
            set_rr(range(7))
            for i in range(3):
                K.V.memset(ap=ubuf[i][:, 0:PAD], constant=0.0)
            wpool_in = wload(W1, l * D, 8, POOL0, 512)
            wpp = wload(WP, l * 128, 1, 0, 512)
            for g in range(4):
                w = 2 ** (g + 1)
                for tt in range(NTT):
                    bk = rr()
                    for kc in range(8):
                        K.T.matmul(out=bk.all(), lhsT=wpool_in(kc, g * 128, (g + 1) * 128), rhs=hcol(kc, tt * TT, (tt + 1) * TT),
                                   start=(kc == 0), stop=(kc == 7))
                    evac_copy(ubuf[0][:, PAD + tt * TT:PAD + (tt + 1) * TT], bk.all())
                src = 0
                for j in range(g + 1):
                    d = 2 ** j
                    dst = 1 if src != 1 else 2
                    K.V.tensor_tensor(out=ubuf[dst][:, PAD:PAD + S], in0=ubuf[src][:, PAD:PAD + S],
                                      in1=ubuf[src][:, PAD - d:PAD - d + S], op=ALU.add)
                    src = dst
                sfin = ubuf[src]
                K.V.scalar_tensor_tensor(out=diff.all(), in0=sfin[:, PAD:PAD + S], scalar=1.0 / w, in1=ubuf[0][:, PAD:PAD + S],
                                         op0=ALU.mult, op1=ALU.subtract)
                K.V.tensor_tensor(out=ptmp[:, 0:w - 1], in0=sfin[:, PAD:PAD + w - 1], in1=cst[:, 320:320 + w - 1], op=ALU.mult)
                K.V.tensor_tensor(out=diff[:, 0:w - 1], in0=ptmp[:, 0:w - 1], in1=ubuf[0][:, PAD:PAD + w - 1], op=ALU.subtract)
                for tt in range(NTT):
                    bk = rr()
                    K.T.matmul(out=bk.all(), lhsT=wpp(0, g * 128, (g + 1) * 128), rhs=diff[:, tt * TT:(tt + 1) * TT], start=True, stop=True)
                    K.V.tensor_scalar(out=ya_in[:, g * S + tt * TT:g * S + (tt + 1) * TT], in0=bk.all(), scalar1=vcol(l, 32 + g),
                                      scalar2=None, op0=ALU.mult)
            if l == 0:
                dbg("ya_in", ya_in.all(), 128, 4 * S, BF16)
            ckpt("pool")

            A.off = mix0
            qdec = A.alloc(128, 2 * S, BF16, "qdec")
            kinv = A.alloc(128, 2 * S, BF16, "kinv")
            v_tm = A.alloc(128, 16 * 512, BF16, "v_tm")
            kinv_tm = A.alloc(128, 16 * 256, BF16, "kinv_tm")
            a1T = A.alloc(16, S, F32, "a1T")
            la = A.alloc(128, 256, F32, "la")
            Eq = A.alloc(128, 2 * TT, F32, "Eq")
            Ek = A.alloc(128, 2 * TT, F32, "Ek")
            dec = A.alloc(128, 2 * 32, F32, "dec")
            attm = [A.alloc(128, 64, BF16, f"attm{i}") for i in range(2)]
            S_f = [A.alloc(128, 128, F32, f"S_f{h}") for h in range(4)]
            S_t = [A.alloc(128, 128, F32, f"S_t{h}") for h in range(4)]
            S_b = [A.alloc(128, 128, BF16, f"S_b{h}") for h in range(4)]
            osq = A.alloc(128, TT, BF16, "osq")
            otmp = A.alloc(128, TT, F32, "otmp")
            wa2_sb = A.alloc(16, 256, F32, "wa2")
            ba_sb = A.alloc(1, 256, F32, "ba")
            set_rr(range(5))
            K.S.dma_start(out=wa2_sb.all(), in_=WA2[l * 16:(l + 1) * 16, :])
            K.S.dma_start(out=ba_sb.all(), in_=BA[l:l + 1, :])
            wr = wload(W1, l * D, 8, R0, 512)
            for fc in range(4):
                for tt in range(NTT):
                    bk = rr()
                    for kc in range(8):
                        K.T.matmul(out=bk.all(), lhsT=wr(kc, fc * 128, (fc + 1) * 128), rhs=hcol(kc, tt * TT, (tt + 1) * TT),
                                   start=(kc == 0), stop=(kc == 7))
                    K.A.activation(out=gla_out[:, fc * S + tt * TT:fc * S + (tt + 1) * TT], in_=bk.all(), func=AF.Silu)
            wsm = wload(W1, l * D, 8, SM0, 80)
            for tt in range(NTT):
                bk = rr()
                for kc in range(8):
                    K.T.matmul(out=bk[0:16, :], lhsT=wsm(kc, 64, 80), rhs=hcol(kc, tt * TT, (tt + 1) * TT), start=(kc == 0), stop=(kc == 7))
                evac_copy(a1T[0:16, tt * TT:(tt + 1) * TT], bk[0:16, :])
            wv = wload(W1, l * D, 8, V0, 512)
            for st in range(16):
                bk = rr()
                for kc in range(8):
                    K.T.matmul(out=bk.all(), lhsT=hcol(kc, st * 128, (st + 1) * 128), rhs=wv(kc, 0, 512), start=(kc == 0), stop=(kc == 7))
                evac_copy(v_tm[:, st * 512:(st + 1) * 512], bk.all())
            wqk = wload(W1, l * D, 8, QK0, 512)
            for tt in range(NTT):
                for s4 in range(4):
                    st = tt * 4 + s4
                    bz = rr()
                    K.T.matmul(out=bz[:, 0:256], lhsT=a1T[0:16, st * 128:(st + 1) * 128], rhs=wa2_sb.all(), start=True, stop=False)
                    K.T.matmul(out=bz[:, 0:256], lhsT=ones_f[0:1, 0:128], rhs=ba_sb.all(), start=False, stop=True)
                    K.A.activation(out=la.all(), in_=bz[:, 0:256], func=AF.Exp, scale=-1.0)
                    K.A.activation(out=la.all(), in_=la.all(), func=AF.Ln, bias=ones_f[:, 0:1], scale=1.0)
                    bc = rr()
                    for fc in range(2):
                        K.T.matmul(out=bc[:, fc * 128:(fc + 1) * 128], lhsT=la[:, fc * 128:(fc + 1) * 128], rhs=U_f, start=True, stop=True)
                    for fc in range(2):
                        K.A.activation(out=Eq[:, fc * TT + s4 * 128:fc * TT + (s4 + 1) * 128], in_=bc[:, fc * 128:(fc + 1) * 128],
                                       func=AF.Exp, scale=-1.0 / 16.0)
                        K.A.activation(out=Ek[:, fc * TT + s4 * 128:fc * TT + (s4 + 1) * 128], in_=bc[:, fc * 128:(fc + 1) * 128],
                                       func=AF.Exp, scale=1.0 / 16.0)
                        for hf in range(2):
                            n = st * 2 + hf
                            K.V.tensor_copy(out=dec[:, fc * 32 + n:fc * 32 + n + 1],
                                            in_=Eq[:, fc * TT + s4 * 128 + hf * 64 + 63:fc * TT + s4 * 128 + hf * 64 + 64])
                for fc in range(2):
                    bq = rr()
                    for kc in range(8):
                        K.T.matmul(out=bq.all(), lhsT=wqk(kc, fc * 128, (fc + 1) * 128), rhs=hcol(kc, tt * TT, (tt + 1) * TT),
                                   start=(kc == 0), stop=(kc == 7))
                    K.V.scalar_tensor_tensor(out=qdec[:, fc * S + tt * TT:fc * S + (tt + 1) * TT], in0=bq.all(), scalar=0.125,
                                             in1=Eq[:, fc * TT:(fc + 1) * TT], op0=ALU.mult, op1=ALU.mult)
                    bk2 = rr()
                    for kc in range(8):
                        K.T.matmul(out=bk2.all(), lhsT=wqk(kc, 256 + fc * 128, 256 + (fc + 1) * 128), rhs=hcol(kc, tt * TT, (tt + 1) * TT),
                                   start=(kc == 0), stop=(kc == 7))
                    K.V.tensor_tensor(out=kinv[:, fc * S + tt * TT:fc * S + (tt + 1) * TT], in0=bk2.all(), in1=Ek[:, fc * TT:(fc + 1) * TT], op=ALU.mult)
            for st in range(16):
                for fc in range(2):
                    K.T.transpose(out=bankT[:, fc * 128:(fc + 1) * 128], in_=kinv[:, fc * S + st * 128:fc * S + (st + 1) * 128], identity=ident_b.all())
                evac_copy(kinv_tm[:, st * 256:(st + 1) * 256], bankT[:, 0:256])
            if l == 0:
                dbg("qdec", qdec.all(), 128, 2 * S, BF16)
                dbg("kinv", kinv.all(), 128, 2 * S, BF16)
                dbg("dec", dec.all(), 128, 64)
            ckpt("gla1")
            set_rr(range(3))
            attm8 = [A.alloc(128, 64, BF16, f"attm8_{i}") for i in range(8)]
            for tt in range(NTT):
                for c8 in range(8):
                    n = tt * 8 + c8
                    st, hf = n // 2, n % 2
                    t0 = n * 64
                    H = slice(hf * 64, hf * 64 + 64)
                    for h in range(4):
                        fc, r0 = h // 2, (h % 2) * 64
                        P = slice(r0, r0 + 64)
                        bo = banks[3 + h]
                        qv = qdec[P, fc * S + t0:fc * S + t0 + 64]
                        ba_ = rr()
                        K.T.matmul(out=ba_[0:64, 0:64], lhsT=kinv[P, fc * S + t0:fc * S + t0 + 64], rhs=qv, start=True, stop=True)
                        am = attm8[h * 2 + n % 2]
                        K.V.tensor_tensor(out=am[H, :], in0=ba_[0:64, 0:64], in1=cst[0:64, 256:320], op=ALU.mult)
                        oc = bo[:, c8 * 64:(c8 + 1) * 64]
                        if n > 0:
                            K.T.matmul(out=oc, lhsT=S_b[h][P, :], rhs=qv, start=True, stop=False)
                        K.T.matmul(out=oc, lhsT=v_tm[H, st * 512 + h * 128:st * 512 + (h + 1) * 128], rhs=am[H, :], start=(n == 0), stop=True)
                        if n < 31:
                            bkv = rr()
                            K.T.matmul(out=bkv[0:64, 0:128], lhsT=kinv_tm[H, st * 256 + h * 64:st * 256 + (h + 1) * 64],
                                       rhs=v_tm[H, st * 512 + h * 128:st * 512 + (h + 1) * 128], start=True, stop=True)
                            dcol = dec[P, fc * 32 + n:fc * 32 + n + 1]
                            if n == 0:
                                K.V.tensor_copy(out=S_t[h][P, :], in_=bkv[0:64, 0:128])
                            else:
                                K.V.tensor_tensor(out=S_t[h][P, :], in0=bkv[0:64, 0:128], in1=S_f[h][P, :], op=ALU.add)
                            K.A.activation(out=S_b[h][P, :], in_=S_t[h][P, :], func=AF.Copy, scale=dcol)
                            K.V.tensor_scalar(out=S_f[h][P, :], in0=S_t[h][P, :], scalar1=dcol, scalar2=None, op0=ALU.mult)
                for h in range(4):
                    bo = banks[3 + h]
                    K.A.activation(out=osq.all(), in_=bo.all(), func=AF.Square)
                    bs = rr()
                    K.T.matmul(out=bs.all(), lhsT=ones_b.all(), rhs=osq.all(), start=True, stop=True)
                    rstd_from(bs.all(), 128)
                    K.V.tensor_tensor(out=otmp.all(), in0=bo.all(), in1=rs_sb.all(), op=ALU.mult)
                    go = gla_out[:, h * S + tt * TT:h * S + (tt + 1) * TT]
                    K.V.scalar_tensor_tensor(out=go, in0=otmp.all(), scalar=vcol(l, 36 + h), in1=go, op0=ALU.mult, op1=ALU.mult)
            if l == 0:
                dbg("gla_out", gla_out.all(), 128, 4 * S, BF16)
            ckpt("gla")

            A.off = mix0
            cqn = A.alloc(128, 3 * S, BF16, "cqn")
            ckvn = A.alloc(128, 2 * S, BF16, "ckvn")
            krope = A.alloc(128, S, BF16, "krope")
            Qh = [A.alloc(128, S, BF16, f"Qh{i}") for i in range(2)]
            Kh = [A.alloc(128, S, BF16, f"Kh{i}") for i in range(2)]
            Vh = [A.alloc(128, 16 * 128, BF16, f"Vh{i}") for i in range(2)]
            pt = [A.alloc(128, TT, BF16, f"pt{i}") for i in range(3)]
            sqh = A.alloc(128, S, BF16, "sqh")
            rt1 = A.alloc(128, TT, F32, "rt1")
            rt2 = A.alloc(128, TT, F32, "rt2")
            rden = A.alloc(128, TT, F32, "rden")
            nrow = A.alloc(1, S, F32, "nrow")
            kmx = A.alloc(1, 8, F32, "kmx")
            negm = [A.alloc(1, S, BF16, f"negm{i}") for i in range(2)]
            set_rr(range(5))
            for i in range(2):
                K.V.memset(ap=Vh[i].all().r("p (s e) -> p s e", e=128).sub(lambda ap: ap[:, :, 64:128]), constant=1.0)
            for (col0, nch, dstb, gb, nfeat) in ((CQ0, 3, cqn, 40, 384), (CKV0, 2, ckvn, 43, 256)):
                wc = wload(W1, l * D, 8, col0, nch * 128)
                for tt in range(NTT):
                    pb = [rr() for _ in range(nch)]
                    for c in range(nch):
                        for kc in range(8):
                            K.T.matmul(out=pb[c].all(), lhsT=wc(kc, c * 128, (c + 1) * 128), rhs=hcol(kc, tt * TT, (tt + 1) * TT),
                                       start=(kc == 0), stop=(kc == 7))
                    bs = rr()
                    for c in range(nch):
                        mq = pt[c]
                        K.A.activation(out=mq.all(), in_=pb[c].all(), func=AF.Square)
                        K.T.matmul(out=bs.all(), lhsT=ones_b.all(), rhs=mq.all(), start=(c == 0), stop=(c == nch - 1))
                    rstd_from(bs.all(), nfeat)
                    for c in range(nch):
                        K.V.scalar_tensor_tensor(out=dstb[:, c * S + tt * TT:c * S + (tt + 1) * TT], in0=pb[c].all(), scalar=vcol(l, gb + c),
                                                 in1=rs_sb.all(), op0=ALU.mult, op1=ALU.mult)
            wsm = wload(W1, l * D, 8, SM0, 80)
            R = slice(64, 96)
            for tt in range(NTT):
                bk = rr()
                for kc in range(8):
                    K.T.matmul(out=bk[0:64, :], lhsT=wsm(kc, 0, 64), rhs=hcol(kc, tt * TT, (tt + 1) * TT), start=(kc == 0), stop=(kc == 7))
                K.V.tensor_tensor(out=rt1[R, :], in0=bk[0:32, :], in1=ropeC[R, tt * TT:(tt + 1) * TT], op=ALU.mult)
                K.V.tensor_tensor(out=rt2[R, :], in0=bk[32:64, :], in1=ropeS[R, tt * TT:(tt + 1) * TT], op=ALU.mult)
                K.V.tensor_tensor(out=krope[R, tt * TT:(tt + 1) * TT], in0=rt1[R, :], in1=rt2[R, :], op=ALU.add)
            wuq = wload(WUQ, l * 384, 3, 0, 768)
            wuqs = wload(WUQS, l * 384, 3, 0, 768)
            wukv = wload(WUKV, l * 256, 2, 0, 1024)
            SCALE = 96.0 ** -0.5
            def mla_prep(h):
                Q, Kt, V = Qh[h % 2], Kh[h % 2], Vh[h % 2]
                for tt in range(NTT):
                    T = slice(tt * TT, (tt + 1) * TT)
                    bq, bqs, bkk = rr(), rr(), rr()
                    for c in range(3):
                        K.T.matmul(out=bq[0:96, :], lhsT=wuq(c, h * 96, (h + 1) * 96), rhs=cqn[:, c * S + tt * TT:c * S + (tt + 1) * TT],
                                   start=(c == 0), stop=(c == 2))
                    for c in range(3):
                        K.T.matmul(out=bqs[0:96, :], lhsT=wuqs(c, h * 96, (h + 1) * 96), rhs=cqn[:, c * S + tt * TT:c * S + (tt + 1) * TT],
                                   start=(c == 0), stop=(c == 2))
                    for c in range(2):
                        K.T.matmul(out=bkk[0:64, :], lhsT=wukv(c, h * 64, (h + 1) * 64), rhs=ckvn[:, c * S + tt * TT:c * S + (tt + 1) * TT],
                                   start=(c == 0), stop=(c == 1))
                    K.A.copy(out=Q[0:64, T], in_=bq[0:64, :])
                    K.V.tensor_tensor(out=rt1[R, :], in0=bq[R, :], in1=ropeC[R, T], op=ALU.mult)
                    K.V.tensor_tensor(out=rt2[R, :], in0=bqs[R, :], in1=ropeS[R, T], op=ALU.mult)
                    K.V.tensor_tensor(out=Q[R, T], in0=rt1[R, :], in1=rt2[R, :], op=ALU.add)
                    K.A.copy(out=Kt[0:64, T], in_=bkk[0:64, :])
                    K.V.tensor_copy(out=Kt[R, T], in_=krope[R, T])
                for st in range(16):
                    bv = rr()
                    for c in range(2):
                        K.T.matmul(out=bv[:, 0:64], lhsT=ckvn[:, c * S + st * 128:c * S + (st + 1) * 128], rhs=wukv(c, 512 + h * 64, 512 + (h + 1) * 64),
                                   start=(c == 0), stop=(c == 1))
                    evac_copy(V[:, st * 128:st * 128 + 64], bv[:, 0:64])
                K.A.activation(out=sqh[0:96, :], in_=Kt[0:96, :], func=AF.Square)
                for tt in range(NTT):
                    bn = rr()
                    K.T.matmul(out=bn[0:1, :], lhsT=ones_b[0:96, 0:1], rhs=sqh[0:96, tt * TT:(tt + 1) * TT], start=True, stop=True)
                    K.V.tensor_reduce(out=kmx[0:1, tt:tt + 1], in_=bn[0:1, :], axis=AX.X, op=ALU.max)
                K.V.tensor_reduce(out=kmx[0:1, 4:5], in_=kmx[0:1, 0:4], axis=AX.X, op=ALU.max)
                K.A.activation(out=sqh[0:96, :], in_=Q[0:96, :], func=AF.Square)
                for tt in range(NTT):
                    bn = rr()
                    K.T.matmul(out=bn[0:1, :], lhsT=ones_b[0:96, 0:1], rhs=sqh[0:96, tt * TT:(tt + 1) * TT], start=True, stop=True)
                    K.V.tensor_scalar(out=nrow[0:1, tt * TT:(tt + 1) * TT], in0=bn[0:1, :], scalar1=kmx[0:1, 4:5], scalar2=None, op0=ALU.mult)
                K.A.activation(out=nrow.all(), in_=nrow.all(), func=AF.Sqrt)
                K.V.tensor_scalar(out=negm[h % 2].all(), in0=nrow.all(), scalar1=-1.0, scalar2=None, op0=ALU.mult)

            pt_i = [0]

            def mla_attend(h):
                Q, Kt, V = Qh[h % 2], Kh[h % 2], Vh[h % 2]
                iters = [(qb, kt) for qb in range(4) for kt in range(4 * qb + 4)]
                sbank = {}
                LOOK = 2

                def score(i):
                    qb, kt = iters[i]
                    r = kt - 4 * qb
                    c0 = max(r, 0) * 128
                    q0 = qb * TT + c0
                    bs_ = rr()
                    sbank[i] = bs_
                    K.T.matmul(out=bs_[:, c0:TT], lhsT=Kt[0:96, kt * 128:(kt + 1) * 128], rhs=Q[0:96, q0:(qb + 1) * TT], start=True, stop=False)
                    K.T.matmul(out=bs_[:, c0:TT], lhsT=ones_b[0:1, 0:128], rhs=negm[h % 2][0:1, q0:(qb + 1) * TT], start=False, stop=(r < 0))
                    if r >= 0:
                        K.T.matmul(out=bs_[:, c0:TT], lhsT=ident_b.all(), rhs=maskneg_b[:, 0:TT - c0], start=False, stop=True)

                def exp_pv(i):
                    qb, kt = iters[i]
                    nk = 4 * qb + 4
                    r = kt - 4 * qb
                    c0 = max(r, 0) * 128
                    bo = banks[5 + (h * 4 + qb) % 2]
                    bs_ = sbank.pop(i)
                    p = pt[pt_i[0] % 3]
                    pt_i[0] += 1
                    K.A.activation(out=p[:, c0:TT], in_=bs_[:, c0:TT], func=AF.Exp, scale=SCALE)
                    K.T.matmul(out=bo[:, c0:TT], lhsT=V[:, kt * 128:(kt + 1) * 128], rhs=p[:, c0:TT], start=(kt == 0), stop=(kt == nk - 1))
                    if kt == nk - 1:
                        K.V.reciprocal(out=rden[64:128, :], in_=bo[64:128, :])
                        rr0 = (h % 2) * 64
                        K.V.tensor_tensor(out=attn_out[rr0:rr0 + 64, (h // 2) * S + qb * TT:(h // 2) * S + (qb + 1) * TT], in0=bo[0:64, :],
                                          in1=rden[64:128, :], op=ALU.mult)

                for i in range(len(iters) + LOOK):
                    if i < len(iters):
                        score(i)
                    if i - LOOK >= 0:
                        exp_pv(i - LOOK)

            mla_prep(0)
            for h in range(8):
                if h + 1 < 8:
                    mla_prep(h + 1)
                mla_attend(h)
            if l == 0:
                dbg("attn_out", attn_out.all(), 128, 4 * S, BF16)
            ckpt("mla")

            A.off = mix0
            merged = A.alloc(128, 8 * S, BF16, "merged")
            sig = [A.alloc(128, TT, F32, f"sig{i}") for i in range(3)]
            acc = [A.alloc(128, TT, F32, f"acc{i}") for i in range(2)]
            yins = (ya_in, gla_out, attn_out)
            set_rr(range(7))
            for m in range(8):
                wg = wload(W1, l * D, 8, G0 + m * 384, 384)
                wy = wload(WABC, l * 1536, 12, m * 128, 128)
                for tt in range(NTT):
                    ac = acc[(m * NTT + tt) % 2]
                    for b in range(3):
                        bg, by = rr(), rr()
                        for kc in range(8):
                            K.T.matmul(out=bg.all(), lhsT=wg(kc, b * 128, (b + 1) * 128), rhs=hcol(kc, tt * TT, (tt + 1) * TT),
                                       start=(kc == 0), stop=(kc == 7))
                        for c in range(4):
                            K.T.matmul(out=by.all(), lhsT=wy(b * 4 + c, 0, 128), rhs=yins[b][:, c * S + tt * TT:c * S + (tt + 1) * TT],
                                       start=(c == 0), stop=(c == 3))
                        K.A.activation(out=sig[b].all(), in_=bg.all(), func=AF.Sigmoid)
                        if b == 0:
                            K.V.tensor_tensor(out=ac.all(), in0=by.all(), in1=sig[b].all(), op=ALU.mult)
                        else:
                            K.V.tensor_tensor(out=sig[b].all(), in0=by.all(), in1=sig[b].all(), op=ALU.mult)
                            if b == 1:
                                K.V.tensor_tensor(out=ac.all(), in0=ac.all(), in1=sig[b].all(), op=ALU.add)
                            else:
                                K.V.tensor_tensor(out=merged[:, m * S + tt * TT:m * S + (tt + 1) * TT], in0=ac.all(), in1=sig[b].all(), op=ALU.add)
            if l == 0:
                dbg("merged", merged.all(), 128, 8 * S, BF16)
            ckpt("merge")

            xt = A.alloc(128, 8 * TT, F32, "xt")
            tt_ = A.alloc(128, 8 * TT, F32, "ttile")
            sq = [A.alloc(128, TT, BF16, f"sq{i}") for i in range(2)]
            wo0 = wload(WO, l * D, 8, 0, 512)
            wo1 = wload(WO, l * D, 8, 512, 512)
            for tt in range(NTT):
                K.S.dma_start(out=xt.all().r("p (c t) -> p c t", c=8), in_=xs_view(tt))
                bsum = banks[6]
                set_rr(range(6))
                for m in range(8):
                    wo_ = wo0 if m < 4 else wo1
                    bk = rr()
                    for kc in range(8):
                        K.T.matmul(out=bk.all(), lhsT=wo_(kc, (m % 4) * 128, (m % 4 + 1) * 128), rhs=merged[:, kc * S + tt * TT:kc * S + (tt + 1) * TT],
                                   start=(kc == 0), stop=(kc == 7))
                    K.V.tensor_copy(out=tt_[:, m * TT:(m + 1) * TT], in_=bk.all())
                    K.A.activation(out=sq[m % 2].all(), in_=bk.all(), func=AF.Square)
                    if m > 0:
                        K.T.matmul(out=bsum.all(), lhsT=ones_b.all(), rhs=sq[(m - 1) % 2].all(), start=(m == 1), stop=False)
                K.T.matmul(out=bsum.all(), lhsT=ones_b.all(), rhs=sq[7 % 2].all(), start=False, stop=True)
                rstd_from(bsum.all(), D)
                for m in range(8):
                    K.V.scalar_tensor_tensor(out=tt_[:, m * TT:(m + 1) * TT], in0=tt_[:, m * TT:(m + 1) * TT], scalar=vcol(l, 8 + m),
                                             in1=rs_sb.all(), op0=ALU.mult, op1=ALU.mult)
                    K.V.tensor_tensor(out=xt[:, m * TT:(m + 1) * TT], in0=xt[:, m * TT:(m + 1) * TT], in1=tt_[:, m * TT:(m + 1) * TT], op=ALU.add)
                K.S.dma_start(out=xs_view(tt), in_=xt.all().r("p (c t) -> p c t", c=8))
                norm_to_h(xt, sq, l, 16, tt)
            if l == 0:
                dbg("h2", hT.all(), 128, 8 * S, BF16)
            ckpt("wo")

            A = Arena()
            act = A.alloc(128, 22 * S, BF16, "act")
            xt = A.alloc(128, 8 * TT, F32, "xt")
            ft = A.alloc(128, 8 * TT, F32, "ftile")
            sq = [A.alloc(128, TT, BF16, f"sq{i}") for i in range(2)]
            sg = [A.alloc(128, TT, F32, f"sg{i}") for i in range(2)]
            otile = [K.sbuf(128, D, F32, f"otile{i}", off=ft.off + i * D * 4) for i in range(2)]
            set_rr(range(7))
            for j in range(11):
                wgu = wload(WGU, l * D, 8, j * 512, 512)
                for pp in range(2):
                    jj = 2 * j + pp
                    for tt in range(NTT):
                        bg, bu = rr(), rr()
                        for kc in range(8):
                            K.T.matmul(out=bg.all(), lhsT=wgu(kc, pp * 256, pp * 256 + 128), rhs=hcol(kc, tt * TT, (tt + 1) * TT),
                                       start=(kc == 0), stop=(kc == 7))
                        for kc in range(8):
                            K.T.matmul(out=bu.all(), lhsT=wgu(kc, pp * 256 + 128, pp * 256 + 256), rhs=hcol(kc, tt * TT, (tt + 1) * TT),
                                       start=(kc == 0), stop=(kc == 7))
                        s_ = sg[(jj * NTT + tt) % 2]
                        K.A.activation(out=s_.all(), in_=bg.all(), func=AF.Silu)
                        K.V.tensor_tensor(out=act[:, jj * S + tt * TT:jj * S + (tt + 1) * TT], in0=bu.all(), in1=s_.all(), op=ALU.mult)
            for tt in range(NTT):
                K.S.dma_start(out=xt.all().r("p (c t) -> p c t", c=8), in_=xs_view(tt))
                bsum = banks[6]
                set_rr(range(6))
                for m in range(8):
                    wd = wload(WD, l * DFF, 22, m * 128, 128)
                    bk = rr()
                    for kc in range(22):
                        K.T.matmul(out=bk.all(), lhsT=wd(kc, 0, 128), rhs=act[:, kc * S + tt * TT:kc * S + (tt + 1) * TT], start=(kc == 0), stop=(kc == 21))
                    K.V.tensor_copy(out=ft[:, m * TT:(m + 1) * TT], in_=bk.all())
                    K.A.activation(out=sq[m % 2].all(), in_=bk.all(), func=AF.Square)
                    if m > 0:
                        K.T.matmul(out=bsum.all(), lhsT=ones_b.all(), rhs=sq[(m - 1) % 2].all(), start=(m == 1), stop=False)
                K.T.matmul(out=bsum.all(), lhsT=ones_b.all(), rhs=sq[7 % 2].all(), start=False, stop=True)
                rstd_from(bsum.all(), D)
                for m in range(8):
                    K.V.scalar_tensor_tensor(out=ft[:, m * TT:(m + 1) * TT], in0=ft[:, m * TT:(m + 1) * TT], scalar=vcol(l, 24 + m),
                                             in1=rs_sb.all(), op0=ALU.mult, op1=ALU.mult)
                    K.V.tensor_tensor(out=xt[:, m * TT:(m + 1) * TT], in0=xt[:, m * TT:(m + 1) * TT], in1=ft[:, m * TT:(m + 1) * TT], op=ALU.add)
                if l < depth - 1:
                    K.S.dma_start(out=xs_view(tt), in_=xt.all().r("p (c t) -> p c t", c=8))
                    norm_to_h(xt, sq, l + 1, 0, tt)
                else:
                    for s4 in range(4):
                        ot = otile[s4 % 2]
                        for half in range(2):
                            bk = rr()
                            for j in range(4):
                                c = half * 4 + j
                                K.T.transpose(out=bk[:, j * 128:(j + 1) * 128], in_=xt[:, c * TT + s4 * 128:c * TT + (s4 + 1) * 128], identity=ident_f)
                            evac_copy(ot[:, half * 512:(half + 1) * 512], bk.all())
                        r0 = (tt * 4 + s4) * 128
                        i = K.S.dma_start(out=dram_view(out_d[r0:r0 + 128, :], "out", r0, r0 + 128), in_=ot.all())
                        out_toks.append(i.tok)

    except _Stop:
        pass
    K.wait_all("sync", out_toks + list(dbg_out.values()))
    K.emit()
    K.close()
    return nc


def host_prep(inputs):
    f = np.float32
    w_in = np.asarray(inputs["w_in"], f)
    Ld = w_in.shape[0]
    O_POOL, O_Q, O_K, O_V, O_R, O_A1, O_CQ, O_CKV, O_KR, O_G = 0, 512, 768, 1024, 1536, 2048, 2064, 2448, 2704, 2736
    idx = []
    idx += list(range(O_POOL, O_POOL + 512))
    idx += list(range(O_Q, O_Q + 256)) + list(range(O_K, O_K + 256))
    idx += list(range(O_R, O_R + 512))
    idx += list(range(O_V, O_V + 512))
    idx += list(range(O_CQ, O_CQ + 384))
    idx += list(range(O_CKV, O_CKV + 256))
    idx += list(range(O_KR, O_KR + 32))
    idx += list(range(O_KR + 16, O_KR + 32)) + list(range(O_KR, O_KR + 16))
    idx += list(range(O_A1, O_A1 + 16))
    for m in range(8):
        for b in range(3):
            idx += list(range(O_G + b * 1024 + m * 128, O_G + b * 1024 + (m + 1) * 128))
    idx = np.asarray(idx)
    assert idx.shape[0] == W1C
    w1 = np.ascontiguousarray(w_in[:, :, idx]).reshape(Ld * D, W1C)
    wp = np.ascontiguousarray(np.asarray(inputs["w_pool"], f).transpose(0, 2, 1, 3)).reshape(Ld * 128, 512)
    wabc = np.concatenate([np.asarray(inputs["w_a"], f), np.asarray(inputs["w_b"], f), np.asarray(inputs["w_c"], f)], axis=1).reshape(Ld * 1536, D)
    wa2 = np.ascontiguousarray(np.asarray(inputs["w_gla_a2"], f)).reshape(Ld * 16, 256)
    ba = np.ascontiguousarray(np.asarray(inputs["b_gla_a"], f)).reshape(Ld, 256)
    w_uq = np.asarray(inputs["w_mla_uq"], f)
    sw = []
    for h in range(8):
        sw += list(range(h * 96, h * 96 + 64)) + list(range(h * 96 + 80, h * 96 + 96)) + list(range(h * 96 + 64, h * 96 + 80))
    wuq = np.ascontiguousarray(w_uq).reshape(Ld * 384, 768)
    wuqs = np.ascontiguousarray(w_uq[:, :, np.asarray(sw)]).reshape(Ld * 384, 768)
    pk = []
    for h in range(8):
        pk += list(range(h * 128, h * 128 + 64))
    for h in range(8):
        pk += list(range(h * 128 + 64, h * 128 + 128))
    wukv = np.ascontiguousarray(np.asarray(inputs["w_mla_ukv"], f)[:, :, np.asarray(pk)]).reshape(Ld * 256, 1024)
    wo = np.ascontiguousarray(np.asarray(inputs["w_o"], f)).reshape(Ld * D, D)
    pg = []
    for j in range(22):
        pg += list(range(j * 128, (j + 1) * 128)) + list(range(DFF + j * 128, DFF + (j + 1) * 128))
    wgu = np.ascontiguousarray(np.asarray(inputs["w_ffn_gu"], f)[:, :, np.asarray(pg)]).reshape(Ld * D, 2 * DFF)
    wd = np.ascontiguousarray(np.asarray(inputs["w_ffn_down"], f)).reshape(Ld * DFF, D)
    vecs = np.zeros((128, Ld * NV), f)
    for l in range(Ld):
        cols = []
        for name, n in (("norm_pre_mix", 8), ("norm_post_mix", 8), ("norm_pre_ffn", 8), ("norm_post_ffn", 8), ("pool_scale", 4),
                        ("gla_norm", 4), ("mla_q_norm", 3), ("mla_kv_norm", 2)):
            v = np.asarray(inputs[name], f)[l]
            cols.append(v.reshape(n, 128).T)
        vecs[:, l * NV:(l + 1) * NV] = np.concatenate(cols, axis=1)
    cst = np.zeros((128, NCST), f)
    cst[:, 0:128] = np.eye(128, dtype=f)
    s = np.arange(128)
    cst[:, 128:256] = ((s[:, None] // 64 == s[None, :] // 64) & (s[:, None] <= s[None, :])).astype(f)
    cst[:, 256:320] = ((s[:, None] % 64) <= np.arange(64)[None, :]).astype(f)
    cst[:, 320:336] = (1.0 / (np.arange(16) + 1.0)).astype(f)[None, :]
    invf = (10000.0 ** (-np.arange(0, 32, 2, dtype=np.float32) / 32.0)).astype(f)
    cst[64:80, 336] = invf
    cst[80:96, 336] = invf
    cst[64:80, 337] = -1.0
    cst[80:96, 337] = 1.0
    q = np.arange(128)
    cst[:, 338:338 + 128] = np.where(q[None, :] < s[:, None], -30000.0, 0.0).astype(f)
    shared = dict(cst=cst, vecs=vecs, w1=w1, wp=wp, wabc=wabc, wa2=wa2, ba=ba, wuq=wuq, wuqs=wuqs, wukv=wukv, wo=wo, wgu=wgu, wd=wd)
    return shared


def kernel(**inputs):
    x = np.asarray(inputs["x"], np.float32)
    pos = np.asarray(inputs["positions"], np.int32)
    B = x.shape[0]
    shared = host_prep(inputs)
    nc = build_program(L_FULL)
    in_maps = []
    for b in range(B):
        m = dict(shared)
        m["x"] = np.ascontiguousarray(x[b])
        m["pos"] = np.ascontiguousarray(np.broadcast_to(pos[b][None, :], (128, S)))
        in_maps.append(m)
    res = run_bass_kernel_spmd(nc, in_maps, core_ids=list(range(B)))
    return np.stack([np.asarray(r["out"], np.float32) for r in res.results], axis=0)
```

```python
import numpy as np
from contextlib import ExitStack
import concourse.bass as bass
import concourse.mybir as mybir

F32 = mybir.dt.float32
BF16 = mybir.dt.bfloat16
I32 = mybir.dt.int32
AF = mybir.ActivationFunctionType
ALU = mybir.AluOpType
AX = mybir.AxisListType
ESZ = {F32: 4, BF16: 2, I32: 4}

SAME_ENGINE_SYNC = True
SEM_EPOCH = 30000
N_DMA_SEMS = 8


class View:
    def __init__(self, ap, space, p0, p1, b0, b1):
        self.ap, self.space, self.p0, self.p1, self.b0, self.b1 = ap, space, p0, p1, b0, b1

    def r(self, pattern, **kw):
        return View(self.ap.rearrange(pattern, **kw), self.space, self.p0, self.p1, self.b0, self.b1)

    def bc(self, shape):
        return View(self.ap.to_broadcast(shape), self.space, self.p0, self.p1, self.b0, self.b1)

    def bitcast(self, dt):
        return View(self.ap.bitcast(dt), self.space, self.p0, self.p1, self.b0, self.b1)

    def sub(self, fn):
        return View(fn(self.ap), self.space, self.p0, self.p1, self.b0, self.b1)


class Buf:
    def __init__(self, handle, space, P, F, dtype, off):
        self.t, self.space, self.P, self.F, self.dtype, self.off = handle, space, P, F, dtype, off
        self.esz = ESZ[dtype]

    def __getitem__(self, key):
        ps, cs = key
        p0, p1, _ = ps.indices(self.P)
        c0, c1, _ = cs.indices(self.F)
        return View(self.t[p0:p1, c0:c1], self.space, p0, p1,
                    self.off + c0 * self.esz, self.off + c1 * self.esz)

    def all(self):
        return self[:, :]


class DView(View):
    pass


def dram_view(ap, name, lo, hi):
    return View(ap, "d:" + name, 0, 1, lo, hi)


class Ins:
    __slots__ = ("eng", "fn", "kw", "waits", "tok", "inc", "is_dma")


class EngProxy:
    def __init__(self, K, eng):
        self.K, self.eng = K, eng

    def __getattr__(self, fn):
        def call(**kw):
            return self.K._record(self.eng, fn, kw)
        return call


OUT_KEYS = ("out", "accum_out", "ap")


class Kern:
    def __init__(self, nc):
        self.nc = nc
        self.es = ExitStack()
        self.sb_off = 229376 - nc.sbuf_bytes_remaining
        self.sb_end = 229376
        self.streams = {e: [] for e in ("tensor", "vector", "scalar", "gpsimd", "sync")}
        self.sems = {}
        self.cnt = {e: 0 for e in self.streams}
        self.dma_sems = {}
        self.dma_rr = {e: 0 for e in self.streams}
        self.seen = {e: {} for e in self.streams}
        self.recs = {}
        self.T = EngProxy(self, "tensor")
        self.V = EngProxy(self, "vector")
        self.A = EngProxy(self, "scalar")
        self.G = EngProxy(self, "gpsimd")
        self.S = EngProxy(self, "sync")
        self.nsem = 0
        self.nbuf = 0

    def sem(self, name):
        self.nsem += 1
        return self.es.enter_context(self.nc.semaphore(name))

    def sbuf(self, P, F, dtype, name=None, off=None):
        esz = ESZ[dtype]
        nbytes = F * esz
        if off is None:
            off = (self.sb_off + 63) // 64 * 64
            self.sb_off = off + nbytes
            assert self.sb_off <= self.sb_end, (name, self.sb_off)
        self.nbuf += 1
        name = f"{name or 'sb'}_{self.nbuf}"
        h = self.nc.alloc_sbuf_tensor_at(name, [P, F], dtype, offset=off)
        return Buf(h, "sb", P, F, dtype, off)

    def psum_banks(self, n=8):
        banks = []
        for i in range(n):
            h = self.es.enter_context(self.nc.psum_tensor(f"psb{i}", [128, 512], F32))
            banks.append(Buf(h, "ps", 128, 512, F32, i * 2048))
        return banks

    def _tok_compute(self, eng):
        self.cnt[eng] += 1
        c = self.cnt[eng]
        ep = (c - 1) // SEM_EPOCH
        lst = self.sems.setdefault(eng, [])
        while len(lst) <= ep:
            lst.append(self.sem(f"s_{eng}_{len(lst)}"))
        return (lst[ep], c - ep * SEM_EPOCH)

    def _record(self, eng, fn, kw):
        ins = Ins()
        ins.eng, ins.fn, ins.kw, ins.is_dma = eng, fn, kw, fn.startswith("dma_start")
        reads, writes = [], []
        for k, v in list(kw.items()):
            if isinstance(v, View):
                if v.space == "ps":
                    bb = (v.b0 // 2048) * 2048
                    v = View(v.ap, "ps", (v.p0 // 32) * 32, ((v.p1 + 31) // 32) * 32, bb, bb + 2048)
                    kw[k] = v
                (writes if k in OUT_KEYS else reads).append(v)
        deps = {}

        def add_dep(tok):
            s, val = tok
            if deps.get(id(s), (None, 0))[1] < val:
                deps[id(s)] = (s, val)

        for v, is_w in [(x, False) for x in reads] + [(x, True) for x in writes]:
            lst = self.recs.setdefault(v.space, [])
            for r in lst:
                if r[0] < v.p1 and v.p0 < r[1] and r[2] < v.b1 and v.b0 < r[3] and (is_w or r[4] or (v.space == "ps" and r[5] != eng)):
                    if r[5] == eng and not r[6] and not ins.is_dma:
                        if eng == "tensor" or not SAME_ENGINE_SYNC:
                            continue
                    add_dep(r[7])
        if ins.is_dma:
            pool = self.dma_sems.setdefault(eng, [])
            if len(pool) < N_DMA_SEMS:
                pool.append([self.sem(f"d_{eng}_{len(pool)}"), 0])
            i = self.dma_rr[eng] % len(pool) if len(pool) == N_DMA_SEMS else len(pool) - 1
            self.dma_rr[eng] += 1
            ent = pool[i]
            if ent[1] > 0:
                add_dep((ent[0], ent[1]))
            ent[1] += 16
            ins.tok, ins.inc = (ent[0], ent[1]), 16
        else:
            ins.tok, ins.inc = self._tok_compute(eng), 1
        seen = self.seen[eng]
        ins.waits = []
        for s, val in deps.values():
            if seen.get(id(s), 0) < val:
                seen[id(s)] = val
                ins.waits.append((s, val))
        for v, is_w in [(x, False) for x in reads] + [(x, True) for x in writes]:
            lst = self.recs[v.space]
            new = []
            for r in lst:
                covered = v.p0 <= r[0] and r[1] <= v.p1 and v.b0 <= r[2] and r[3] <= v.b1
                if covered and (is_w or (not r[4] and ((r[5] == eng and not r[6] and not ins.is_dma) or v.space == "ps"))):
                    continue
                new.append(r)
            new.append([v.p0, v.p1, v.b0, v.b1, is_w, eng, ins.is_dma, ins.tok])
            self.recs[v.space] = new
        self.streams[eng].append(ins)
        return ins

    def wait_all(self, eng, toks):
        ins = Ins()
        ins.eng, ins.fn, ins.kw, ins.is_dma = eng, None, {}, False
        ins.waits = list(toks)
        ins.tok, ins.inc = None, 0
        self.streams[eng].append(ins)

    def emit(self):
        nc = self.nc
        block = self.es.enter_context(nc.Block())

        def run(engname):
            def body(e):
                for ins in self.streams[engname]:
                    for s, val in ins.waits:
                        e.wait_ge(s, val)
                    if ins.fn is None:
                        continue
                    kw = {k: (v.ap if isinstance(v, View) else v) for k, v in ins.kw.items()}
                    r = getattr(e, ins.fn)(**kw)
                    r.then_inc(ins.tok[0], ins.inc)
            return body

        block.tensor(run("tensor"))
        block.vector(run("vector"))
        block.scalar(run("scalar"))
        block.gpsimd(run("gpsimd"))
        block.sync(run("sync"))

    def close(self):
        self.es.close()


import math
from concourse.bass_utils import run_bass_kernel_spmd

D = 1024
S = 2048
L_FULL = 4
DFF = 2816
NV = 45
NCST = 850
EPS = 1e-6
TT = 512
NTT = S // TT
W1C = 5840
POOL0, QK0, R0, V0, CQ0, CKV0, SM0, G0 = 0, 512, 1024, 1536, 2048, 2432, 2688, 2768
WSLOT = 4096
NSLOT = 3
C1_2PI = 6.28125
C2_2PI = 2.0 * math.pi - 6.28125
import os
NOALIAS0 = bool(int(os.environ.get('NOALIAS0', '0')))


class _Stop(Exception):
    pass


def build_program(depth=L_FULL, debug=(), stop=None):
    nc = bass.Bass("TRN2", target_bir_lowering=False)
    K = Kern(nc)

    def din(name, shape, dt=F32):
        return nc.dram_tensor(name, shape, dt, kind="ExternalInput").ap()

    x_in = din("x", [S, D])
    pos_in = din("pos", [128, S], I32)
    cst_in = din("cst", [128, NCST])
    vecs_in = din("vecs", [128, L_FULL * NV])
    W1 = din("w1", [L_FULL * D, W1C])
    WP = din("wp", [L_FULL * 128, 512])
    WABC = din("wabc", [L_FULL * 1536, D])
    WA2 = din("wa2", [L_FULL * 16, 256])
    BA = din("ba", [L_FULL, 256])
    WUQ = din("wuq", [L_FULL * 384, 768])
    WUQS = din("wuqs", [L_FULL * 384, 768])
    WUKV = din("wukv", [L_FULL * 256, 1024])
    WO = din("wo", [L_FULL * D, D])
    WGU = din("wgu", [L_FULL * D, 2 * DFF])
    WD = din("wd", [L_FULL * DFF, D])
    out_d = nc.dram_tensor("out", [S, D], F32, kind="ExternalOutput").ap()
    xs_d = nc.dram_tensor("xs", [128, 8 * S], F32, kind="Internal").ap()
    dbg_out = {}

    banks = []
    for i in range(7):
        h = K.es.enter_context(nc.psum_tensor(f"psb{i}", [128, 512], F32))
        banks.append(Buf(h, "ps", 128, 512, F32, i * 2048))
    hb = K.es.enter_context(nc.psum_tensor("psb7", [128, 1024], BF16))
    bankT = Buf(hb, "ps", 128, 1024, BF16, 7 * 2048)
    rr_state = {"list": list(range(7)), "i": 0}

    def set_rr(lst):
        rr_state["list"], rr_state["i"] = list(lst), 0

    def rr():
        b = banks[rr_state["list"][rr_state["i"] % len(rr_state["list"])]]
        rr_state["i"] += 1
        return b

    cst = K.sbuf(128, NCST, F32, "cst")
    vecs = K.sbuf(128, L_FULL * NV, F32, "vecs")
    ident_b = K.sbuf(128, 128, BF16, "identb")
    ones_b = K.sbuf(128, 128, BF16, "onesb")
    ones_f = K.sbuf(128, 128, F32, "onesf")
    maskneg_b = K.sbuf(128, 512, BF16, "maskneg")
    epsc = K.sbuf(128, 1, F32, "eps")
    ropeC = K.sbuf(128, S, F32, "ropeC")
    ropeS = K.sbuf(128, S, F32, "ropeS")
    hT = K.sbuf(128, 8 * S, BF16, "hT")
    wslots = [K.sbuf(128, WSLOT, BF16, f"wslot{i}") for i in range(NSLOT)]
    rs_sb = K.sbuf(128, 512, F32, "rs")
    ident_fb = K.sbuf(128, 128, F32, "identf")
    U_fb = K.sbuf(128, 128, F32, "Uf")
    ident_f = ident_fb.all()
    U_f = U_fb.all()
    arena0 = K.sb_off
    ARENA_END = K.sb_end
    gmask = cst[:, 256:320]
    invcnt = cst[:, 320:336]
    invf = cst[:, 336:337]
    sgn = cst[:, 337:338]

    class Arena:
        def __init__(self):
            self.off = arena0

        def alloc(self, P, F, dt, name):
            b = K.sbuf(P, F, dt, name, off=(self.off + 63) // 64 * 64)
            self.off = b.off + F * ESZ[dt]
            assert self.off <= ARENA_END, (name, self.off, ARENA_END)
            return b

    ws_i = [0]

    def wload(W2d, row0, nk, c0, ncols):
        assert nk * ncols <= WSLOT
        slot = wslots[ws_i[0] % NSLOT]
        ws_i[0] += 1
        dst = slot[:, 0:nk * ncols]
        src = W2d[row0:row0 + nk * 128, c0:c0 + ncols].rearrange("(k p) n -> p k n", p=128)
        K.G.dma_start(out=dst.r("p (k n) -> p k n", k=nk), in_=src)

        def w(k, a, b):
            return slot[:, k * ncols + a:k * ncols + b]
        return w

    evac_i = [0]

    def evac_copy(out, in_):
        evac_i[0] += 1
        if evac_i[0] % 2:
            K.A.copy(out=out, in_=in_)
        else:
            K.V.tensor_copy(out=out, in_=in_)

    def hcol(c, t0, t1):
        return hT[:, c * S + t0:c * S + t1]

    def vcol(l, j):
        return vecs[:, l * NV + j:l * NV + j + 1]

    def dbg(name, view, P, F, dt=F32):
        if name not in debug:
            return
        d = nc.dram_tensor("dbg_" + name, [P, F], dt, kind="ExternalOutput").ap()
        i = K.S.dma_start(out=dram_view(d, "dbg_" + name, 0, 1), in_=view)
        dbg_out[name] = i.tok

    def rstd_from(bank_view, n, P=128):
        K.A.activation(out=rs_sb[0:P, :], in_=bank_view, func=AF.Sqrt, bias=epsc[0:P, :], scale=1.0 / n)
        K.V.reciprocal(out=rs_sb[0:P, :], in_=rs_sb[0:P, :])

    def norm_to_h(xt, sq, l_next, gbase, tt):
        bk = rr()
        for c in range(8):
            K.A.activation(out=sq[c % 2].all(), in_=xt[:, c * TT:(c + 1) * TT], func=AF.Square)
            K.T.matmul(out=bk.all(), lhsT=ones_b.all(), rhs=sq[c % 2].all(), start=(c == 0), stop=(c == 7))
        rstd_from(bk.all(), D)
        for c in range(8):
            K.V.scalar_tensor_tensor(out=hcol(c, tt * TT, (tt + 1) * TT), in0=xt[:, c * TT:(c + 1) * TT],
                                     scalar=vcol(l_next, gbase + c), in1=rs_sb.all(), op0=ALU.mult, op1=ALU.mult)

    def xs_view(tt):
        ap = xs_d.rearrange("p (c t) -> p c t", c=8)[:, :, tt * TT:(tt + 1) * TT]
        return dram_view(ap, "xs", tt, tt + 1)

    out_toks = []

    def ckpt(name):
        if stop == name:
            raise _Stop()

    try:
        K.S.dma_start(out=cst.all(), in_=cst_in)
        K.S.dma_start(out=vecs.all(), in_=vecs_in)
        K.V.tensor_copy(out=ident_f, in_=cst[:, 0:128])
        K.V.tensor_copy(out=U_f, in_=cst[:, 128:256])
        K.V.tensor_copy(out=ident_b.all(), in_=cst[:, 0:128])
        K.V.memset(ap=ones_b.all(), constant=1.0)
        K.V.memset(ap=ones_f.all(), constant=1.0)
        K.V.memset(ap=epsc.all(), constant=EPS)
        K.V.tensor_copy(out=maskneg_b.all(), in_=cst[:, 338:850])

        A = Arena()
        pos_i = A.alloc(128, S, I32, "posi")
        ang = A.alloc(128, S, F32, "ang")
        nfl = A.alloc(128, S, F32, "nfl")
        n_i = A.alloc(128, S, I32, "ni")
        K.S.dma_start(out=pos_i.all(), in_=pos_in)
        R = slice(64, 96)
        K.V.tensor_copy(out=ang[R, :], in_=pos_i[R, :])
        K.V.tensor_scalar(out=ang[R, :], in0=ang[R, :], scalar1=cst[R, 336:337], scalar2=None, op0=ALU.mult)
        for which, table in ((0, ropeS), (1, ropeC)):
            if which == 1:
                K.V.tensor_scalar(out=ang[R, :], in0=ang[R, :], scalar1=math.pi / 2, scalar2=None, op0=ALU.add)
            K.V.tensor_scalar(out=nfl[R, :], in0=ang[R, :], scalar1=1.0 / (2 * math.pi), scalar2=None, op0=ALU.mult)
            K.V.tensor_copy(out=n_i[R, :], in_=nfl[R, :])
            K.V.tensor_copy(out=nfl[R, :], in_=n_i[R, :])
            K.V.scalar_tensor_tensor(out=table[R, :], in0=nfl[R, :], scalar=-C1_2PI, in1=ang[R, :], op0=ALU.mult, op1=ALU.add)
            K.V.scalar_tensor_tensor(out=table[R, :], in0=nfl[R, :], scalar=-C2_2PI, in1=table[R, :], op0=ALU.mult, op1=ALU.add)
            K.V.tensor_scalar(out=table[R, :], in0=table[R, :], scalar1=-3.14159, scalar2=3.14159, op0=ALU.max, op1=ALU.min)
            K.A.activation(out=table[R, :], in_=table[R, :], func=AF.Sin)
        K.V.tensor_scalar(out=ropeS[R, :], in0=ropeS[R, :], scalar1=cst[R, 337:338], scalar2=None, op0=ALU.mult)
        dbg("ropeC", ropeC[R, :], 32, S)
        dbg("ropeS", ropeS[R, :], 32, S)
        ckpt("setup")

        if not NOALIAS0:
            A = Arena()
        xt = A.alloc(128, 8 * TT, F32, "xt")
        sq = [A.alloc(128, TT, BF16, f"sq{i}") for i in range(2)]
        xin = [A.alloc(128, D, F32, f"xin{i}") for i in range(2)]
        set_rr(range(7))
        for tt in range(NTT):
            for st in range(4):
                xi = xin[(tt * 4 + st) % 2]
                r0 = (tt * 4 + st) * 128
                K.S.dma_start(out=xi.all(), in_=x_in[r0:r0 + 128, :])
                for half in range(2):
                    bk = rr()
                    for j in range(4):
                        c = half * 4 + j
                        K.T.transpose(out=bk[:, j * 128:(j + 1) * 128], in_=xi[:, c * 128:(c + 1) * 128], identity=ident_f)
                    for j in range(4):
                        c = half * 4 + j
                        evac_copy(xt[:, c * TT + st * 128:c * TT + (st + 1) * 128], bk[:, j * 128:(j + 1) * 128])
            dbg("xt0", xt.all(), 128, 8 * TT)
            ckpt("p0a")
            K.S.dma_start(out=xs_view(tt), in_=xt.all().r("p (c t) -> p c t", c=8))
            ckpt("p0b")
            norm_to_h(xt, sq, 0, 0, tt)
            ckpt("p0c")
        dbg("h0", hT.all(), 128, 8 * S, BF16)
        ckpt("phase0")

        for l in range(depth):
            A = Arena()
            ya_in = A.alloc(128, 4 * S, BF16, "ya_in")
            gla_out = A.alloc(128, 4 * S, BF16, "gla_out")
            attn_out = A.alloc(128, 4 * S, BF16, "attn_out")
            mix0 = A.off
            PAD = 16
            ubuf = [A.alloc(128, PAD + S, F32, f"ubuf{i}") for i in range(3)]
            diff = A.alloc(128, S, BF16, "diff")
            ptmp = A.alloc(128, 16, F32, "ptmp")
            set_rr(range(7))
            for i in range(3):
                K.V.memset(ap=ubuf[i][:, 0:PAD], constant=0.0)
            wpool_in = wload(W1, l * D, 8, POOL0, 512)
            wpp = wload(WP, l * 128, 1, 0, 512)
            for g in range(4):
                w = 2 ** (g + 1)
                for tt in range(NTT):
                    bk = rr()
                    for kc in range(8):
                        K.T.matmul(out=bk.all(), lhsT=wpool_in(kc, g * 128, (g + 1) * 128), rhs=hcol(kc, tt * TT, (tt + 1) * TT),
                                   start=(kc == 0), stop=(kc == 7))
                    evac_copy(ubuf[0][:, PAD + tt * TT:PAD + (tt + 1) * TT], bk.all())
                src = 0
                for j in range(g + 1):
                    d = 2 ** j
                    dst = 1 if src != 1 else 2
                    K.V.tensor_tensor(out=ubuf[dst][:, PAD:PAD + S], in0=ubuf[src][:, PAD:PAD + S],
                                      in1=ubuf[src][:, PAD - d:PAD - d + S], op=ALU.add)
                    src = dst
                sfin = ubuf[src]
                K.V.scalar_tensor_tensor(out=diff.all(), in0=sfin[:, PAD:PAD + S], scalar=1.0 / w, in1=ubuf[0][:, PAD:PAD + S],
                                         op0=ALU.mult, op1=ALU.subtract)
                K.V.tensor_tensor(out=ptmp[:, 0:w - 1], in0=sfin[:, PAD:PAD + w - 1], in1=cst[:, 320:320 + w - 1], op=ALU.mult)
                K.V.tensor_tensor(out=diff[:, 0:w - 1], in0=ptmp[:, 0:w - 1], in1=ubuf[0][:, PAD:PAD + w - 1], op=ALU.subtract)
                for tt in range(NTT):
                    bk = rr()
                    K.T.matmul(out=bk.all(), lhsT=wpp(0, g * 128, (g + 1) * 128), rhs=diff[:, tt * TT:(tt + 1) * TT], start=True, stop=True)
                    K.V.tensor_scalar(out=ya_in[:, g * S + tt * TT:g * S + (tt + 1) * TT], in0=bk.all(), scalar1=vcol(l, 32 + g),
                                      scalar2=None, op0=ALU.mult)
            if l == 0:
                dbg("ya_in", ya_in.all(), 128, 4 * S, BF16)
            ckpt("pool")

            A.off = mix0
            qdec = A.alloc(128, 2 * S, BF16, "qdec")
            kinv = A.alloc(128, 2 * S, BF16, "kinv")
            v_tm = A.alloc(128, 16 * 512, BF16, "v_tm")
            kinv_tm = A.alloc(128, 16 * 256, BF16, "kinv_tm")
            a1T = A.alloc(16, S, F32, "a1T")
            la = A.alloc(128, 256, F32, "la")
            Eq = A.alloc(128, 2 * TT, F32, "Eq")
            Ek = A.alloc(128, 2 * TT, F32, "Ek")
            dec = A.alloc(128, 2 * 32, F32, "dec")
            attm = [A.alloc(128, 64, BF16, f"attm{i}") for i in range(2)]
            S_f = [A.alloc(128, 128, F32, f"S_f{h}") for h in range(4)]
            S_t = [A.alloc(128, 128, F32, f"S_t{h}") for h in range(4)]
            S_b = [A.alloc(128, 128, BF16, f"S_b{h}") for h in range(4)]
            osq = A.alloc(128, TT, BF16, "osq")
            otmp = A.alloc(128, TT, F32, "otmp")
            wa2_sb = A.alloc(16, 256, F32, "wa2")
            ba_sb = A.alloc(1, 256, F32, "ba")
            set_rr(range(5))
            K.S.dma_start(out=wa2_sb.all(), in_=WA2[l * 16:(l + 1) * 16, :])
            K.S.dma_start(out=ba_sb.all(), in_=BA[l:l + 1, :])
            wr = wload(W1, l * D, 8, R0, 512)
            for fc in range(4):
                for tt in range(NTT):
                    bk = rr()
                    for kc in range(8):
                        K.T.matmul(out=bk.all(), lhsT=wr(kc, fc * 128, (fc + 1) * 128), rhs=hcol(kc, tt * TT, (tt + 1) * TT),
                                   start=(kc == 0), stop=(kc == 7))
                    K.A.activation(out=gla_out[:, fc * S + tt * TT:fc * S + (tt + 1) * TT], in_=bk.all(), func=AF.Silu)
            wsm = wload(W1, l * D, 8, SM0, 80)
            for tt in range(NTT):
                bk = rr()
                for kc in range(8):
                    K.T.matmul(out=bk[0:16, :], lhsT=wsm(kc, 64, 80), rhs=hcol(kc, tt * TT, (tt + 1) * TT), start=(kc == 0), stop=(kc == 7))
                evac_copy(a1T[0:16, tt * TT:(tt + 1) * TT], bk[0:16, :])
            wv = wload(W1, l * D, 8, V0, 512)
            for st in range(16):
                bk = rr()
                for kc in range(8):
                    K.T.matmul(out=bk.all(), lhsT=hcol(kc, st * 128, (st + 1) * 128), rhs=wv(kc, 0, 512), start=(kc == 0), stop=(kc == 7))
                evac_copy(v_tm[:, st * 512:(st + 1) * 512], bk.all())
            wqk = wload(W1, l * D, 8, QK0, 512)
            for tt in range(NTT):
                for s4 in range(4):
                    st = tt * 4 + s4
                    bz = rr()
                    K.T.matmul(out=bz[:, 0:256], lhsT=a1T[0:16, st * 128:(st + 1) * 128], rhs=wa2_sb.all(), start=True, stop=False)
                    K.T.matmul(out=bz[:, 0:256], lhsT=ones_f[0:1, 0:128], rhs=ba_sb.all(), start=False, stop=True)
                    K.A.activation(out=la.all(), in_=bz[:, 0:256], func=AF.Exp, scale=-1.0)
                    K.A.activation(out=la.all(), in_=la.all(), func=AF.Ln, bias=ones_f[:, 0:1], scale=1.0)
                    bc = rr()
                    for fc in range(2):
                        K.T.matmul(out=bc[:, fc * 128:(fc + 1) * 128], lhsT=la[:, fc * 128:(fc + 1) * 128], rhs=U_f, start=True, stop=True)
                    for fc in range(2):
                        K.A.activation(out=Eq[:, fc * TT + s4 * 128:fc * TT + (s4 + 1) * 128], in_=bc[:, fc * 128:(fc + 1) * 128],
                                       func=AF.Exp, scale=-1.0 / 16.0)
                        K.A.activation(out=Ek[:, fc * TT + s4 * 128:fc * TT + (s4 + 1) * 128], in_=bc[:, fc * 128:(fc + 1) * 128],
                                       func=AF.Exp, scale=1.0 / 16.0)
                        for hf in range(2):
                            n = st * 2 + hf
                            K.V.tensor_copy(out=dec[:, fc * 32 + n:fc * 32 + n + 1],
                                            in_=Eq[:, fc * TT + s4 * 128 + hf * 64 + 63:fc * TT + s4 * 128 + hf * 64 + 64])
                for fc in range(2):
                    bq = rr()
                    for kc in range(8):
                        K.T.matmul(out=bq.all(), lhsT=wqk(kc, fc * 128, (fc + 1) * 128), rhs=hcol(kc, tt * TT, (tt + 1) * TT),
                                   start=(kc == 0), stop=(kc == 7))
                    K.V.scalar_tensor_tensor(out=qdec[:, fc * S + tt * TT:fc * S + (tt + 1) * TT], in0=bq.all(), scalar=0.125,
                                             in1=Eq[:, fc * TT:(fc + 1) * TT], op0=ALU.mult, op1=ALU.mult)
                    bk2 = rr()
                    for kc in range(8):
                        K.T.matmul(out=bk2.all(), lhsT=wqk(kc, 256 + fc * 128, 256 + (fc + 1) * 128), rhs=hcol(kc, tt * TT, (tt + 1) * TT),
                                   start=(kc == 0), stop=(kc == 7))
                    K.V.tensor_tensor(out=kinv[:, fc * S + tt * TT:fc * S + (tt + 1) * TT], in0=bk2.all(), in1=Ek[:, fc * TT:(fc + 1) * TT], op=ALU.mult)
            for st in range(16):
                for fc in range(2):
                    K.T.transpose(out=bankT[:, fc * 128:(fc + 1) * 128], in_=kinv[:, fc * S + st * 128:fc * S + (st + 1) * 128], identity=ident_b.all())
                evac_copy(kinv_tm[:, st * 256:(st + 1) * 256], bankT[:, 0:256])
            if l == 0:
                dbg("qdec", qdec.all(), 128, 2 * S, BF16)
                dbg("kinv", kinv.all(), 128, 2 * S, BF16)
                dbg("dec", dec.all(), 128, 64)
            ckpt("gla1")
            set_rr(range(3))
            attm8 = [A.alloc(128, 64, BF16, f"attm8_{i}") for i in range(8)]
            for tt in range(NTT):
                for c8 in range(8):
                    n = tt * 8 + c8
                    st, hf = n // 2, n % 2
                    t0 = n * 64
                    H = slice(hf * 64, hf * 64 + 64)
                    for h in range(4):
                        fc, r0 = h // 2, (h % 2) * 64
                        P = slice(r0, r0 + 64)
                        bo = banks[3 + h]
                        qv = qdec[P, fc * S + t0:fc * S + t0 + 64]
                        ba_ = rr()
                        K.T.matmul(out=ba_[0:64, 0:64], lhsT=kinv[P, fc * S + t0:fc * S + t0 + 64], rhs=qv, start=True, stop=True)
                        am = attm8[h * 2 + n % 2]
                        K.V.tensor_tensor(out=am[H, :], in0=ba_[0:64, 0:64], in1=cst[0:64, 256:320], op=ALU.mult)
                        oc = bo[:, c8 * 64:(c8 + 1) * 64]
                        if n > 0:
                            K.T.matmul(out=oc, lhsT=S_b[h][P, :], rhs=qv, start=True, stop=False)
                        K.T.matmul(out=oc, lhsT=v_tm[H, st * 512 + h * 128:st * 512 + (h + 1) * 128], rhs=am[H, :], start=(n == 0), stop=True)
                        if n < 31:
                            bkv = rr()
                            K.T.matmul(out=bkv[0:64, 0:128], lhsT=kinv_tm[H, st * 256 + h * 64:st * 256 + (h + 1) * 64],
                                       rhs=v_tm[H, st * 512 + h * 128:st * 512 + (h + 1) * 128], start=True, stop=True)
                            dcol = dec[P, fc * 32 + n:fc * 32 + n + 1]
                            if n == 0:
                                K.V.tensor_copy(out=S_t[h][P, :], in_=bkv[0:64, 0:128])
                            else:
                                K.V.tensor_tensor(out=S_t[h][P, :], in0=bkv[0:64, 0:128], in1=S_f[h][P, :], op=ALU.add)
                            K.A.activation(out=S_b[h][P, :], in_=S_t[h][P, :], func=AF.Copy, scale=dcol)
                            K.V.tensor_scalar(out=S_f[h][P, :], in0=S_t[h][P, :], scalar1=dcol, scalar2=None, op0=ALU.mult)
                for h in range(4):
                    bo = banks[3 + h]
                    K.A.activation(out=osq.all(), in_=bo.all(), func=AF.Square)
                    bs = rr()
                    K.T.matmul(out=bs.all(), lhsT=ones_b.all(), rhs=osq.all(), start=True, stop=True)
                    rstd_from(bs.all(), 128)
                    K.V.tensor_tensor(out=otmp.all(), in0=bo.all(), in1=rs_sb.all(), op=ALU.mult)
                    go = gla_out[:, h * S + tt * TT:h * S + (tt + 1) * TT]
                    K.V.scalar_tensor_tensor(out=go, in0=otmp.all(), scalar=vcol(l, 36 + h), in1=go, op0=ALU.mult, op1=ALU.mult)
            if l == 0:
                dbg("gla_out", gla_out.all(), 128, 4 * S, BF16)
            ckpt("gla")

            A.off = mix0
            cqn = A.alloc(128, 3 * S, BF16, "cqn")
            ckvn = A.alloc(128, 2 * S, BF16, "ckvn")
            krope = A.alloc(128, S, BF16, "krope")
            Qh = [A.alloc(128, S, BF16, f"Qh{i}") for i in range(2)]
            Kh = [A.alloc(128, S, BF16, f"Kh{i}") for i in range(2)]
            Vh = [A.alloc(128, 16 * 128, BF16, f"Vh{i}") for i in range(2)]
            pt = [A.alloc(128, TT, BF16, f"pt{i}") for i in range(3)]
            sqh = A.alloc(128, S, BF16, "sqh")
            rt1 = A.alloc(128, TT, F32, "rt1")
            rt2 = A.alloc(128, TT, F32, "rt2")
            rden = A.alloc(128, TT, F32, "rden")
            nrow = A.alloc(1, S, F32, "nrow")
            kmx = A.alloc(1, 8, F32, "kmx")
            negm = [A.alloc(1, S, BF16, f"negm{i}") for i in range(2)]
            set_rr(range(5))
            for i in range(2):
                K.V.memset(ap=Vh[i].all().r("p (s e) -> p s e", e=128).sub(lambda ap: ap[:, :, 64:128]), constant=1.0)
            for (col0, nch, dstb, gb, nfeat) in ((CQ0, 3, cqn, 40, 384), (CKV0, 2, ckvn, 43, 256)):
                wc = wload(W1, l * D, 8, col0, nch * 128)
                for tt in range(NTT):
                    pb = [rr() for _ in range(nch)]
                    for c in range(nch):
                        for kc in range(8):
                            K.T.matmul(out=pb[c].all(), lhsT=wc(kc, c * 128, (c + 1) * 128), rhs=hcol(kc, tt * TT, (tt + 1) * TT),
                                       start=(kc == 0), stop=(kc == 7))
                    bs = rr()
                    for c in range(nch):
                        mq = pt[c]
                        K.A.activation(out=mq.all(), in_=pb[c].all(), func=AF.Square)
                        K.T.matmul(out=bs.all(), lhsT=ones_b.all(), rhs=mq.all(), start=(c == 0), stop=(c == nch - 1))
                    rstd_from(bs.all(), nfeat)
                    for c in range(nch):
                        K.V.scalar_tensor_tensor(out=dstb[:, c * S + tt * TT:c * S + (tt + 1) * TT], in0=pb[c].all(), scalar=vcol(l, gb + c),
                                                 in1=rs_sb.all(), op0=ALU.mult, op1=ALU.mult)
            wsm = wload(W1, l * D, 8, SM0, 80)
            R = slice(64, 96)
            for tt in range(NTT):
                bk = rr()
                for kc in range(8):
                    K.T.matmul(out=bk[0:64, :], lhsT=wsm(kc, 0, 64), rhs=hcol(kc, tt * TT, (tt + 1) * TT), start=(kc == 0), stop=(kc == 7))
                K.V.tensor_tensor(out=rt1[R, :], in0=bk[0:32, :], in1=ropeC[R, tt * TT:(tt + 1) * TT], op=ALU.mult)
                K.V.tensor_tensor(out=rt2[R, :], in0=bk[32:64, :], in1=ropeS[R, tt * TT:(tt + 1) * TT], op=ALU.mult)
                K.V.tensor_tensor(out=krope[R, tt * TT:(tt + 1) * TT], in0=rt1[R, :], in1=rt2[R, :], op=ALU.add)
            wuq = wload(WUQ, l * 384, 3, 0, 768)
            wuqs = wload(WUQS, l * 384, 3, 0, 768)
            wukv = wload(WUKV, l * 256, 2, 0, 1024)
            SCALE = 96.0 ** -0.5
            def mla_prep(h):
                Q, Kt, V = Qh[h % 2], Kh[h % 2], Vh[h % 2]
                for tt in range(NTT):
                    T = slice(tt * TT, (tt + 1) * TT)
                    bq, bqs, bkk = rr(), rr(), rr()
                    for c in range(3):
                        K.T.matmul(out=bq[0:96, :], lhsT=wuq(c, h * 96, (h + 1) * 96), rhs=cqn[:, c * S + tt * TT:c * S + (tt + 1) * TT],
                                   start=(c == 0), stop=(c == 2))
                    for c in range(3):
                        K.T.matmul(out=bqs[0:96, :], lhsT=wuqs(c, h * 96, (h + 1) * 96), rhs=cqn[:, c * S + tt * TT:c * S + (tt + 1) * TT],
                                   start=(c == 0), stop=(c == 2))
                    for c in range(2):
                        K.T.matmul(out=bkk[0:64, :], lhsT=wukv(c, h * 64, (h + 1) * 64), rhs=ckvn[:, c * S + tt * TT:c * S + (tt + 1) * TT],
                                   start=(c == 0), stop=(c == 1))
                    K.A.copy(out=Q[0:64, T], in_=bq[0:64, :])
                    K.V.tensor_tensor(out=rt1[R, :], in0=bq[R, :], in1=ropeC[R, T], op=ALU.mult)
                    K.V.tensor_tensor(out=rt2[R, :], in0=bqs[R, :], in1=ropeS[R, T], op=ALU.mult)
                    K.V.tensor_tensor(out=Q[R, T], in0=rt1[R, :], in1=rt2[R, :], op=ALU.add)
                    K.A.copy(out=Kt[0:64, T], in_=bkk[0:64, :])
                    K.V.tensor_copy(out=Kt[R, T], in_=krope[R, T])
                for st in range(16):
                    bv = rr()
                    for c in range(2):
                        K.T.matmul(out=bv[:, 0:64], lhsT=ckvn[:, c * S + st * 128:c * S + (st + 1) * 128], rhs=wukv(c, 512 + h * 64, 512 + (h + 1) * 64),
                                   start=(c == 0), stop=(c == 1))
                    evac_copy(V[:, st * 128:st * 128 + 64], bv[:, 0:64])
                K.A.activation(out=sqh[0:96, :], in_=Kt[0:96, :], func=AF.Square)
                for tt in range(NTT):
                    bn = rr()
                    K.T.matmul(out=bn[0:1, :], lhsT=ones_b[0:96, 0:1], rhs=sqh[0:96, tt * TT:(tt + 1) * TT], start=True, stop=True)
                    K.V.tensor_reduce(out=kmx[0:1, tt:tt + 1], in_=bn[0:1, :], axis=AX.X, op=ALU.max)
                K.V.tensor_reduce(out=kmx[0:1, 4:5], in_=kmx[0:1, 0:4], axis=AX.X, op=ALU.max)
                K.A.activation(out=sqh[0:96, :], in_=Q[0:96, :], func=AF.Square)
                for tt in range(NTT):
                    bn = rr()
                    K.T.matmul(out=bn[0:1, :], lhsT=ones_b[0:96, 0:1], rhs=sqh[0:96, tt * TT:(tt + 1) * TT], start=True, stop=True)
                    K.V.tensor_scalar(out=nrow[0:1, tt * TT:(tt + 1) * TT], in0=bn[0:1, :], scalar1=kmx[0:1, 4:5], scalar2=None, op0=ALU.mult)
                K.A.activation(out=nrow.all(), in_=nrow.all(), func=AF.Sqrt)
                K.V.tensor_scalar(out=negm[h % 2].all(), in0=nrow.all(), scalar1=-1.0, scalar2=None, op0=ALU.mult)

            pt_i = [0]

            def mla_attend(h):
                Q, Kt, V = Qh[h % 2], Kh[h % 2], Vh[h % 2]
                iters = [(qb, kt) for qb in range(4) for kt in range(4 * qb + 4)]
                sbank = {}
                LOOK = 2

                def score(i):
                    qb, kt = iters[i]
                    r = kt - 4 * qb
                    c0 = max(r, 0) * 128
                    q0 = qb * TT + c0
                    bs_ = rr()
                    sbank[i] = bs_
                    K.T.matmul(out=bs_[:, c0:TT], lhsT=Kt[0:96, kt * 128:(kt + 1) * 128], rhs=Q[0:96, q0:(qb + 1) * TT], start=True, stop=False)
                    K.T.matmul(out=bs_[:, c0:TT], lhsT=ones_b[0:1, 0:128], rhs=negm[h % 2][0:1, q0:(qb + 1) * TT], start=False, stop=(r < 0))
                    if r >= 0:
                        K.T.matmul(out=bs_[:, c0:TT], lhsT=ident_b.all(), rhs=maskneg_b[:, 0:TT - c0], start=False, stop=True)

                def exp_pv(i):
                    qb, kt = iters[i]
                    nk = 4 * qb + 4
                    r = kt - 4 * qb
                    c0 = max(r, 0) * 128
                    bo = banks[5 + (h * 4 + qb) % 2]
                    bs_ = sbank.pop(i)
                    p = pt[pt_i[0] % 3]
                    pt_i[0] += 1
                    K.A.activation(out=p[:, c0:TT], in_=bs_[:, c0:TT], func=AF.Exp, scale=SCALE)
                    K.T.matmul(out=bo[:, c0:TT], lhsT=V[:, kt * 128:(kt + 1) * 128], rhs=p[:, c0:TT], start=(kt == 0), stop=(kt == nk - 1))
                    if kt == nk - 1:
                        K.V.reciprocal(out=rden[64:128, :], in_=bo[64:128, :])
                        rr0 = (h % 2) * 64
                        K.V.tensor_tensor(out=attn_out[rr0:rr0 + 64, (h // 2) * S + qb * TT:(h // 2) * S + (qb + 1) * TT], in0=bo[0:64, :],
                                          in1=rden[64:128, :], op=ALU.mult)

                for i in range(len(iters) + LOOK):
                    if i < len(iters):
                        score(i)
                    if i - LOOK >= 0:
                        exp_pv(i - LOOK)

            mla_prep(0)
            for h in range(8):
                if h + 1 < 8:
                    mla_prep(h + 1)
                mla_attend(h)
            if l == 0:
                dbg("attn_out", attn_out.all(), 128, 4 * S, BF16)
            ckpt("mla")

            A.off = mix0
            merged = A.alloc(128, 8 * S, BF16, "merged")
            sig = [A.alloc(128, TT, F32, f"sig{i}") for i in range(3)]
            acc = [A.alloc(128, TT, F32, f"acc{i}") for i in range(2)]
            yins = (ya_in, gla_out, attn_out)
            set_rr(range(7))
            for m in range(8):
                wg = wload(W1, l * D, 8, G0 + m * 384, 384)
                wy = wload(WABC, l * 1536, 12, m * 128, 128)
                for tt in range(NTT):
                    ac = acc[(m * NTT + tt) % 2]
                    for b in range(3):
                        bg, by = rr(), rr()
                        for kc in range(8):
                            K.T.matmul(out=bg.all(), lhsT=wg(kc, b * 128, (b + 1) * 128), rhs=hcol(kc, tt * TT, (tt + 1) * TT),
                                       start=(kc == 0), stop=(kc == 7))
                        for c in range(4):
                            K.T.matmul(out=by.all(), lhsT=wy(b * 4 + c, 0, 128), rhs=yins[b][:, c * S + tt * TT:c * S + (tt + 1) * TT],
                                       start=(c == 0), stop=(c == 3))
                        K.A.activation(out=sig[b].all(), in_=bg.all(), func=AF.Sigmoid)
                        if b == 0:
                            K.V.tensor_tensor(out=ac.all(), in0=by.all(), in1=sig[b].all(), op=ALU.mult)
                        else:
                            K.V.tensor_tensor(out=sig[b].all(), in0=by.all(), in1=sig[b].all(), op=ALU.mult)
                            if b == 1:
                                K.V.tensor_tensor(out=ac.all(), in0=ac.all(), in1=sig[b].all(), op=ALU.add)
                            else:
                                K.V.tensor_tensor(out=merged[:, m * S + tt * TT:m * S + (tt + 1) * TT], in0=ac.all(), in1=sig[b].all(), op=ALU.add)
            if l == 0:
                dbg("merged", merged.all(), 128, 8 * S, BF16)
            ckpt("merge")

            xt = A.alloc(128, 8 * TT, F32, "xt")
            tt_ = A.alloc(128, 8 * TT, F32, "ttile")
            sq = [A.alloc(128, TT, BF16, f"sq{i}") for i in range(2)]
            tt2 = K.sbuf(128, 8 * TT, F32, "ttile2", off=ya_in.off)
            sqn = [A.alloc(128, TT, BF16, f"sqn{i}") for i in range(2)]
            tts = [tt_, tt2]
            wo0 = wload(WO, l * D, 8, 0, 512)
            wo1 = wload(WO, l * D, 8, 512, 512)
            set_rr(range(5))

            def wo_groups(tt):
                tb, bsum = tts[tt % 2], banks[5 + tt % 2]
                for m in range(8):
                    wo_ = wo0 if m < 4 else wo1
                    bk = rr()
                    for kc in range(8):
                        K.T.matmul(out=bk.all(), lhsT=wo_(kc, (m % 4) * 128, (m % 4 + 1) * 128), rhs=merged[:, kc * S + tt * TT:kc * S + (tt + 1) * TT],
                                   start=(kc == 0), stop=(kc == 7))
                    K.V.tensor_copy(out=tb[:, m * TT:(m + 1) * TT], in_=bk.all())
                    K.A.activation(out=sq[m % 2].all(), in_=bk.all(), func=AF.Square)
                    if m > 0:
                        K.T.matmul(out=bsum.all(), lhsT=ones_b.all(), rhs=sq[(m - 1) % 2].all(), start=(m == 1), stop=False)
                K.T.matmul(out=bsum.all(), lhsT=ones_b.all(), rhs=sq[7 % 2].all(), start=False, stop=True)

            def wo_finish(tt):
                tb, bsum = tts[tt % 2], banks[5 + tt % 2]
                K.S.dma_start(out=xt.all().r("p (c t) -> p c t", c=8), in_=xs_view(tt))
                rstd_from(bsum.all(), D)
                for m in range(8):
                    K.V.scalar_tensor_tensor(out=tb[:, m * TT:(m + 1) * TT], in0=tb[:, m * TT:(m + 1) * TT], scalar=vcol(l, 8 + m),
                                             in1=rs_sb.all(), op0=ALU.mult, op1=ALU.mult)
                    K.V.tensor_tensor(out=xt[:, m * TT:(m + 1) * TT], in0=xt[:, m * TT:(m + 1) * TT], in1=tb[:, m * TT:(m + 1) * TT], op=ALU.add)
                K.S.dma_start(out=xs_view(tt), in_=xt.all().r("p (c t) -> p c t", c=8))
                norm_to_h(xt, sqn, l, 16, tt)

            wo_groups(0)
            for tt in range(NTT):
                if tt + 1 < NTT:
                    wo_groups(tt + 1)
                wo_finish(tt)
            if l == 0:
                dbg("h2", hT.all(), 128, 8 * S, BF16)
            ckpt("wo")

            A = Arena()
            act = A.alloc(128, 22 * S, BF16, "act")
            xt = A.alloc(128, 8 * TT, F32, "xt")
            ft = A.alloc(128, 8 * TT, F32, "ftile")
            sq = [A.alloc(128, TT, BF16, f"sq{i}") for i in range(2)]
            sg = [A.alloc(128, TT, F32, f"sg{i}") for i in range(2)]
            otile = [K.sbuf(128, D, F32, f"otile{i}", off=ft.off + i * D * 4) for i in range(2)]
            set_rr(range(7))
            for j in range(11):
                wgu = wload(WGU, l * D, 8, j * 512, 512)
                for pp in range(2):
                    jj = 2 * j + pp
                    for tt in range(NTT):
                        bg, bu = rr(), rr()
                        for kc in range(8):
                            K.T.matmul(out=bg.all(), lhsT=wgu(kc, pp * 256, pp * 256 + 128), rhs=hcol(kc, tt * TT, (tt + 1) * TT),
                                       start=(kc == 0), stop=(kc == 7))
                        for kc in range(8):
                            K.T.matmul(out=bu.all(), lhsT=wgu(kc, pp * 256 + 128, pp * 256 + 256), rhs=hcol(kc, tt * TT, (tt + 1) * TT),
                                       start=(kc == 0), stop=(kc == 7))
                        s_ = sg[(jj * NTT + tt) % 2]
                        K.A.activation(out=s_.all(), in_=bg.all(), func=AF.Silu)
                        K.V.tensor_tensor(out=act[:, jj * S + tt * TT:jj * S + (tt + 1) * TT], in0=bu.all(), in1=s_.all(), op=ALU.mult)
            for tt in range(NTT):
                K.S.dma_start(out=xt.all().r("p (c t) -> p c t", c=8), in_=xs_view(tt))
                bsum = banks[6]
                set_rr(range(6))
                for m in range(8):
                    wd = wload(WD, l * DFF, 22, m * 128, 128)
                    bk = rr()
                    for kc in range(22):
                        K.T.matmul(out=bk.all(), lhsT=wd(kc, 0, 128), rhs=act[:, kc * S + tt * TT:kc * S + (tt + 1) * TT], start=(kc == 0), stop=(kc == 21))
                    K.V.tensor_copy(out=ft[:, m * TT:(m + 1) * TT], in_=bk.all())
                    K.A.activation(out=sq[m % 2].all(), in_=bk.all(), func=AF.Square)
                    if m > 0:
                        K.T.matmul(out=bsum.all(), lhsT=ones_b.all(), rhs=sq[(m - 1) % 2].all(), start=(m == 1), stop=False)
                K.T.matmul(out=bsum.all(), lhsT=ones_b.all(), rhs=sq[7 % 2].all(), start=False, stop=True)
                rstd_from(bsum.all(), D)
                for m in range(8):
                    K.V.scalar_tensor_tensor(out=ft[:, m * TT:(m + 1) * TT], in0=ft[:, m * TT:(m + 1) * TT], scalar=vcol(l, 24 + m),
                                             in1=rs_sb.all(), op0=ALU.mult, op1=ALU.mult)
                    K.V.tensor_tensor(out=xt[:, m * TT:(m + 1) * TT], in0=xt[:, m * TT:(m + 1) * TT], in1=ft[:, m * TT:(m + 1) * TT], op=ALU.add)
                if l < depth - 1:
                    K.S.dma_start(out=xs_view(tt), in_=xt.all().r("p (c t) -> p c t", c=8))
                    norm_to_h(xt, sq, l + 1, 0, tt)
                else:
                    for s4 in range(4):
                        ot = otile[s4 % 2]
                        for half in range(2):
                            bk = rr()
                            for j in range(4):
                                c = half * 4 + j
                                K.T.transpose(out=bk[:, j * 128:(j + 1) * 128], in_=xt[:, c * TT + s4 * 128:c * TT + (s4 + 1) * 128], identity=ident_f)
                            evac_copy(ot[:, half * 512:(half + 1) * 512], bk.all())
                        r0 = (tt * 4 + s4) * 128
                        i = K.S.dma_start(out=dram_view(out_d[r0:r0 + 128, :], "out", r0, r0 + 128), in_=ot.all())
                        out_toks.append(i.tok)

    except _Stop:
        pass
    K.wait_all("sync", out_toks + list(dbg_out.values()))
    K.emit()
    K.close()
    return nc


def host_prep(inputs):
    f = np.float32
    w_in = np.asarray(inputs["w_in"], f)
    Ld = w_in.shape[0]
    O_POOL, O_Q, O_K, O_V, O_R, O_A1, O_CQ, O_CKV, O_KR, O_G = 0, 512, 768, 1024, 1536, 2048, 2064, 2448, 2704, 2736
    idx = []
    idx += list(range(O_POOL, O_POOL + 512))
    idx += list(range(O_Q, O_Q + 256)) + list(range(O_K, O_K + 256))
    idx += list(range(O_R, O_R + 512))
    idx += list(range(O_V, O_V + 512))
    idx += list(range(O_CQ, O_CQ + 384))
    idx += list(range(O_CKV, O_CKV + 256))
    idx += list(range(O_KR, O_KR + 32))
    idx += list(range(O_KR + 16, O_KR + 32)) + list(range(O_KR, O_KR + 16))
    idx += list(range(O_A1, O_A1 + 16))
    for m in range(8):
        for b in range(3):
            idx += list(range(O_G + b * 1024 + m * 128, O_G + b * 1024 + (m + 1) * 128))
    idx = np.asarray(idx)
    assert idx.shape[0] == W1C
    w1 = np.ascontiguousarray(w_in[:, :, idx]).reshape(Ld * D, W1C)
    wp = np.ascontiguousarray(np.asarray(inputs["w_pool"], f).transpose(0, 2, 1, 3)).reshape(Ld * 128, 512)
    wabc = np.concatenate([np.asarray(inputs["w_a"], f), np.asarray(inputs["w_b"], f), np.asarray(inputs["w_c"], f)], axis=1).reshape(Ld * 1536, D)
    wa2 = np.ascontiguousarray(np.asarray(inputs["w_gla_a2"], f)).reshape(Ld * 16, 256)
    ba = np.ascontiguousarray(np.asarray(inputs["b_gla_a"], f)).reshape(Ld, 256)
    w_uq = np.asarray(inputs["w_mla_uq"], f)
    sw = []
    for h in range(8):
        sw += list(range(h * 96, h * 96 + 64)) + list(range(h * 96 + 80, h * 96 + 96)) + list(range(h * 96 + 64, h * 96 + 80))
    wuq = np.ascontiguousarray(w_uq).reshape(Ld * 384, 768)
    wuqs = np.ascontiguousarray(w_uq[:, :, np.asarray(sw)]).reshape(Ld * 384, 768)
    pk = []
    for h in range(8):
        pk += list(range(h * 128, h * 128 + 64))
    for h in range(8):
        pk += list(range(h * 128 + 64, h * 128 + 128))
    wukv = np.ascontiguousarray(np.asarray(inputs["w_mla_ukv"], f)[:, :, np.asarray(pk)]).reshape(Ld * 256, 1024)
    wo = np.ascontiguousarray(np.asarray(inputs["w_o"], f)).reshape(Ld * D, D)
    pg = []
    for j in range(22):
        pg += list(range(j * 128, (j + 1) * 128)) + list(range(DFF + j * 128, DFF + (j + 1) * 128))
    wgu = np.ascontiguousarray(np.asarray(inputs["w_ffn_gu"], f)[:, :, np.asarray(pg)]).reshape(Ld * D, 2 * DFF)
    wd = np.ascontiguousarray(np.asarray(inputs["w_ffn_down"], f)).reshape(Ld * DFF, D)
    vecs = np.zeros((128, Ld * NV), f)
    for l in range(Ld):
        cols = []
        for name, n in (("norm_pre_mix", 8), ("norm_post_mix", 8), ("norm_pre_ffn", 8), ("norm_post_ffn", 8), ("pool_scale", 4),
                        ("gla_norm", 4), ("mla_q_norm", 3), ("mla_kv_norm", 2)):
            v = np.asarray(inputs[name], f)[l]
            cols.append(v.reshape(n, 128).T)
        vecs[:, l * NV:(l + 1) * NV] = np.concatenate(cols, axis=1)
    cst = np.zeros((128, NCST), f)
    cst[:, 0:128] = np.eye(128, dtype=f)
    s = np.arange(128)
    cst[:, 128:256] = ((s[:, None] // 64 == s[None, :] // 64) & (s[:, None] <= s[None, :])).astype(f)
    cst[:, 256:320] = ((s[:, None] % 64) <= np.arange(64)[None, :]).astype(f)
    cst[:, 320:336] = (1.0 / (np.arange(16) + 1.0)).astype(f)[None, :]
    invf = (10000.0 ** (-np.arange(0, 32, 2, dtype=np.float32) / 32.0)).astype(f)
    cst[64:80, 336] = invf
    cst[80:96, 336] = invf
    cst[64:80, 337] = -1.0
    cst[80:96, 337] = 1.0
    q = np.arange(128)
    cst[:, 338:338 + 128] = np.where(q[None, :] < s[:, None], -30000.0, 0.0).astype(f)
    shared = dict(cst=cst, vecs=vecs, w1=w1, wp=wp, wabc=wabc, wa2=wa2, ba=ba, wuq=wuq, wuqs=wuqs, wukv=wukv, wo=wo, wgu=wgu, wd=wd)
    return shared


def kernel(**inputs):
    x = np.asarray(inputs["x"], np.float32)
    pos = np.asarray(inputs["positions"], np.int32)
    B = x.shape[0]
    shared = host_prep(inputs)
    nc = build_program(L_FULL)
    in_maps = []
    for b in range(B):
        m = dict(shared)
        m["x"] = np.ascontiguousarray(x[b])
        m["pos"] = np.ascontiguousarray(np.broadcast_to(pos[b][None, :], (128, S)))
        in_maps.append(m)
    res = run_bass_kernel_spmd(nc, in_maps, core_ids=list(range(B)))
    return np.stack([np.asarray(r["out"], np.float32) for r in res.results], axis=0)
```
